# Optimizing a Trainium2 kernel written in Bass

```python
import jax, jax.numpy as jnp
from jax import lax
import numpy as np

D_MODEL = 2048
BATCH = 8
SEQ = 4096
DEPTH = 1

CHUNK = 64
Q_BLOCK = 128
FOX_HEADS = 8
FOX_HEAD_DIM = 128
FOX_WIDTH = FOX_HEADS * FOX_HEAD_DIM
CONV_WIDTH = 1024
CONV_K = 3
N_BRANCHES = 2
PLE_DIM = 256
N_GROUPS = 4
EXPERTS_PER_GROUP = 8
N_EXPERTS = N_GROUPS * EXPERTS_PER_GROUP
TOP_K_IN_GROUP = 2
EXPERT_FF = 512
NORM_EPS = 1e-6
IN_COLS = 3 * FOX_WIDTH + FOX_HEADS + 3 * CONV_WIDTH + N_BRANCHES * D_MODEL

kernel_name = "hybrid_fox_shortconv_hiermoe_block"


def rmsnorm(x, g):
    xf = x.astype(jnp.float32)
    y = xf * lax.rsqrt(jnp.mean(xf * xf, axis=-1, keepdims=True) + NORM_EPS)
    return (y * g.astype(jnp.float32)).astype(x.dtype)


def split_points():
    sizes = [FOX_WIDTH, FOX_WIDTH, FOX_WIDTH, FOX_HEADS, CONV_WIDTH, CONV_WIDTH, CONV_WIDTH]
    return tuple(int(v) for v in np.cumsum(sizes))


def fox_attention(q, k, v, log_f):
    b, s, h, dh = q.shape
    nb = s // Q_BLOCK
    c = jnp.cumsum(log_f, axis=1).transpose(0, 2, 1)
    scale = dh ** -0.5
    kpos = jnp.arange(s)
    q_blocks = q.reshape(b, nb, Q_BLOCK, h, dh).transpose(1, 0, 2, 3, 4)
    c_blocks = c.reshape(b, h, nb, Q_BLOCK).transpose(2, 0, 1, 3)
    starts = jnp.arange(nb) * Q_BLOCK

    def block(args):
        qb, cb, start = args
        logits = jnp.einsum('bqhd,bkhd->bhqk', qb, k).astype(jnp.float32) * scale
        logits = logits + cb[..., :, None] - c[:, :, None, :]
        qpos = start + jnp.arange(Q_BLOCK)
        mask = kpos[None, :] <= qpos[:, None]
        logits = jnp.where(mask, logits, -jnp.inf)
        probs = jax.nn.softmax(logits, axis=-1).astype(v.dtype)
        return jnp.einsum('bhqk,bkhd->bqhd', probs, v)

    out = lax.map(block, (q_blocks, c_blocks, starts))
    return out.transpose(1, 0, 2, 3, 4).reshape(b, s, h * dh)


def causal_short_conv(z, w):
    s = z.shape[1]
    zp = jnp.pad(z, ((0, 0), (CONV_K - 1, 0), (0, 0)))
    return sum(w[j] * zp[:, j:j + s] for j in range(CONV_K))


def hier_moe(hn, w_rg, b_rg, w_re, b_re, w_gu, w_down):
    t = hn.shape[0]
    g_logits = (hn @ w_rg).astype(jnp.float32) + b_rg
    g_probs = jax.nn.softmax(g_logits, axis=-1)
    grp = jnp.argmax(g_logits, axis=-1).astype(jnp.int32)
    p_grp = jnp.take_along_axis(g_probs, grp[:, None], axis=1)
    e_logits = ((hn @ w_re).astype(jnp.float32) + b_re).reshape(t, N_GROUPS, EXPERTS_PER_GROUP)
    e_sel = jnp.take_along_axis(e_logits, grp[:, None, None], axis=1)[:, 0]
    e_probs = jax.nn.softmax(e_sel, axis=-1)
    vals, idx = lax.top_k(e_probs, TOP_K_IN_GROUP)
    wts = p_grp * vals / jnp.sum(vals, axis=-1, keepdims=True)
    eid = grp[:, None] * EXPERTS_PER_GROUP + idx.astype(jnp.int32)

    flat_e = eid.reshape(-1)
    order = jnp.argsort(flat_e)
    tok = order // TOP_K_IN_GROUP
    group_sizes = jnp.bincount(flat_e, length=N_EXPERTS).astype(jnp.int32)
    xs = hn[tok]
    gu = lax.ragged_dot(xs, w_gu, group_sizes)
    gate, up = jnp.split(gu, 2, axis=-1)
    ys = lax.ragged_dot(jax.nn.silu(gate) * up, w_down, group_sizes)
    ys = ys * wts.reshape(-1)[order][:, None].astype(ys.dtype)
    return jax.ops.segment_sum(ys, tok, num_segments=t)


def setup_inputs(seed: int = 0) -> dict:
    key = jax.random.key(seed)
    ks = jax.random.split(key, 20)
    f32 = jnp.float32
    D, L = D_MODEL, DEPTH
    nrm = lambda k, shape, fan_in: jax.random.normal(k, shape, f32) * (fan_in ** -0.5)
    gain = lambda k, shape: 1.0 + 0.01 * jax.random.normal(k, shape, f32)
    return {
        "x": jax.random.normal(ks[0], (BATCH, SEQ, D), f32),
        "p": jax.random.normal(ks[1], (DEPTH, BATCH, SEQ, PLE_DIM), f32),
        "g_mix": gain(ks[2], (L, D)),
        "w_in": nrm(ks[3], (L, D, IN_COLS), D),
        "b_f": jax.random.uniform(ks[4], (L, FOX_HEADS), f32, 1.0, 4.0),
        "conv_w": nrm(ks[5], (L, CONV_K, CONV_WIDTH), CONV_K),
        "w_branch_a": nrm(ks[6], (L, FOX_WIDTH, D), FOX_WIDTH),
        "w_branch_b": nrm(ks[7], (L, CONV_WIDTH, D), CONV_WIDTH),
        "w_out": nrm(ks[8], (L, D, D), D),
        "g_ffn": gain(ks[9], (L, D)),
        "w_router_group": nrm(ks[10], (L, D, N_GROUPS), D),
        "b_router_group": 0.01 * jax.random.normal(ks[11], (L, N_GROUPS), f32),
        "w_router_expert": nrm(ks[12], (L, D, N_EXPERTS), D),
        "b_router_expert": 0.01 * jax.random.normal(ks[13], (L, N_EXPERTS), f32),
        "w_gate_up": nrm(ks[14], (L, N_EXPERTS, D, 2 * EXPERT_FF), D),
        "w_down": nrm(ks[15], (L, N_EXPERTS, EXPERT_FF, D), EXPERT_FF),
        "g_ple": gain(ks[16], (L, D)),
        "w_ple_gate": nrm(ks[17], (L, D, D), D),
        "w_ple_proj": nrm(ks[18], (L, PLE_DIM, D), PLE_DIM),
        "g_final": gain(ks[19], (D,)),
    }


def reference(x, p, g_mix, w_in, b_f, conv_w, w_branch_a, w_branch_b, w_out, g_ffn,
              w_router_group, b_router_group, w_router_expert, b_router_expert,
              w_gate_up, w_down, g_ple, w_ple_gate, w_ple_proj, g_final):
    b, s, d = x.shape
    cuts = split_points()
    for i in range(DEPTH):
        h = rmsnorm(x, g_mix[i])
        proj = h @ w_in[i]
        q, k, v, f_logit, u, bg, cg, gate_logit = jnp.split(proj, cuts, axis=-1)
        log_f = jax.nn.log_sigmoid(f_logit.astype(jnp.float32) + b_f[i])
        shp = (b, s, FOX_HEADS, FOX_HEAD_DIM)
        attn = fox_attention(q.reshape(shp), k.reshape(shp), v.reshape(shp), log_f)
        conv = bg * causal_short_conv(cg * u, conv_w[i])
        ga, gb = jnp.split(jax.nn.sigmoid(gate_logit), N_BRANCHES, axis=-1)
        merged = ga * (attn @ w_branch_a[i]) + gb * (conv @ w_branch_b[i])
        x = x + merged @ w_out[i]
        hn = rmsnorm(x, g_ffn[i]).reshape(b * s, d)
        x = x + hier_moe(hn, w_router_group[i], b_router_group[i], w_router_expert[i],
                         b_router_expert[i], w_gate_up[i], w_down[i]).reshape(b, s, d)
        hp = rmsnorm(x, g_ple[i])
        x = x + jax.nn.sigmoid(hp @ w_ple_gate[i]) * (p[i] @ w_ple_proj[i])
    return rmsnorm(x, g_final)
```

```python
import contextlib
import numpy as np
import concourse.bass as bass
import concourse.mybir as mybir
from concourse.bass_utils import run_bass_kernel_spmd

F32 = mybir.dt.float32
BF16 = mybir.dt.bfloat16
I32 = mybir.dt.int32
ALU = mybir.AluOpType
AF = mybir.ActivationFunctionType
AX = mybir.AxisListType

ENGS = ["tensor", "vector", "scalar", "gpsimd", "sync"]

T = 4096
D = 2048
KC = 16
NH = 8
NE = 32
CAP = 384
CS = CAP + 1
NRT = 97
TW = 128
EPS = 1e-6
QSCALE = 128 ** -0.5
C_ID, C_ONE, C_TRI, C_US, C_IOTA, C_ES = 0, 128, 256, 384, 512, 513
NCF = 545


class Buf:
    __slots__ = ("name", "w", "r", "sem", "cnt", "multi")

    def __init__(self, name, multi=False):
        self.name = name
        self.w = {}
        self.r = {}
        self.sem = None
        self.cnt = 0
        self.multi = multi


class Op:
    __slots__ = ("eng", "fn", "deps", "dma", "signal", "idx", "seq", "phase")


class Prog:
    def __init__(self, nc, stack):
        self.nc = nc
        self.stack = stack
        self.ops = {e: [] for e in ENGS}
        self.esem = {e: stack.enter_context(nc.semaphore("es_" + e)) for e in ENGS}
        self.ecnt = {e: 0 for e in ENGS}
        self.nsem = len(ENGS)
        self.phase = 0
        self.seq = 0
        self.dmabufs = []
        self.stats = []

    def _mk(self, eng, fn, deps, dma):
        o = Op()
        o.eng = eng
        o.fn = fn
        o.dma = dma
        o.signal = False
        o.idx = 0
        o.deps = deps
        o.phase = self.phase
        self.seq += 1
        o.seq = self.seq
        if dma is not None:
            if dma.sem is None:
                dma.sem = self.stack.enter_context(self.nc.semaphore("ds%d" % self.nsem))
                self.nsem += 1
                self.dmabufs.append(dma)
            dma.cnt += 16
        self.ops[eng].append(o)
        return o

    def op(self, eng, fn, reads=(), writes=(), dma=None):
        deps = {}

        def add(d):
            if d.phase < self.phase:
                return
            if d.dma is not None:
                deps[id(d.dma)] = ("d", d.dma, d.dma.cnt)
            else:
                if d.eng == "tensor" and eng == "tensor" and dma is None:
                    return
                prev = deps.get(d.eng)
                if prev is None or prev[1].seq < d.seq:
                    deps[d.eng] = ("e", d, 0)

        for b in reads:
            for d in b.w.values():
                add(d)
        for b in writes:
            if not b.multi:
                for d in b.w.values():
                    add(d)
            for d in b.r.values():
                add(d)
        o = self._mk(eng, fn, list(deps.values()), dma)
        key = id(dma) if dma is not None else eng
        for b in reads:
            b.r[key] = o
        for b in writes:
            if b.multi:
                b.w[key] = o
            else:
                b.w = {key: o}
                b.r = {}
        return o

    def barrier(self):
        a = {}
        for e in ENGS:
            a[e] = self._mk(e, (lambda eng: eng.drain()), [], None)
        for e in ENGS:
            deps = [("e", a[e2], 0) for e2 in ENGS if e2 != e]
            deps += [("d", b, b.cnt) for b in self.dmabufs if b.cnt > 0]
            self._mk(e, (lambda eng: eng.nop()), deps, None)

    def emit(self):
        nc = self.nc
        for e in ENGS:
            for o in self.ops[e]:
                for dep in o.deps:
                    if dep[0] == "e":
                        dep[1].signal = True
        for e in ENGS:
            for o in self.ops[e]:
                if o.signal and o.dma is None:
                    self.ecnt[e] += 1
                    o.idx = self.ecnt[e]
        stats = {}
        with nc.Block() as block:
            def run(engname, eng):
                known = {}
                nw = 0
                for o in self.ops[engname]:
                    for dep in o.deps:
                        if dep[0] == "d":
                            sem, val = dep[1].sem, dep[2]
                        else:
                            sem, val = self.esem[dep[1].eng], dep[1].idx
                        if known.get(id(sem), 0) >= val:
                            continue
                        eng.wait_ge(sem, val)
                        nw += 1
                        known[id(sem)] = val
                    if o.fn is None:
                        continue
                    ins = o.fn(eng)
                    if o.dma is not None:
                        ins.then_inc(o.dma.sem, 16)
                    elif o.signal:
                        ins.then_inc(self.esem[engname], 1)
                stats[engname] = (len(self.ops[engname]), nw)

            @block.tensor
            def _(eng):
                run("tensor", eng)

            @block.vector
            def _(eng):
                run("vector", eng)

            @block.scalar
            def _(eng):
                run("scalar", eng)

            @block.gpsimd
            def _(eng):
                run("gpsimd", eng)

            @block.sync
            def _(eng):
                run("sync", eng)
        self.stats.append(stats)
        self.ops = {e: [] for e in ENGS}
        self.phase += 1

    def mm(self, out, lhsT, rhs, start, stop, R, W, **kw):
        return self.op("tensor", lambda e: e.matmul(out, lhsT=lhsT, rhs=rhs, start=start, stop=stop, **kw), R, W)

    def tr(self, out, in_, ident, R, W):
        return self.op("tensor", lambda e: e.transpose(out, in_, ident), R, W)

    def act(self, out, in_, func, R, W, **kw):
        return self.op("scalar", lambda e: e.activation(out=out, in_=in_, func=func, **kw), R, W)

    def tt(self, eng, out, in0, in1, op, R, W):
        return self.op(eng, lambda e: e.tensor_tensor(out=out, in0=in0, in1=in1, op=op), R, W)

    def ts(self, eng, out, in0, s1, s2, op0, op1, R, W):
        if s2 is None:
            return self.op(eng, lambda e: e.tensor_scalar(out=out, in0=in0, scalar1=s1, scalar2=None, op0=op0), R, W)
        return self.op(eng, lambda e: e.tensor_scalar(out=out, in0=in0, scalar1=s1, scalar2=s2, op0=op0, op1=op1), R, W)

    def stt(self, out, in0, scalar, in1, op0, op1, R, W):
        return self.op("vector", lambda e: e.scalar_tensor_tensor(out=out, in0=in0, scalar=scalar, in1=in1, op0=op0, op1=op1), R, W)

    def cp(self, eng, out, in_, R, W):
        if eng == "scalar":
            return self.op(eng, lambda e: e.copy(out=out, in_=in_), R, W)
        return self.op(eng, lambda e: e.tensor_copy(out=out, in_=in_), R, W)

    def dma(self, eng, out, in_, R, W, sem):
        return self.op(eng, lambda e: e.dma_start(out=out, in_=in_), R, W, dma=sem)


def build(debug=False, stage=99):
    nc = bass.Bass("TRN2", target_bir_lowering=False)

    def din(name, shape):
        return nc.dram_tensor(name, shape, F32, kind="ExternalInput").ap()

    skind = "ExternalOutput" if debug else "Internal"

    def dsc(name, shape, dt):
        return nc.dram_tensor(name, shape, dt, kind=skind).ap()

    xT = din("xT", [D, T])
    pT = din("pT", [256, T])
    w_in = din("w_in", [D, 10248])
    gvec = din("gvec", [128, 64])
    b_f = din("b_f", [8, 1])
    conv_w = din("conv_w", [128, 24])
    w_a = din("w_a", [1024, D])
    w_b = din("w_b", [1024, D])
    w_o = din("w_o", [D, D])
    w_r = din("w_r", [D, 36])
    b_r = din("b_r", [128, 36])
    w_gu = din("w_gu", [NE, D, 1024])
    w_dn = din("w_dn", [NE, 512, D])
    w_pg = din("w_pg", [D, D])
    w_pe = din("w_pe", [256, D])
    cfd = din("cf", [128, NCF])
    sel8d = din("sel8", [8, 1032])
    tabinit = din("tabinit", [128, TW])
    yT = nc.dram_tensor("yT", [D, T], F32, kind="ExternalOutput").ap()

    qT_d = dsc("qT_d", [1024, T], BF16)
    kT_d = dsc("kT_d", [1024, T], BF16)
    v_d = dsc("v_d", [T, 1024], BF16)
    bmT_d = dsc("bmT_d", [1024, T], BF16)
    gaT_d = dsc("gaT_d", [D, T], BF16)
    gbT_d = dsc("gbT_d", [D, T], BF16)
    negc_d = dsc("negc_d", [8, T], F32)
    attnT_d = dsc("attnT_d", [1024, T], BF16)
    mgT_d = dsc("mgT_d", [D, T], BF16)
    x1T_d = dsc("x1T_d", [D, T], F32)
    hn_d = dsc("hn_d", [T, D], BF16)
    tab_d = dsc("tab_d", [NRT * 128, TW], F32)
    ybuf_d = dsc("ybuf_d", [2 * T + 128, D], F32)
    dbg_x2 = dsc("dbg_x2", [D, T], F32) if debug else None

    def chunked(ap):
        return ap.rearrange("(c p) n -> p c n", p=128)

    with contextlib.ExitStack() as glob:
        P = Prog(nc, glob)

        sbn = [0]

        def sb(st, name, shape, dt):
            sbn[0] += 1
            return st.enter_context(nc.sbuf_tensor("s%d_%s" % (sbn[0], name), shape, dt))

        cf = sb(glob, "cf", [128, NCF], F32)
        cb = sb(glob, "cb", [128, 512], BF16)
        gv = sb(glob, "gv", [128, 64], F32)
        sel8 = sb(glob, "sel8", [8, 1032], F32)
        Bc = Buf("consts")
        ps = [glob.enter_context(nc.psum_tensor("ps%d" % i, [128, 512], F32)) for i in range(6)]
        Bps = [Buf("ps%d" % i) for i in range(6)]
        pt = [glob.enter_context(nc.psum_tensor("pt%d" % i, [128, 1024], BF16)) for i in range(2)]
        Bpt = [Buf("pt%d" % i) for i in range(2)]
        Bd = Buf("dram", multi=True)

        P.dma("sync", cf[:], cfd, [], [Bc], Bc)
        P.dma("sync", gv[:], gvec, [], [Bc], Bc)
        P.dma("sync", sel8[:], sel8d, [], [Bc], Bc)
        P.dma("gpsimd", cb[:], cfd[:, 0:512], [], [Bc], Bc)
        identb = cb[:, C_ID:C_ID + 128]
        onesb = cb[:, C_ONE:C_ONE + 128]
        trib = cb[:, C_TRI:C_TRI + 128]
        usb = cb[:, C_US:C_US + 128]
        identf = cf[:, C_ID:C_ID + 128]
        G_MIX, G_FFN, G_PLE, G_FIN = 0, 16, 32, 48

        evac_ctr = [0]

        def evac(out, in_, R, W):
            evac_ctr[0] += 1
            if evac_ctr[0] % 2:
                return P.cp("scalar", out, in_, R, W)
            return P.cp("vector", out, in_, R, W)

        def rms_stats(xblk, Bx, n, sq, Bsq, psn, Bpsn, srt, Bsrt, rstd, Brstd):
            P.act(sq[:, :, 0:n], xblk[:, :, 0:n], AF.Square, [Bx], [Bsq])
            for c in range(KC):
                P.mm(psn[:, 0:n], onesb, sq[:, c, 0:n], c == 0, c == KC - 1, [Bsq, Bc], [Bpsn])
            P.act(srt[:, 0:n], psn[:, 0:n], AF.Sqrt, [Bpsn], [Bsrt], bias=EPS, scale=1.0 / D)
            P.op("vector", lambda e: e.reciprocal(out=rstd[:, 0:n], in_=srt[:, 0:n]), [Bsrt], [Brstd])

        with contextlib.ExitStack() as sA:
            hT = sb(sA, "hT", [128, KC, T], BF16)
            BhT = [Buf("hT%d" % i) for i in range(16)]
            with contextlib.ExitStack() as s0:
                xb = [sb(s0, "xb%d" % i, [128, KC, 256], F32) for i in range(2)]
                Bxb = [Buf("xb%d" % i) for i in range(2)]
                sq = sb(s0, "sq", [128, KC, 256], BF16)
                Bsq = Buf("sq")
                srt = sb(s0, "srt", [128, 256], F32)
                Bsrt = Buf("srt")
                rstd = sb(s0, "rstd", [128, 256], F32)
                Brstd = Buf("rstd")
                xTv = chunked(xT)
                for tb in range(16):
                    b = tb % 2
                    P.dma("sync", xb[b][:], xTv[:, :, tb * 256:(tb + 1) * 256], [], [Bxb[b]], Bxb[b])
                    rms_stats(xb[b], Bxb[b], 256, sq, Bsq, ps[0], Bps[0], srt, Bsrt, rstd, Brstd)
                    for c in range(KC):
                        P.stt(hT[:, c, tb * 256:(tb + 1) * 256], xb[b][:, c, :], gv[:, G_MIX + c:G_MIX + c + 1],
                              rstd[:], ALU.mult, ALU.mult, [Bxb[b], Brstd, Bc], [BhT[tb]])
                P.barrier()
                P.emit()
            if stage >= 1:
              with contextlib.ExitStack() as s1:
                wsl = [sb(s1, "wsl%d" % i, [128, KC, 512], BF16) for i in range(2)]
                Bw = [Buf("wsl%d" % i) for i in range(2)]
                stg = [sb(s1, "stg%d" % i, [128, 4, 512], BF16) for i in range(2)]
                Bstg = [Buf("stg%d" % i) for i in range(2)]
                usb_ = sb(s1, "u_sb", [128, 512], F32)
                Bus = Buf("u_sb")
                zb = sb(s1, "zb", [128, 514], F32)
                Bz = Buf("zb")
                acc = sb(s1, "cacc", [128, 512], F32)
                Bacc = Buf("cacc")
                cw = sb(s1, "cw", [128, 24], F32)
                negb = sb(s1, "negb", [8, 1], F32)
                lsp = sb(s1, "lsp", [8, 512], F32)
                Blsp = Buf("lsp")
                one8 = sb(s1, "one8", [8, 512], F32)
                ncs = sb(s1, "ncs", [8, T], F32)
                Bncs = Buf("ncs")
                Bcw = Buf("cw")
                P.dma("sync", cw[:], conv_w, [], [Bcw], Bcw)
                P.dma("sync", negb[:], b_f, [], [Bcw], Bcw)
                P.ts("vector", negb[:], negb[:], -1.0, None, ALU.mult, None, [Bcw], [Bcw])
                P.op("vector", lambda e: e.memset(one8[:], 1.0), [], [Bcw])
                w_in_v = chunked(w_in)
                allh = list(BhT)
                slab_i = [0]
                psr = [0]

                def next_ps():
                    psr[0] = (psr[0] + 1) % 6
                    return psr[0]

                def load_slab(cols):
                    i = slab_i[0] % 2
                    slab_i[0] += 1
                    o = 0
                    for (c0, n) in cols:
                        P.dma("gpsimd", wsl[i][:, :, o:o + n], w_in_v[:, :, c0:c0 + n], [], [Bw[i]], Bw[i])
                        o += n
                    return i

                stg_i = [0]

                def fm_plain(col0, dest, row0, sigmoid):
                    i = load_slab([(col0, 512)])
                    for tb in range(8):
                        si = stg_i[0] % 2
                        stg_i[0] += 1
                        for m in range(4):
                            pi = next_ps()
                            for c in range(KC):
                                P.mm(ps[pi][:], wsl[i][:, c, m * 128:(m + 1) * 128], hT[:, c, tb * 512:(tb + 1) * 512],
                                     c == 0, c == KC - 1, [Bw[i], BhT[2 * tb], BhT[2 * tb + 1]], [Bps[pi]])
                            if sigmoid:
                                P.act(stg[si][:, m, :], ps[pi][:], AF.Sigmoid, [Bps[pi]], [Bstg[si]])
                            else:
                                evac(stg[si][:, m, :], ps[pi][:], [Bps[pi]], [Bstg[si]])
                        P.dma("sync", dest[row0:row0 + 512, tb * 512:(tb + 1) * 512].rearrange("(m p) t -> p m t", p=128),
                              stg[si][:], [Bstg[si]], [Bd], Bstg[si])

                for s in range(2):
                    fm_plain(s * 512, qT_d, s * 512, False)
                for s in range(2):
                    fm_plain(1024 + s * 512, kT_d, s * 512, False)
                for s in range(2):
                    i = load_slab([(2048 + s * 512, 512)])
                    for t4 in range(8):
                        si = stg_i[0] % 2
                        stg_i[0] += 1
                        for j in range(4):
                            tt_ = t4 * 4 + j
                            pi = next_ps()
                            for c in range(KC):
                                P.mm(ps[pi][:], hT[:, c, tt_ * 128:(tt_ + 1) * 128], wsl[i][:, c, :],
                                     c == 0, c == KC - 1, [Bw[i], BhT[tt_ // 2]], [Bps[pi]])
                            evac(stg[si][:, j, :], ps[pi][:], [Bps[pi]], [Bstg[si]])
                        P.dma("sync", v_d[t4 * 512:(t4 + 1) * 512, s * 512:(s + 1) * 512].rearrange("(j p) n -> p j n", p=128),
                              stg[si][:], [Bstg[si]], [Bd], Bstg[si])
                i = load_slab([(3072, 8)])
                for tb in range(8):
                    pi = next_ps()
                    for c in range(KC):
                        P.mm(ps[pi][0:8, :], wsl[i][:, c, 0:8], hT[:, c, tb * 512:(tb + 1) * 512],
                             c == 0, c == KC - 1, [Bw[i], BhT[2 * tb], BhT[2 * tb + 1]], [Bps[pi]])
                    P.act(lsp[:], ps[pi][0:8, :], AF.Exp, [Bps[pi], Bcw], [Blsp], bias=negb[:, 0:1], scale=-1.0)
                    P.act(lsp[:], lsp[:], AF.Ln, [Blsp], [Blsp], bias=1.0, scale=1.0)
                    init = 0.0 if tb == 0 else ncs[:, tb * 512 - 1:tb * 512]
                    P.op("vector", lambda e, tb=tb, init=init: e.tensor_tensor_scan(
                        out=ncs[:, tb * 512:(tb + 1) * 512], data0=one8[:], data1=lsp[:], initial=init,
                        op0=ALU.mult, op1=ALU.add), [Blsp, Bcw, Bncs], [Bncs])
                P.dma("sync", negc_d, ncs[:], [Bncs], [Bd], Bncs)
                for j in range(8):
                    i = load_slab([(3080 + j * 128, 128), (4104 + j * 128, 128), (5128 + j * 128, 128)])
                    P.op("vector", lambda e: e.memset(zb[:, 0:2], 0.0), [], [Bz])
                    for tb in range(8):
                        if tb % 4 == 0:
                            si = stg_i[0] % 2
                            stg_i[0] += 1
                        pis = []
                        for m in range(3):
                            pi = next_ps()
                            pis.append(pi)
                            for c in range(KC):
                                P.mm(ps[pi][:], wsl[i][:, c, m * 128:(m + 1) * 128], hT[:, c, tb * 512:(tb + 1) * 512],
                                     c == 0, c == KC - 1, [Bw[i], BhT[2 * tb], BhT[2 * tb + 1]], [Bps[pi]])
                        pu, pbg, pcg = pis
                        P.cp("scalar", usb_[:], ps[pu][:], [Bps[pu]], [Bus])
                        P.tt("vector", zb[:, 2:514], ps[pcg][:], usb_[:], ALU.mult, [Bps[pcg], Bus], [Bz])
                        P.ts("vector", acc[:], zb[:, 2:514], cw[:, j * 3 + 2:j * 3 + 3], None, ALU.mult, None, [Bz, Bcw], [Bacc])
                        P.stt(acc[:], zb[:, 1:513], cw[:, j * 3 + 1:j * 3 + 2], acc[:], ALU.mult, ALU.add, [Bz, Bcw, Bacc], [Bacc])
                        P.stt(acc[:], zb[:, 0:512], cw[:, j * 3 + 0:j * 3 + 1], acc[:], ALU.mult, ALU.add, [Bz, Bcw, Bacc], [Bacc])
                        P.tt("vector", stg[si][:, tb % 4, :], ps[pbg][:], acc[:], ALU.mult, [Bps[pbg], Bacc], [Bstg[si]])
                        P.cp("vector", zb[:, 0:2], zb[:, 512:514], [Bz], [Bz])
                        if tb % 4 == 3:
                            t0 = (tb - 3) * 512
                            P.dma("sync", bmT_d[j * 128:(j + 1) * 128, t0:t0 + 2048].rearrange("p (m t) -> p m t", m=4),
                                  stg[si][:], [Bstg[si]], [Bd], Bstg[si])
                for s in range(4):
                    fm_plain(6152 + s * 512, gaT_d, s * 512, True)
                for s in range(4):
                    fm_plain(6152 + 2048 + s * 512, gbT_d, s * 512, True)
                P.barrier()
                P.emit()

        if stage >= 2:
          with contextlib.ExitStack() as sB:
            qh = [sb(sB, "qh%d" % i, [128, T], BF16) for i in range(2)]
            kh = [sb(sB, "kh%d" % i, [128, T], BF16) for i in range(2)]
            vh = [sb(sB, "vh%d" % i, [128, 32, 129], BF16) for i in range(2)]
            ncq = [sb(sB, "ncq%d" % i, [128, T], F32) for i in range(2)]
            Bq = [Buf("qh%d" % i) for i in range(2)]
            Bk = [Buf("kh%d" % i) for i in range(2)]
            Bv = [Buf("vh%d" % i) for i in range(2)]
            Bncq = [Buf("ncq%d" % i) for i in range(2)]
            ncs2 = sb(sB, "ncs2", [8, T], F32)
            Bn2 = Buf("ncs2")
            nck = sb(sB, "nck", [128, 32, 8], F32)
            Bnck = Buf("nck")
            rden = sb(sB, "rden", [128, 4], F32)
            Brd = Buf("rden")
            atok = [sb(sB, "atok%d" % i, [128, 4, 128], BF16) for i in range(2)]
            Bat = [Buf("atok%d" % i) for i in range(2)]
            ast = [sb(sB, "ast%d" % i, [128, 512], BF16) for i in range(2)]
            Bast = [Buf("ast%d" % i) for i in range(2)]
            P.dma("sync", ncs2[:], negc_d, [Bd], [Bn2], Bn2)
            for j in range(32):
                P.mm(ps[5][:, j * 8:(j + 1) * 8], ncs2[:, j * 128:(j + 1) * 128], sel8[:, 1024:1032], True, True, [Bn2, Bc], [Bps[5]])
            P.cp("vector", nck[:].rearrange("p j h -> p (j h)"), ps[5][:, 0:256], [Bps[5]], [Bnck])
            for i in range(2):
                P.op("gpsimd", lambda e, i=i: e.memset(vh[i][:, :, 128:129], 1.0), [], [Bv[i]])
            pend = []
            sidx = [0]
            blk = [0]
            ACCB = [2, 3, 4, 5]
            rden = sb(sB, "rden2", [128, 2, 4], F32)

            def loads(h):
                hb = h % 2
                P.dma("sync", qh[hb][:], qT_d[h * 128:(h + 1) * 128, :], [Bd], [Bq[hb]], Bq[hb])
                P.dma("sync", kh[hb][:], kT_d[h * 128:(h + 1) * 128, :], [Bd], [Bk[hb]], Bk[hb])
                P.dma("sync", vh[hb][:, :, 0:128], v_d[:, h * 128:(h + 1) * 128].rearrange("(j p) d -> p j d", p=128),
                      [Bd], [Bv[hb]], Bv[hb])
                P.dma("sync", ncq[hb][:], negc_d[h, :].partition_broadcast(128), [Bd], [Bncq[hb]], Bncq[hb])

            loads(0)
            NT_ = 5
            SB_ = [ps[0], ps[1], pt[1][:].bitcast(F32)]
            BSB_ = [Bps[0], Bps[1], Bpt[1]]
            tS = [sb(sB, "tSx%d" % i, [128, 512], F32) for i in range(NT_)]
            BtS = [Buf("tSx%d" % i) for i in range(NT_)]
            pTt = [sb(sB, "pTx%d" % i, [128, 512], BF16) for i in range(NT_)]
            BpT = [Buf("pTx%d" % i) for i in range(NT_)]
            LOOK = 3
            tiles = []
            for h in range(NH):
                for qb in range(8):
                    for kt in range(4 * (qb + 1)):
                        tiles.append((h, qb, kt))

            def front(n):
                h, qb, kt = tiles[n]
                hb = h % 2
                dj = kt - 4 * qb
                qlo = max(dj, 0) * 128
                si = n % 3
                ti = n % NT_
                q0 = qb * 512 + qlo
                q1 = (qb + 1) * 512
                P.mm(SB_[si][:, qlo:512], kh[hb][:, kt * 128:(kt + 1) * 128], qh[hb][:, q0:q1], True, True,
                     [Bk[hb], Bq[hb]], [BSB_[si]])
                P.stt(tS[ti][:, qlo:512], SB_[si][:, qlo:512], QSCALE, ncq[hb][:, q0:q1], ALU.mult, ALU.subtract,
                      [BSB_[si], Bncq[hb]], [BtS[ti]])
                P.act(pTt[ti][:, qlo:512], tS[ti][:, qlo:512], AF.Exp, [BtS[ti], Bnck], [BpT[ti]],
                      bias=nck[:, kt, h:h + 1], scale=1.0)
                if dj >= 0:
                    P.tt("gpsimd", pTt[ti][:, qlo:qlo + 128], pTt[ti][:, qlo:qlo + 128], trib, ALU.mult,
                         [BpT[ti], Bc], [BpT[ti]])

            def back(n):
                nonlocal pend
                h, qb, kt = tiles[n]
                hb = h % 2
                if qb == 0 and kt == 0 and h + 1 < NH:
                    loads(h + 1)
                dj = kt - 4 * qb
                ti = n % NT_
                for qs in range(4):
                    if qs < dj:
                        continue
                    a = ACCB[qs]
                    P.mm(ps[a][:, 0:129], pTt[ti][:, qs * 128:(qs + 1) * 128], vh[hb][:, kt, :],
                         kt == 0, kt == 4 * qb + qs, [BpT[ti], Bv[hb]], [Bps[a]])
                if kt == 1 and pend:
                    for f in pend:
                        f()
                    pend = []
                if kt == 4 * (qb + 1) - 1:
                    ab = blk[0] % 2
                    blk[0] += 1
                    for qs in range(4):
                        a = ACCB[qs]
                        P.op("vector", lambda e, a=a, qs=qs, ab=ab: e.reciprocal(out=rden[:, ab, qs:qs + 1], in_=ps[a][:, 128:129]),
                             [Bps[a]], [Brd])
                        P.act(atok[ab][:, qs, :], ps[a][:, 0:128], AF.Copy, [Bps[a], Brd], [Bat[ab]], scale=rden[:, ab, qs:qs + 1])

                    def fin(h=h, qb=qb, ab=ab):
                        for qs in range(4):
                            P.tr(pt[0][:, qs * 128:(qs + 1) * 128], atok[ab][:, qs, :], identb, [Bat[ab], Bc], [Bpt[0]])
                        P.cp("vector", ast[ab][:], pt[0][:, 0:512], [Bpt[0]], [Bast[ab]])
                        P.dma("sync", attnT_d[h * 128:(h + 1) * 128, qb * 512:(qb + 1) * 512], ast[ab][:], [Bast[ab]], [Bd], Bast[ab])
                    pend.append(fin)

            for n in range(len(tiles) + LOOK):
                if n < len(tiles):
                    front(n)
                if n - LOOK >= 0:
                    back(n - LOOK)
            for f in pend:
                f()
            P.barrier()
            P.emit()

        if stage >= 3:
          with contextlib.ExitStack() as sC:
            wa = sb(sC, "wa", [128, 8, D], BF16)
            wb = sb(sC, "wb", [128, 8, D], BF16)
            Bwab = Buf("wab")
            P.dma("gpsimd", wa[:], chunked(w_a), [], [Bwab], Bwab)
            P.dma("gpsimd", wb[:], chunked(w_b), [], [Bwab], Bwab)
            at = [sb(sC, "at%d" % i, [128, 8, 512], BF16) for i in range(2)]
            bm = [sb(sC, "bm%d" % i, [128, 8, 512], BF16) for i in range(2)]
            ga = [sb(sC, "ga%d" % i, [128, KC, 512], BF16) for i in range(2)]
            gb = [sb(sC, "gb%d" % i, [128, KC, 512], BF16) for i in range(2)]
            Bin = [Buf("c1in%d" % i) for i in range(2)]
            t1 = [sb(sC, "t1_%d" % i, [128, 512], F32) for i in range(2)]
            t2 = [sb(sC, "t2_%d" % i, [128, 512], F32) for i in range(2)]
            Bt1 = [Buf("t1_%d" % i) for i in range(2)]
            Bt2 = [Buf("t2_%d" % i) for i in range(2)]
            mst = [sb(sC, "mst%d" % i, [128, 4, 512], BF16) for i in range(2)]
            Bmst = [Buf("mst%d" % i) for i in range(2)]

            def c1_loads(tb):
                b = tb % 2
                sl = slice(tb * 512, (tb + 1) * 512)
                P.dma("sync", at[b][:], chunked(attnT_d)[:, :, sl], [Bd], [Bin[b]], Bin[b])
                P.dma("sync", bm[b][:], chunked(bmT_d)[:, :, sl], [Bd], [Bin[b]], Bin[b])
                P.dma("sync", ga[b][:], chunked(gaT_d)[:, :, sl], [Bd], [Bin[b]], Bin[b])
                P.dma("sync", gb[b][:], chunked(gbT_d)[:, :, sl], [Bd], [Bin[b]], Bin[b])

            c1_loads(0)
            k = 0
            for tb in range(8):
                b = tb % 2
                if tb + 1 < 8:
                    c1_loads(tb + 1)
                for fg in range(KC):
                    pa = (2 * k) % 6
                    pb = (2 * k + 1) % 6
                    tb_ = k % 2
                    k += 1
                    if fg % 4 == 0:
                        mi = (tb * 4 + fg // 4) % 2
                    for c in range(8):
                        P.mm(ps[pa][:], wa[:, c, fg * 128:(fg + 1) * 128], at[b][:, c, :], c == 0, c == 7, [Bwab, Bin[b]], [Bps[pa]])
                    for c in range(8):
                        P.mm(ps[pb][:], wb[:, c, fg * 128:(fg + 1) * 128], bm[b][:, c, :], c == 0, c == 7, [Bwab, Bin[b]], [Bps[pb]])
                    P.tt("vector", t1[tb_][:], ps[pa][:], ga[b][:, fg, :], ALU.mult, [Bps[pa], Bin[b]], [Bt1[tb_]])
                    P.tt("vector", t2[tb_][:], ps[pb][:], gb[b][:, fg, :], ALU.mult, [Bps[pb], Bin[b]], [Bt2[tb_]])
                    P.tt("gpsimd", mst[mi][:, fg % 4, :], t1[tb_][:], t2[tb_][:], ALU.add, [Bt1[tb_], Bt2[tb_]], [Bmst[mi]])
                    if fg % 4 == 3:
                        r0 = (fg - 3) * 128
                        P.dma("sync", mgT_d[r0:r0 + 512, tb * 512:(tb + 1) * 512].rearrange("(m p) t -> p m t", p=128),
                              mst[mi][:], [Bmst[mi]], [Bd], Bmst[mi])
            P.barrier()
            P.emit()

        if stage >= 4:
          with contextlib.ExitStack() as sC:
            wo = sb(sC, "wo", [128, KC, D], BF16)
            Bwo = Buf("wo")
            P.dma("gpsimd", wo[:, 0:8, :], chunked(w_o)[:, 0:8, :], [], [Bwo], Bwo)
            P.dma("gpsimd", wo[:, 8:16, :], chunked(w_o)[:, 8:16, :], [], [Bwo], Bwo)
            mg = [sb(sC, "mg%d" % i, [128, KC, 256], BF16) for i in range(3)]
            xb = [sb(sC, "xb%d" % i, [128, KC, 256], F32) for i in range(3)]
            Bmg = [Buf("mg%d" % i) for i in range(3)]
            Bxb = [Buf("xb%d" % i) for i in range(3)]
            sq = sb(sC, "sq", [128, KC, 256], BF16)
            Bsq = Buf("sq")
            srt = sb(sC, "srt", [128, 256], F32)
            Bsrt = Buf("srt")
            rstd = sb(sC, "rstd", [128, 256], F32)
            Brstd = Buf("rstd")
            hnT = sb(sC, "hnT", [128, KC, 256], BF16)
            BhnT = Buf("hnT")
            hnt = [sb(sC, "hnt%d" % i, [128, D], BF16) for i in range(2)]
            Bhnt = [Buf("hnt%d" % i) for i in range(2)]
            wr = sb(sC, "wr", [128, KC, 36], F32)
            brb = sb(sC, "brb", [128, 36], F32)
            Bwr = Buf("wr")
            P.dma("sync", wr[:], chunked(w_r), [], [Bwr], Bwr)
            P.dma("sync", brb[:], b_r, [], [Bwr], Bwr)
            for c in range(KC):
                P.ts("vector", wr[:, c, :], wr[:, c, :], gv[:, G_FFN + c:G_FFN + c + 1], None, ALU.mult, None, [Bwr, Bc], [Bwr])
            R4 = [sb(sC, "rt%d" % i, [128, 256], F32) for i in range(4)]
            Brt4 = [Buf("rt%d" % i) for i in range(4)]
            carry = sb(sC, "carry", [128, 32], F32)
            Bcar = Buf("carry")
            Ab4 = [sb(sC, "Ab%d" % i, [128, 32], BF16) for i in range(4)]
            BAb4 = [Buf("Ab%d" % i) for i in range(4)]
            pay = [sb(sC, "pay%d" % i, [128, TW], F32) for i in range(4)]
            Bpay = [Buf("pay%d" % i) for i in range(4)]
            dsti = [sb(sC, "dsti%d" % i, [128, 1], I32) for i in range(4)]
            tin = sb(sC, "tin", [128, TW], F32)
            Btin = Buf("tin")
            P.dma("sync", tin[:], tabinit, [], [Btin], Btin)
            Btab = Buf("tab", multi=True)
            tabv = tab_d.rearrange("(j p) w -> p j w", p=128)
            for j in range(NRT):
                P.dma("sync", tabv[:, j, :], tin[:], [Btin], [Btab], Btin)
            P.op("vector", lambda e: e.memset(carry[:], 0.0), [], [Bcar])
            for i in range(4):
                P.op("vector", lambda e, i=i: e.memset(pay[i][:], 0.0), [], [Bpay[i]])
            P.op("gpsimd", lambda e: e.nop(), [Btab], [])

            def c2_loads(tb):
                b = tb % 3
                sl = slice(tb * 256, (tb + 1) * 256)
                P.dma("sync", mg[b][:], chunked(mgT_d)[:, :, sl], [Bd], [Bmg[b]], Bmg[b])
                P.dma("sync", xb[b][:], chunked(xT)[:, :, sl], [], [Bxb[b]], Bxb[b])

            LG, OHG, EG, ESEL, OH1, E2, OH2, A1, A2, POS, TMP = 0, 36, 40, 44, 52, 60, 68, 76, 108, 140, 172
            SC = 204
            pay_i = [0]

            def route1(tile_i, b, tsl):
                R_ = R4[tile_i % 4]
                Brt = Brt4[tile_i % 4]
                Ab = Ab4[tile_i % 4]
                BAb = BAb4[tile_i % 4]
                for c in range(KC):
                    P.mm(ps[4][:, 0:36], xb[b][:, c, tsl], wr[:, c, :], c == 0, c == KC - 1, [Bxb[b], Bwr], [Bps[4]])
                for c in range(KC):
                    P.mm(ps[5][:, 0:1], sq[:, c, tsl], onesb[:, 0:1], c == 0, c == KC - 1, [Bsq, Bc], [Bps[5]])
                sc = lambda n: R_[:, SC + n:SC + n + 1]
                RW = ([Brt], [Brt])
                P.act(sc(14), ps[5][:, 0:1], AF.Sqrt, [Bps[5]], [Brt], bias=EPS, scale=1.0 / D)
                P.op("vector", lambda e: e.reciprocal(out=sc(14), in_=sc(14)), *RW)
                P.stt(R_[:, LG:LG + 36], ps[4][:, 0:36], sc(14), brb[:], ALU.mult, ALU.add, [Bps[4], Brt, Bwr], [Brt])
                P.op("vector", lambda e: e.reduce_max(out=sc(0), in_=R_[:, LG:LG + 4], axis=AX.X), *RW)
                P.ts("vector", R_[:, OHG:OHG + 4], R_[:, LG:LG + 4], sc(0), None, ALU.is_equal, None, *RW)
                P.ts("vector", sc(1), sc(0), -1.0, None, ALU.mult, None, *RW)
                P.act(R_[:, EG:EG + 4], R_[:, LG:LG + 4], AF.Exp, *RW, bias=sc(1), scale=1.0)
                P.op("vector", lambda e: e.reduce_sum(out=sc(2), in_=R_[:, EG:EG + 4], axis=AX.X), *RW)
                P.op("vector", lambda e: e.reciprocal(out=sc(3), in_=sc(2)), *RW)
                P.ts("vector", R_[:, ESEL:ESEL + 8], R_[:, LG + 4:LG + 12], R_[:, OHG:OHG + 1], None, ALU.mult, None, *RW)
                for g in range(1, 4):
                    P.stt(R_[:, ESEL:ESEL + 8], R_[:, LG + 4 + 8 * g:LG + 12 + 8 * g], R_[:, OHG + g:OHG + g + 1],
                          R_[:, ESEL:ESEL + 8], ALU.mult, ALU.add, *RW)
                P.op("vector", lambda e: e.reduce_max(out=sc(4), in_=R_[:, ESEL:ESEL + 8], axis=AX.X), *RW)
                P.ts("vector", R_[:, OH1:OH1 + 8], R_[:, ESEL:ESEL + 8], sc(4), None, ALU.is_equal, None, *RW)
                P.stt(R_[:, E2:E2 + 8], R_[:, OH1:OH1 + 8], -1e30, R_[:, ESEL:ESEL + 8], ALU.mult, ALU.add, *RW)
                P.op("vector", lambda e: e.reduce_max(out=sc(5), in_=R_[:, E2:E2 + 8], axis=AX.X), *RW)
                P.ts("vector", R_[:, OH2:OH2 + 8], R_[:, E2:E2 + 8], sc(5), None, ALU.is_equal, None, *RW)
                P.tt("vector", sc(6), sc(5), sc(4), ALU.subtract, *RW)
                P.act(sc(7), sc(6), AF.Exp, *RW)
                P.ts("vector", sc(7), sc(7), 1.0, None, ALU.add, None, *RW)
                P.op("vector", lambda e: e.reciprocal(out=sc(7), in_=sc(7)), *RW)
                P.tt("vector", sc(8), sc(7), sc(3), ALU.mult, *RW)
                P.tt("vector", sc(9), sc(3), sc(8), ALU.subtract, *RW)
                for g in range(4):
                    P.ts("vector", R_[:, A1 + 8 * g:A1 + 8 * g + 8], R_[:, OH1:OH1 + 8], R_[:, OHG + g:OHG + g + 1], None, ALU.mult, None, *RW)
                    P.ts("vector", R_[:, A2 + 8 * g:A2 + 8 * g + 8], R_[:, OH2:OH2 + 8], R_[:, OHG + g:OHG + g + 1], None, ALU.mult, None, *RW)
                P.tt("vector", Ab[:], R_[:, A1:A1 + 32], R_[:, A2:A2 + 32], ALU.add, [Brt], [BAb])

            def route2(tile_i):
                R_ = R4[tile_i % 4]
                Brt = Brt4[tile_i % 4]
                Ab = Ab4[tile_i % 4]
                BAb = BAb4[tile_i % 4]
                sc = lambda n: R_[:, SC + n:SC + n + 1]
                RW = ([Brt], [Brt])
                P.mm(ps[2][:, 0:32], usb, Ab[:], True, True, [BAb, Bc], [Bps[2]])
                P.mm(ps[2][:, 64:96], onesb, Ab[:], True, True, [BAb, Bc], [Bps[2]])
                P.tt("vector", R_[:, POS:POS + 32], ps[2][:, 0:32], carry[:], ALU.add, [Bps[2], Bcar, Brt], [Brt])
                P.tt("vector", carry[:], ps[2][:, 64:96], carry[:], ALU.add, [Bps[2], Bcar], [Bcar])
                P.ts("vector", R_[:, POS:POS + 32], R_[:, POS:POS + 32], float(CAP), None, ALU.min, None, *RW)
                P.tt("vector", R_[:, POS:POS + 32], R_[:, POS:POS + 32], cf[:, C_ES:C_ES + 32], ALU.add, [Brt, Bc], [Brt])
                P.ts("vector", sc(12), cf[:, C_IOTA:C_IOTA + 1], float(tile_i * 128), None, ALU.add, None, [Bc, Brt], [Brt])
                P.ts("vector", sc(13), sc(12), float(T), None, ALU.add, None, *RW)
                for kk in range(2):
                    AK = A1 if kk == 0 else A2
                    P.tt("vector", R_[:, TMP:TMP + 32], R_[:, POS:POS + 32], R_[:, AK:AK + 32], ALU.mult, *RW)
                    P.op("vector", lambda e, kk=kk: e.reduce_sum(out=sc(10 + kk), in_=R_[:, TMP:TMP + 32], axis=AX.X), *RW)
                    pi_ = pay_i[0] % 4
                    pay_i[0] += 1
                    P.cp("vector", dsti[pi_][:], sc(10 + kk), [Brt], [Bpay[pi_]])
                    P.cp("vector", pay[pi_][:, 0:1], sc(12), [Brt], [Bpay[pi_]])
                    P.cp("vector", pay[pi_][:, 1:2], sc(12 + kk), [Brt], [Bpay[pi_]])
                    P.cp("vector", pay[pi_][:, 2:3], sc(8 + kk), [Brt], [Bpay[pi_]])
                    P.op("gpsimd", lambda e, pi_=pi_: e.indirect_dma_start(
                        out=tab_d, out_offset=bass.IndirectOffsetOnAxis(ap=dsti[pi_][:, 0:1], axis=0),
                        in_=pay[pi_][:], in_offset=None), [Bpay[pi_]], [Bd], dma=Bpay[pi_])

            def x_steps(tb):
                b = tb % 3
                steps = []
                for fg in range(KC):
                    def st(fg=fg):
                        pi = fg % 2
                        for c in range(KC):
                            P.mm(ps[pi][:, 0:256], wo[:, c, fg * 128:(fg + 1) * 128], mg[b][:, c, :], c == 0, c == KC - 1,
                                 [Bwo, Bmg[b]], [Bps[pi]])
                        P.tt("vector", xb[b][:, fg, :], ps[pi][:, 0:256], xb[b][:, fg, :], ALU.add, [Bps[pi], Bxb[b]], [Bxb[b]])
                    steps.append(st)
                return steps

            def x_fin(tb):
                b = tb % 3
                P.dma("sync", chunked(x1T_d)[:, :, tb * 256:(tb + 1) * 256], xb[b][:], [Bxb[b]], [Bd], Bxb[b])

            def y_steps(tb):
                b = tb % 3

                def y0_():
                    rms_stats(xb[b], Bxb[b], 256, sq, Bsq, ps[3], Bps[3], srt, Bsrt, rstd, Brstd)
                    for c in range(KC):
                        P.stt(hnT[:, c, :], xb[b][:, c, :], gv[:, G_FFN + c:G_FFN + c + 1], rstd[:], ALU.mult, ALU.mult,
                              [Bxb[b], Brstd, Bc], [BhnT])

                def ytile(tt_):
                    tile_i = tb * 2 + tt_
                    hb_ = tile_i % 2
                    tsl = slice(tt_ * 128, (tt_ + 1) * 128)
                    for c4 in range(4):
                        pti = c4 % 2
                        for cc in range(4):
                            c = c4 * 4 + cc
                            P.tr(pt[pti][:, cc * 128:(cc + 1) * 128], hnT[:, c, tsl], identb, [BhnT, Bc], [Bpt[pti]])
                        evac(hnt[hb_][:, c4 * 512:(c4 + 1) * 512], pt[pti][:, 0:512], [Bpt[pti]], [Bhnt[hb_]])
                    P.dma("sync", hn_d[tile_i * 128:(tile_i + 1) * 128, :], hnt[hb_][:], [Bhnt[hb_]], [Bd], Bhnt[hb_])
                    route1(tile_i, b, tsl)
                    pend2.append(tile_i)
                return [y0_, (lambda: ytile(0)), (lambda: ytile(1))]

            pend2 = []
            c2_loads(0)
            for tb in range(17):
                if tb + 1 < 16:
                    c2_loads(tb + 1)
                X = x_steps(tb) if tb < 16 else []
                Y = y_steps(tb - 1) if tb >= 1 else []
                flush = list(pend2)
                pend2 = []
                for fg in range(KC):
                    if X:
                        X[fg]()
                    if fg == 1 and Y:
                        Y[0]()
                    if fg == 3:
                        for t_ in flush:
                            route2(t_)
                    if fg == 6 and Y:
                        Y[1]()
                    if fg == 11 and Y:
                        Y[2]()
                if tb < 16:
                    x_fin(tb)
            for t_ in pend2:
                route2(t_)
            P.barrier()
            P.emit()

        if stage >= 5:
          with contextlib.ExitStack() as sE:
            wgu = [sb(sE, "wgu%d" % i, [128, KC, 1024], BF16) for i in range(2)]
            wdn = [sb(sE, "wdn%d" % i, [128, 4, D], BF16) for i in range(2)]
            Bwe = [Buf("we%d" % i) for i in range(2)]
            tb3 = [sb(sE, "tb3_%d" % i, [128, 3, TW], F32) for i in range(2)]
            Btb3 = [Buf("tb3_%d" % i) for i in range(2)]
            idx = [sb(sE, "idx%d" % i, [128, 3, 2], I32) for i in range(2)]
            Bidx = [Buf("idx%d" % i) for i in range(2)]
            xg = [[sb(sE, "xg%d_%d" % (i, s_), [128, D], BF16) for s_ in range(3)] for i in range(2)]
            Bxg = [[Buf("xg%d_%d" % (i, s_)) for s_ in range(3)] for i in range(2)]
            xgT = sb(sE, "xgT", [128, KC, CAP], BF16)
            BxgT = Buf("xgT")
            hTe = sb(sE, "hTe", [128, 4, CAP], BF16)
            BhTe = Buf("hTe")
            sg = [sb(sE, "sg%d" % i, [128, CAP], F32) for i in range(2)]
            Bsg = [Buf("sg%d" % i) for i in range(2)]
            ys = [sb(sE, "ys%d" % i, [128, D], F32) for i in range(3)]
            Bys = [Buf("ys%d" % i) for i in range(3)]

            def e_loads(e):
                b = e % 2
                P.dma("gpsimd", wgu[b][:, 0:8, :], chunked(w_gu[e])[:, 0:8, :], [], [Bwe[b]], Bwe[b])
                P.dma("gpsimd", wgu[b][:, 8:16, :], chunked(w_gu[e])[:, 8:16, :], [], [Bwe[b]], Bwe[b])
                P.dma("gpsimd", wdn[b][:], chunked(w_dn[e]), [], [Bwe[b]], Bwe[b])
                P.dma("sync", tb3[b][:], tab_d[e * CS:e * CS + CAP, :].rearrange("(s p) w -> p s w", p=128),
                      [Bd], [Btb3[b]], Btb3[b])
                P.cp("vector", idx[b][:], tb3[b][:, :, 0:2], [Btb3[b]], [Bidx[b]])
                for s_ in range(3):
                    P.op("gpsimd", lambda eng, b=b, s_=s_: eng.indirect_dma_start(
                        out=xg[b][s_][:], out_offset=None, in_=hn_d,
                        in_offset=bass.IndirectOffsetOnAxis(ap=idx[b][:, s_, 0:1], axis=0)),
                        [Bidx[b], Bd], [Bxg[b][s_]], dma=Bxg[b][s_])

            e_loads(0)
            k = 0
            for e in range(NE):
                b = e % 2
                if e + 1 < NE:
                    e_loads(e + 1)
                for s_ in range(3):
                    for c4 in range(4):
                        pti = k % 2
                        k += 1
                        for cc in range(4):
                            c = c4 * 4 + cc
                            P.tr(pt[pti][:, cc * 128:(cc + 1) * 128], xg[b][s_][:, c * 128:(c + 1) * 128], identb,
                                 [Bxg[b][s_], Bc], [Bpt[pti]])
                        evac(xgT[:, c4 * 4:c4 * 4 + 4, s_ * 128:(s_ + 1) * 128],
                             pt[pti][:, 0:512].rearrange("p (c t) -> p c t", c=4), [Bpt[pti]], [BxgT])
                for m in range(4):
                    pg_, pu_ = (2 * m) % 4, (2 * m + 1) % 4
                    sgi = m % 2
                    for c in range(KC):
                        P.mm(ps[pg_][:, 0:CAP], wgu[b][:, c, m * 128:(m + 1) * 128], xgT[:, c, :], c == 0, c == KC - 1,
                             [Bwe[b], BxgT], [Bps[pg_]])
                    for c in range(KC):
                        P.mm(ps[pu_][:, 0:CAP], wgu[b][:, c, 512 + m * 128:512 + (m + 1) * 128], xgT[:, c, :], c == 0, c == KC - 1,
                             [Bwe[b], BxgT], [Bps[pu_]])
                    P.act(sg[sgi][:], ps[pg_][:, 0:CAP], AF.Silu, [Bps[pg_]], [Bsg[sgi]])
                    P.tt("vector", hTe[:, m, :], sg[sgi][:], ps[pu_][:, 0:CAP], ALU.mult, [Bsg[sgi], Bps[pu_]], [BhTe])
                for s_ in range(3):
                    for n in range(4):
                        pi = 4 + (n % 2)
                        for m in range(4):
                            P.mm(ps[pi][:], hTe[:, m, s_ * 128:(s_ + 1) * 128], wdn[b][:, m, n * 512:(n + 1) * 512], m == 0, m == 3,
                                 [BhTe, Bwe[b]], [Bps[pi]])
                        if n % 2:
                            P.act(ys[s_][:, n * 512:(n + 1) * 512], ps[pi][:], AF.Copy, [Bps[pi], Btb3[b]], [Bys[s_]],
                                  scale=tb3[b][:, s_, 2:3])
                        else:
                            P.ts("vector", ys[s_][:, n * 512:(n + 1) * 512], ps[pi][:], tb3[b][:, s_, 2:3], None, ALU.mult, None,
                                 [Bps[pi], Btb3[b]], [Bys[s_]])
                    P.op("gpsimd", lambda eng, b=b, s_=s_: eng.indirect_dma_start(
                        out=ybuf_d, out_offset=bass.IndirectOffsetOnAxis(ap=idx[b][:, s_, 1:2], axis=0),
                        in_=ys[s_][:], in_offset=None), [Bys[s_], Bidx[b]], [Bd], dma=Bys[s_])
            P.barrier()
            P.emit()

        if stage >= 6:
          with contextlib.ExitStack() as sF:
            wpg = sb(sF, "wpg", [128, KC, D], BF16)
            wpe = sb(sF, "wpe", [128, 2, D], BF16)
            Bwp = Buf("wp")
            P.dma("gpsimd", wpg[:, 0:8, :], chunked(w_pg)[:, 0:8, :], [], [Bwp], Bwp)
            P.dma("gpsimd", wpg[:, 8:16, :], chunked(w_pg)[:, 8:16, :], [], [Bwp], Bwp)
            P.dma("gpsimd", wpe[:], chunked(w_pe), [], [Bwp], Bwp)
            xb = [sb(sF, "xb%d" % i, [128, KC, 256], F32) for i in range(3)]
            Bxb = [Buf("xb%d" % i) for i in range(3)]
            y0 = [sb(sF, "y0_%d" % i, [128, D], F32) for i in range(4)]
            By = [Buf("y%d" % i) for i in range(4)]
            pbf = [sb(sF, "pbf%d" % i, [128, 2, 256], BF16) for i in range(3)]
            Bpb = [Buf("pbf%d" % i) for i in range(3)]
            sqA = sb(sF, "sqA", [128, KC, 256], BF16)
            sqB = sqA
            BsqA = Buf("sqA")
            BsqB = BsqA
            srtA = sb(sF, "srtA", [128, 256], F32)
            srtB = sb(sF, "srtB", [128, 256], F32)
            BsrtA, BsrtB = Buf("srtA"), Buf("srtB")
            rstdA = sb(sF, "rstdA", [128, 256], F32)
            rstdB = sb(sF, "rstdB", [128, 256], F32)
            BrstdA, BrstdB = Buf("rstdA"), Buf("rstdB")
            hp = [sb(sF, "hp%d" % i, [128, KC, 256], BF16) for i in range(2)]
            Bhp = [Buf("hp%d" % i) for i in range(2)]
            sgt = [sb(sF, "sgt%d" % i, [128, 256], F32) for i in range(2)]
            Bsgt = [Buf("sgt%d" % i) for i in range(2)]

            def f_loads(tb):
                b = tb % 3
                sl = slice(tb * 256, (tb + 1) * 256)
                P.dma("sync", xb[b][:], chunked(x1T_d)[:, :, sl], [Bd], [Bxb[b]], Bxb[b])
                P.dma("gpsimd", pbf[b][:], chunked(pT)[:, :, sl], [], [Bpb[b]], Bpb[b])
                for tt_ in range(2):
                    tile_i = tb * 2 + tt_
                    yi = (tb % 2) * 2 + tt_
                    P.dma("sync", y0[yi][:], ybuf_d[tile_i * 128:(tile_i + 1) * 128, :], [Bd], [By[yi]], By[yi])
                    P.op("gpsimd", lambda e, yi=yi, tile_i=tile_i: e.dma_start(
                        out=y0[yi][:], in_=ybuf_d[T + tile_i * 128:T + (tile_i + 1) * 128, :], accum_op=ALU.add),
                        [Bd], [By[yi]], dma=By[yi])

            def f_front_steps(tb):
                b = tb % 3
                hb2 = tb % 2
                steps = []

                def pool_():
                    pass
                steps.append(pool_)
                for c in range(KC):
                    def st(c=c):
                        pi = c % 2
                        for tt_ in range(2):
                            yi = (tb % 2) * 2 + tt_
                            P.tr(ps[pi][:, tt_ * 128:(tt_ + 1) * 128], y0[yi][:, c * 128:(c + 1) * 128], identf, [By[yi], Bc], [Bps[pi]])
                        P.tt("vector", xb[b][:, c, :], ps[pi][:, 0:256], xb[b][:, c, :], ALU.add, [Bps[pi], Bxb[b]], [Bxb[b]])
                    steps.append(st)

                def tail_():
                    if debug:
                        P.dma("sync", chunked(dbg_x2)[:, :, tb * 256:(tb + 1) * 256], xb[b][:], [Bxb[b]], [Bd], Bxb[b])
                    rms_stats(xb[b], Bxb[b], 256, sqA, BsqA, ps[5], Bps[5], srtA, BsrtA, rstdA, BrstdA)
                    for c in range(KC):
                        P.stt(hp[hb2][:, c, :], xb[b][:, c, :], gv[:, G_PLE + c:G_PLE + c + 1], rstdA[:], ALU.mult, ALU.mult,
                              [Bxb[b], BrstdA, Bc], [Bhp[hb2]])
                steps.append(tail_)
                return steps

            def f_back_steps(tb):
                b = tb % 3
                hb2 = tb % 2
                steps = []
                for fg in range(KC):
                    def st(fg=fg):
                        si_ = fg % 2
                        pg_ = 2 + (fg % 2)
                        for c in range(KC):
                            P.mm(ps[pg_][:, 0:256], wpg[:, c, fg * 128:(fg + 1) * 128], hp[hb2][:, c, :], c == 0, c == KC - 1, [Bwp, Bhp[hb2]], [Bps[pg_]])
                        for c in range(2):
                            P.mm(ps[4][:, 0:256], wpe[:, c, fg * 128:(fg + 1) * 128], pbf[b][:, c, :], c == 0, c == 1, [Bwp, Bpb[b]], [Bps[4]])
                        P.act(sgt[si_][:], ps[pg_][:, 0:256], AF.Sigmoid, [Bps[pg_]], [Bsgt[si_]])
                        P.tt("vector", sgt[si_][:], sgt[si_][:], ps[4][:, 0:256], ALU.mult, [Bsgt[si_], Bps[4]], [Bsgt[si_]])
                        P.tt("gpsimd", xb[b][:, fg, :], xb[b][:, fg, :], sgt[si_][:], ALU.add, [Bsgt[si_], Bxb[b]], [Bxb[b]])
                    steps.append(st)

                def tail_():
                    rms_stats(xb[b], Bxb[b], 256, sqB, BsqB, ps[5], Bps[5], srtB, BsrtB, rstdB, BrstdB)
                    for c in range(KC):
                        P.stt(xb[b][:, c, :], xb[b][:, c, :], gv[:, G_FIN + c:G_FIN + c + 1], rstdB[:], ALU.mult, ALU.mult,
                              [Bxb[b], BrstdB, Bc], [Bxb[b]])
                    P.dma("sync", chunked(yT)[:, :, tb * 256:(tb + 1) * 256], xb[b][:], [Bxb[b]], [Bd], Bxb[b])
                steps.append(tail_)
                return steps

            f_loads(0)
            f_loads(1)
            for st_ in f_front_steps(0):
                st_()
            for tb in range(16):
                Fs = []
                if tb + 2 < 16:
                    f_loads(tb + 2)
                if tb + 1 < 16:
                    Fs = f_front_steps(tb + 1)
                Bs = f_back_steps(tb)
                if Fs:
                    Fs[0]()
                for i_ in range(KC):
                    Bs[i_]()
                    if Fs and i_ < 8:
                        Fs[1 + 2 * i_]()
                        Fs[2 + 2 * i_]()
                    if Fs and i_ == 8:
                        Fs[17]()
                Bs[16]()
            P.barrier()
            P.emit()
        if stage < 6:
            pass
    return nc


def _consts():
    cf = np.zeros((128, NCF), np.float32)
    cf[:, C_ID:C_ID + 128] = np.eye(128, dtype=np.float32)
    cf[:, C_ONE:C_ONE + 128] = 1.0
    k = np.arange(128)[:, None]
    q = np.arange(128)[None, :]
    cf[:, C_TRI:C_TRI + 128] = (q >= k)
    cf[:, C_US:C_US + 128] = (k < q)
    cf[:, C_IOTA] = np.arange(128)
    cf[:, C_ES:C_ES + 32] = (np.arange(32) * CS)[None, :]
    sel8 = np.zeros((8, 1032), np.float32)
    for h in range(8):
        sel8[h, h * 128:(h + 1) * 128] = 1.0
        sel8[h, 1024 + h] = 1.0
    tabinit = np.zeros((128, TW), np.float32)
    tabinit[:, 1] = 2 * T
    return cf, sel8, tabinit


def _shared_inputs(inp):
    f = lambda a: np.ascontiguousarray(a, dtype=np.float32)
    cf, sel8, tabinit = _consts()
    gl = lambda g: g.reshape(16, 128).T
    gvec = np.concatenate([gl(inp["g_mix"][0]), gl(inp["g_ffn"][0]), gl(inp["g_ple"][0]), gl(inp["g_final"])], axis=1)
    cw = inp["conv_w"][0].reshape(3, 8, 128).transpose(2, 1, 0).reshape(128, 24)
    w_r = np.concatenate([inp["w_router_group"][0], inp["w_router_expert"][0]], axis=1)
    b_r = np.concatenate([inp["b_router_group"][0], inp["b_router_expert"][0]])[None, :].repeat(128, axis=0)
    return {
        "w_in": f(inp["w_in"][0]), "gvec": f(gvec), "b_f": f(inp["b_f"][0].reshape(8, 1)), "conv_w": f(cw),
        "w_a": f(inp["w_branch_a"][0]), "w_b": f(inp["w_branch_b"][0]), "w_o": f(inp["w_out"][0]),
        "w_r": f(w_r), "b_r": f(b_r), "w_gu": f(inp["w_gate_up"][0]), "w_dn": f(inp["w_down"][0]),
        "w_pg": f(inp["w_ple_gate"][0]), "w_pe": f(inp["w_ple_proj"][0]),
        "cf": cf, "sel8": sel8, "tabinit": tabinit,
    }


def _core_inputs(inp, b, shared):
    m = dict(shared)
    m["xT"] = np.ascontiguousarray(np.asarray(inp["x"][b], dtype=np.float32).T)
    m["pT"] = np.ascontiguousarray(np.asarray(inp["p"][0, b], dtype=np.float32).T)
    return m


def kernel(**inputs):
    inp = {k: np.asarray(v) for k, v in inputs.items()}
    nb = inp["x"].shape[0]
    shared = _shared_inputs(inp)
    nc = build()
    in_maps = [_core_inputs(inp, b, shared) for b in range(nb)]
    res = run_bass_kernel_spmd(nc, in_maps, core_ids=list(range(nb)))
    out = np.empty((nb, T, D), np.float32)
    for b in range(nb):
        out[b] = np.asarray(res.results[b]["yT"]).T
    return out
```

```python
import contextlib
import numpy as np
import concourse.bass as bass
import concourse.mybir as mybir
from concourse.bass_utils import run_bass_kernel_spmd

F32 = mybir.dt.float32
BF16 = mybir.dt.bfloat16
I32 = mybir.dt.int32
ALU = mybir.AluOpType
AF = mybir.ActivationFunctionType
AX = mybir.AxisListType

ENGS = ["tensor", "vector", "scalar", "gpsimd", "sync"]

T = 4096
D = 2048
KC = 16
NH = 8
NE = 32
CAP = 384
CS = CAP + 1
NRT = 97
TW = 128
EPS = 1e-6
QSCALE = 128 ** -0.5
C_ID, C_ONE, C_TRI, C_US, C_IOTA, C_ES, C_TOK = 0, 128, 256, 384, 512, 513, 545
NCF = 577


class Buf:
    __slots__ = ("name", "w", "r", "sem", "cnt", "multi")

    def __init__(self, name, multi=False):
        self.name = name
        self.w = {}
        self.r = {}
        self.sem = None
        self.cnt = 0
        self.multi = multi


class Op:
    __slots__ = ("eng", "fn", "deps", "dma", "signal", "idx", "seq", "phase")


class Prog:
    def __init__(self, nc, stack):
        self.nc = nc
        self.stack = stack
        self.ops = {e: [] for e in ENGS}
        self.esem = {e: stack.enter_context(nc.semaphore("es_" + e)) for e in ENGS}
        self.ecnt = {e: 0 for e in ENGS}
        self.nsem = len(ENGS)
        self.phase = 0
        self.seq = 0
        self.dmabufs = []
        self.stats = []

    def _mk(self, eng, fn, deps, dma):
        o = Op()
        o.eng = eng
        o.fn = fn
        o.dma = dma
        o.signal = False
        o.idx = 0
        o.deps = deps
        o.phase = self.phase
        self.seq += 1
        o.seq = self.seq
        if dma is not None:
            if dma.sem is None:
                dma.sem = self.stack.enter_context(self.nc.semaphore("ds%d" % self.nsem))
                self.nsem += 1
                self.dmabufs.append(dma)
            dma.cnt += 16
        self.ops[eng].append(o)
        return o

    def op(self, eng, fn, reads=(), writes=(), dma=None):
        deps = {}

        def add(d):
            if d.phase < self.phase:
                return
            if d.dma is not None:
                deps[id(d.dma)] = ("d", d.dma, d.dma.cnt)
            else:
                if d.eng == "tensor" and eng == "tensor" and dma is None:
                    return
                prev = deps.get(d.eng)
                if prev is None or prev[1].seq < d.seq:
                    deps[d.eng] = ("e", d, 0)

        for b in reads:
            for d in b.w.values():
                add(d)
        for b in writes:
            if not b.multi:
                for d in b.w.values():
                    add(d)
            for d in b.r.values():
                add(d)
        o = self._mk(eng, fn, list(deps.values()), dma)
        key = id(dma) if dma is not None else eng
        for b in reads:
            b.r[key] = o
        for b in writes:
            if b.multi:
                b.w[key] = o
            else:
                b.w = {key: o}
                b.r = {}
        return o

    def barrier(self):
        a = {}
        for e in ENGS:
            a[e] = self._mk(e, (lambda eng: eng.drain()), [], None)
        for e in ENGS:
            deps = [("e", a[e2], 0) for e2 in ENGS if e2 != e]
            deps += [("d", b, b.cnt) for b in self.dmabufs if b.cnt > 0]
            self._mk(e, (lambda eng: eng.nop()), deps, None)

    def emit(self):
        nc = self.nc
        for e in ENGS:
            for o in self.ops[e]:
                for dep in o.deps:
                    if dep[0] == "e":
                        dep[1].signal = True
        for e in ENGS:
            for o in self.ops[e]:
                if o.signal and o.dma is None:
                    self.ecnt[e] += 1
                    o.idx = self.ecnt[e]
        stats = {}
        with nc.Block() as block:
            def run(engname, eng):
                known = {}
                nw = 0
                for o in self.ops[engname]:
                    for dep in o.deps:
                        if dep[0] == "d":
                            sem, val = dep[1].sem, dep[2]
                        else:
                            sem, val = self.esem[dep[1].eng], dep[1].idx
                        if known.get(id(sem), 0) >= val:
                            continue
                        eng.wait_ge(sem, val)
                        nw += 1
                        known[id(sem)] = val
                    if o.fn is None:
                        continue
                    ins = o.fn(eng)
                    if o.dma is not None:
                        ins.then_inc(o.dma.sem, 16)
                    elif o.signal:
                        ins.then_inc(self.esem[engname], 1)
                stats[engname] = (len(self.ops[engname]), nw)

            @block.tensor
            def _(eng):
                run("tensor", eng)

            @block.vector
            def _(eng):
                run("vector", eng)

            @block.scalar
            def _(eng):
                run("scalar", eng)

            @block.gpsimd
            def _(eng):
                run("gpsimd", eng)

            @block.sync
            def _(eng):
                run("sync", eng)
        self.stats.append(stats)
        self.ops = {e: [] for e in ENGS}
        self.phase += 1

    def mm(self, out, lhsT, rhs, start, stop, R, W, **kw):
        return self.op("tensor", lambda e: e.matmul(out, lhsT=lhsT, rhs=rhs, start=start, stop=stop, **kw), R, W)

    def tr(self, out, in_, ident, R, W):
        return self.op("tensor", lambda e: e.transpose(out, in_, ident), R, W)

    def act(self, out, in_, func, R, W, **kw):
        return self.op("scalar", lambda e: e.activation(out=out, in_=in_, func=func, **kw), R, W)

    def tt(self, eng, out, in0, in1, op, R, W):
        return self.op(eng, lambda e: e.tensor_tensor(out=out, in0=in0, in1=in1, op=op), R, W)

    def ts(self, eng, out, in0, s1, s2, op0, op1, R, W):
        if s2 is None:
            return self.op(eng, lambda e: e.tensor_scalar(out=out, in0=in0, scalar1=s1, scalar2=None, op0=op0), R, W)
        return self.op(eng, lambda e: e.tensor_scalar(out=out, in0=in0, scalar1=s1, scalar2=s2, op0=op0, op1=op1), R, W)

    def stt(self, out, in0, scalar, in1, op0, op1, R, W):
        return self.op("vector", lambda e: e.scalar_tensor_tensor(out=out, in0=in0, scalar=scalar, in1=in1, op0=op0, op1=op1), R, W)

    def cp(self, eng, out, in_, R, W):
        if eng == "scalar":
            return self.op(eng, lambda e: e.copy(out=out, in_=in_), R, W)
        return self.op(eng, lambda e: e.tensor_copy(out=out, in_=in_), R, W)

    def dma(self, eng, out, in_, R, W, sem):
        return self.op(eng, lambda e: e.dma_start(out=out, in_=in_), R, W, dma=sem)


def build(debug=False, stage=99):
    nc = bass.Bass("TRN2", target_bir_lowering=False)

    def din(name, shape):
        return nc.dram_tensor(name, shape, F32, kind="ExternalInput").ap()

    skind = "ExternalOutput" if debug else "Internal"

    def dsc(name, shape, dt):
        return nc.dram_tensor(name, shape, dt, kind=skind).ap()

    xT = din("xT", [D, T])
    pT = din("pT", [256, T])
    w_in = din("w_in", [D, 10248])
    gvec = din("gvec", [128, 64])
    b_f = din("b_f", [8, 1])
    conv_w = din("conv_w", [128, 24])
    w_a = din("w_a", [1024, D])
    w_b = din("w_b", [1024, D])
    w_o = din("w_o", [D, D])
    w_r = din("w_r", [D, 36])
    b_r = din("b_r", [128, 36])
    w_gu = din("w_gu", [NE, D, 1024])
    w_dn = din("w_dn", [NE, 512, D])
    w_pg = din("w_pg", [D, D])
    w_pe = din("w_pe", [256, D])
    cfd = din("cf", [128, NCF])
    sel8d = din("sel8", [8, 1032])
    tabinit = din("tabinit", [128, TW])
    yT = nc.dram_tensor("yT", [D, T], F32, kind="ExternalOutput").ap()

    qT_d = dsc("qT_d", [1024, T], BF16)
    kT_d = dsc("kT_d", [1024, T], BF16)
    v_d = dsc("v_d", [T, 1024], BF16)
    bmT_d = dsc("bmT_d", [1024, T], BF16)
    gaT_d = dsc("gaT_d", [D, T], BF16)
    gbT_d = dsc("gbT_d", [D, T], BF16)
    negc_d = dsc("negc_d", [8, T], F32)
    attnT_d = dsc("attnT_d", [1024, T], BF16)
    mgT_d = dsc("mgT_d", [D, T], BF16)
    x1T_d = dsc("x1T_d", [D, T], F32)
    hn_d = dsc("hn_d", [T, D], BF16)
    tab_d = dsc("tab_d", [NRT * 128, TW], F32)
    ybuf_d = dsc("ybuf_d", [2 * T + 128, D], F32)
    dbg_x2 = dsc("dbg_x2", [D, T], F32) if debug else None

    def chunked(ap):
        return ap.rearrange("(c p) n -> p c n", p=128)

    with contextlib.ExitStack() as glob:
        P = Prog(nc, glob)

        sbn = [0]

        def sb(st, name, shape, dt):
            sbn[0] += 1
            return st.enter_context(nc.sbuf_tensor("s%d_%s" % (sbn[0], name), shape, dt))

        cf = sb(glob, "cf", [128, NCF], F32)
        cb = sb(glob, "cb", [128, 512], BF16)
        gv = sb(glob, "gv", [128, 64], F32)
        sel8 = sb(glob, "sel8", [8, 1032], F32)
        Bc = Buf("consts")
        ps = [glob.enter_context(nc.psum_tensor("ps%d" % i, [128, 512], F32)) for i in range(6)]
        Bps = [Buf("ps%d" % i) for i in range(6)]
        pt = [glob.enter_context(nc.psum_tensor("pt%d" % i, [128, 1024], BF16)) for i in range(2)]
        Bpt = [Buf("pt%d" % i) for i in range(2)]
        Bd = Buf("dram", multi=True)

        P.dma("sync", cf[:], cfd, [], [Bc], Bc)
        P.dma("sync", gv[:], gvec, [], [Bc], Bc)
        P.dma("sync", sel8[:], sel8d, [], [Bc], Bc)
        P.dma("gpsimd", cb[:], cfd[:, 0:512], [], [Bc], Bc)
        identb = cb[:, C_ID:C_ID + 128]
        onesb = cb[:, C_ONE:C_ONE + 128]
        trib = cb[:, C_TRI:C_TRI + 128]
        usb = cb[:, C_US:C_US + 128]
        identf = cf[:, C_ID:C_ID + 128]
        G_MIX, G_FFN, G_PLE, G_FIN = 0, 16, 32, 48

        evac_ctr = [0]

        def evac(out, in_, R, W):
            evac_ctr[0] += 1
            if evac_ctr[0] % 2:
                return P.cp("scalar", out, in_, R, W)
            return P.cp("vector", out, in_, R, W)

        def rms_stats(xblk, Bx, n, sq, Bsq, psn, Bpsn, srt, Bsrt, rstd, Brstd):
            P.act(sq[:, :, 0:n], xblk[:, :, 0:n], AF.Square, [Bx], [Bsq])
            for c in range(KC):
                P.mm(psn[:, 0:n], onesb, sq[:, c, 0:n], c == 0, c == KC - 1, [Bsq, Bc], [Bpsn])
            P.act(srt[:, 0:n], psn[:, 0:n], AF.Sqrt, [Bpsn], [Bsrt], bias=EPS, scale=1.0 / D)
            P.op("vector", lambda e: e.reciprocal(out=rstd[:, 0:n], in_=srt[:, 0:n]), [Bsrt], [Brstd])

        with contextlib.ExitStack() as sA:
            hT = sb(sA, "hT", [128, KC, T], BF16)
            BhT = [Buf("hT%d" % i) for i in range(16)]
            with contextlib.ExitStack() as s0:
                xb = [sb(s0, "xb%d" % i, [128, KC, 256], F32) for i in range(2)]
                Bxb = [Buf("xb%d" % i) for i in range(2)]
                sq = sb(s0, "sq", [128, KC, 256], BF16)
                Bsq = Buf("sq")
                srt = sb(s0, "srt", [128, 256], F32)
                Bsrt = Buf("srt")
                rstd = sb(s0, "rstd", [128, 256], F32)
                Brstd = Buf("rstd")
                xTv = chunked(xT)
                for tb in range(16):
                    b = tb % 2
                    P.dma("sync", xb[b][:], xTv[:, :, tb * 256:(tb + 1) * 256], [], [Bxb[b]], Bxb[b])
                    rms_stats(xb[b], Bxb[b], 256, sq, Bsq, ps[0], Bps[0], srt, Bsrt, rstd, Brstd)
                    for c in range(KC):
                        P.stt(hT[:, c, tb * 256:(tb + 1) * 256], xb[b][:, c, :], gv[:, G_MIX + c:G_MIX + c + 1],
                              rstd[:], ALU.mult, ALU.mult, [Bxb[b], Brstd, Bc], [BhT[tb]])
                P.barrier()
                P.emit()
            if stage >= 1:
              with contextlib.ExitStack() as s1:
                wsl = [sb(s1, "wsl%d" % i, [128, KC, 512], BF16) for i in range(2)]
                Bw = [Buf("wsl%d" % i) for i in range(2)]
                stg = [sb(s1, "stg%d" % i, [128, 4, 512], BF16) for i in range(2)]
                Bstg = [Buf("stg%d" % i) for i in range(2)]
                usb_ = sb(s1, "u_sb", [128, 512], F32)
                Bus = Buf("u_sb")
                zb = sb(s1, "zb", [128, 514], F32)
                Bz = Buf("zb")
                acc = sb(s1, "cacc", [128, 512], F32)
                Bacc = Buf("cacc")
                cw = sb(s1, "cw", [128, 24], F32)
                negb = sb(s1, "negb", [8, 1], F32)
                lsp = sb(s1, "lsp", [8, 512], F32)
                Blsp = Buf("lsp")
                one8 = sb(s1, "one8", [8, 512], F32)
                ncs = sb(s1, "ncs", [8, T], F32)
                Bncs = Buf("ncs")
                Bcw = Buf("cw")
                P.dma("sync", cw[:], conv_w, [], [Bcw], Bcw)
                P.dma("sync", negb[:], b_f, [], [Bcw], Bcw)
                P.ts("vector", negb[:], negb[:], -1.0, None, ALU.mult, None, [Bcw], [Bcw])
                P.op("vector", lambda e: e.memset(one8[:], 1.0), [], [Bcw])
                w_in_v = chunked(w_in)
                allh = list(BhT)
                slab_i = [0]
                psr = [0]

                def next_ps():
                    psr[0] = (psr[0] + 1) % 6
                    return psr[0]

                def load_slab(cols):
                    i = slab_i[0] % 2
                    slab_i[0] += 1
                    o = 0
                    for (c0, n) in cols:
                        P.dma("gpsimd", wsl[i][:, :, o:o + n], w_in_v[:, :, c0:c0 + n], [], [Bw[i]], Bw[i])
                        o += n
                    return i

                stg_i = [0]

                def fm_plain(col0, dest, row0, sigmoid):
                    i = load_slab([(col0, 512)])
                    for tb in range(8):
                        si = stg_i[0] % 2
                        stg_i[0] += 1
                        for m in range(4):
                            pi = next_ps()
                            for c in range(KC):
                                P.mm(ps[pi][:], wsl[i][:, c, m * 128:(m + 1) * 128], hT[:, c, tb * 512:(tb + 1) * 512],
                                     c == 0, c == KC - 1, [Bw[i], BhT[2 * tb], BhT[2 * tb + 1]], [Bps[pi]])
                            if sigmoid:
                                P.act(stg[si][:, m, :], ps[pi][:], AF.Sigmoid, [Bps[pi]], [Bstg[si]])
                            else:
                                evac(stg[si][:, m, :], ps[pi][:], [Bps[pi]], [Bstg[si]])
                        P.dma("sync", dest[row0:row0 + 512, tb * 512:(tb + 1) * 512].rearrange("(m p) t -> p m t", p=128),
                              stg[si][:], [Bstg[si]], [Bd], Bstg[si])

                for s in range(2):
                    fm_plain(s * 512, qT_d, s * 512, False)
                for s in range(2):
                    fm_plain(1024 + s * 512, kT_d, s * 512, False)
                for s in range(2):
                    i = load_slab([(2048 + s * 512, 512)])
                    for t4 in range(8):
                        si = stg_i[0] % 2
                        stg_i[0] += 1
                        for j in range(4):
                            tt_ = t4 * 4 + j
                            pi = next_ps()
                            for c in range(KC):
                                P.mm(ps[pi][:], hT[:, c, tt_ * 128:(tt_ + 1) * 128], wsl[i][:, c, :],
                                     c == 0, c == KC - 1, [Bw[i], BhT[tt_ // 2]], [Bps[pi]])
                            evac(stg[si][:, j, :], ps[pi][:], [Bps[pi]], [Bstg[si]])
                        P.dma("sync", v_d[t4 * 512:(t4 + 1) * 512, s * 512:(s + 1) * 512].rearrange("(j p) n -> p j n", p=128),
                              stg[si][:], [Bstg[si]], [Bd], Bstg[si])
                i = load_slab([(3072, 8)])
                for tb in range(8):
                    pi = next_ps()
                    for c in range(KC):
                        P.mm(ps[pi][0:8, :], wsl[i][:, c, 0:8], hT[:, c, tb * 512:(tb + 1) * 512],
                             c == 0, c == KC - 1, [Bw[i], BhT[2 * tb], BhT[2 * tb + 1]], [Bps[pi]])
                    P.act(lsp[:], ps[pi][0:8, :], AF.Exp, [Bps[pi], Bcw], [Blsp], bias=negb[:, 0:1], scale=-1.0)
                    P.act(lsp[:], lsp[:], AF.Ln, [Blsp], [Blsp], bias=1.0, scale=1.0)
                    init = 0.0 if tb == 0 else ncs[:, tb * 512 - 1:tb * 512]
                    P.op("vector", lambda e, tb=tb, init=init: e.tensor_tensor_scan(
                        out=ncs[:, tb * 512:(tb + 1) * 512], data0=one8[:], data1=lsp[:], initial=init,
                        op0=ALU.mult, op1=ALU.add), [Blsp, Bcw, Bncs], [Bncs])
                P.dma("sync", negc_d, ncs[:], [Bncs], [Bd], Bncs)
                for j in range(8):
                    i = load_slab([(3080 + j * 128, 128), (4104 + j * 128, 128), (5128 + j * 128, 128)])
                    P.op("vector", lambda e: e.memset(zb[:, 0:2], 0.0), [], [Bz])
                    for tb in range(8):
                        if tb % 4 == 0:
                            si = stg_i[0] % 2
                            stg_i[0] += 1
                        pis = []
                        for m in range(3):
                            pi = next_ps()
                            pis.append(pi)
                            for c in range(KC):
                                P.mm(ps[pi][:], wsl[i][:, c, m * 128:(m + 1) * 128], hT[:, c, tb * 512:(tb + 1) * 512],
                                     c == 0, c == KC - 1, [Bw[i], BhT[2 * tb], BhT[2 * tb + 1]], [Bps[pi]])
                        pu, pbg, pcg = pis
                        P.cp("scalar", usb_[:], ps[pu][:], [Bps[pu]], [Bus])
                        P.tt("vector", zb[:, 2:514], ps[pcg][:], usb_[:], ALU.mult, [Bps[pcg], Bus], [Bz])
                        P.ts("vector", acc[:], zb[:, 2:514], cw[:, j * 3 + 2:j * 3 + 3], None, ALU.mult, None, [Bz, Bcw], [Bacc])
                        P.stt(acc[:], zb[:, 1:513], cw[:, j * 3 + 1:j * 3 + 2], acc[:], ALU.mult, ALU.add, [Bz, Bcw, Bacc], [Bacc])
                        P.stt(acc[:], zb[:, 0:512], cw[:, j * 3 + 0:j * 3 + 1], acc[:], ALU.mult, ALU.add, [Bz, Bcw, Bacc], [Bacc])
                        P.tt("vector", stg[si][:, tb % 4, :], ps[pbg][:], acc[:], ALU.mult, [Bps[pbg], Bacc], [Bstg[si]])
                        P.cp("vector", zb[:, 0:2], zb[:, 512:514], [Bz], [Bz])
                        if tb % 4 == 3:
                            t0 = (tb - 3) * 512
                            P.dma("sync", bmT_d[j * 128:(j + 1) * 128, t0:t0 + 2048].rearrange("p (m t) -> p m t", m=4),
                                  stg[si][:], [Bstg[si]], [Bd], Bstg[si])
                for s in range(4):
                    fm_plain(6152 + s * 512, gaT_d, s * 512, True)
                for s in range(4):
                    fm_plain(6152 + 2048 + s * 512, gbT_d, s * 512, True)
                P.barrier()
                P.emit()

        if stage >= 2:
          with contextlib.ExitStack() as sB:
            qh = [sb(sB, "qh%d" % i, [128, T], BF16) for i in range(2)]
            kh = [sb(sB, "kh%d" % i, [128, T], BF16) for i in range(2)]
            vh = [sb(sB, "vh%d" % i, [128, 32, 129], BF16) for i in range(2)]
            ncq = [sb(sB, "ncq%d" % i, [128, T], F32) for i in range(2)]
            Bq = [Buf("qh%d" % i) for i in range(2)]
            Bk = [Buf("kh%d" % i) for i in range(2)]
            Bv = [Buf("vh%d" % i) for i in range(2)]
            Bncq = [Buf("ncq%d" % i) for i in range(2)]
            ncs2 = sb(sB, "ncs2", [8, T], F32)
            Bn2 = Buf("ncs2")
            nck = sb(sB, "nck", [128, 32, 8], F32)
            Bnck = Buf("nck")
            rden = sb(sB, "rden", [128, 4], F32)
            Brd = Buf("rden")
            atok = [sb(sB, "atok%d" % i, [128, 4, 128], BF16) for i in range(2)]
            Bat = [Buf("atok%d" % i) for i in range(2)]
            ast = [sb(sB, "ast%d" % i, [128, 512], BF16) for i in range(2)]
            Bast = [Buf("ast%d" % i) for i in range(2)]
            P.dma("sync", ncs2[:], negc_d, [Bd], [Bn2], Bn2)
            for j in range(32):
                P.mm(ps[5][:, j * 8:(j + 1) * 8], ncs2[:, j * 128:(j + 1) * 128], sel8[:, 1024:1032], True, True, [Bn2, Bc], [Bps[5]])
            P.cp("vector", nck[:].rearrange("p j h -> p (j h)"), ps[5][:, 0:256], [Bps[5]], [Bnck])
            for i in range(2):
                P.op("gpsimd", lambda e, i=i: e.memset(vh[i][:, :, 128:129], 1.0), [], [Bv[i]])
            pend = []
            sidx = [0]
            blk = [0]
            ACCB = [2, 3, 4, 5]
            rden = sb(sB, "rden2", [128, 2, 4], F32)

            def loads(h):
                hb = h % 2
                P.dma("sync", qh[hb][:], qT_d[h * 128:(h + 1) * 128, :], [Bd], [Bq[hb]], Bq[hb])
                P.dma("sync", kh[hb][:], kT_d[h * 128:(h + 1) * 128, :], [Bd], [Bk[hb]], Bk[hb])
                P.dma("sync", vh[hb][:, :, 0:128], v_d[:, h * 128:(h + 1) * 128].rearrange("(j p) d -> p j d", p=128),
                      [Bd], [Bv[hb]], Bv[hb])
                P.dma("sync", ncq[hb][:], negc_d[h, :].partition_broadcast(128), [Bd], [Bncq[hb]], Bncq[hb])

            loads(0)
            NT_ = 5
            SB_ = [ps[0], ps[1], pt[1][:].bitcast(F32)]
            BSB_ = [Bps[0], Bps[1], Bpt[1]]
            tS = [sb(sB, "tSx%d" % i, [128, 512], F32) for i in range(NT_)]
            BtS = [Buf("tSx%d" % i) for i in range(NT_)]
            pTt = [sb(sB, "pTx%d" % i, [128, 512], BF16) for i in range(NT_)]
            BpT = [Buf("pTx%d" % i) for i in range(NT_)]
            LOOK = 3
            tiles = []
            for h in range(NH):
                for qb in range(8):
                    for kt in range(4 * (qb + 1)):
                        tiles.append((h, qb, kt))

            def front(n):
                h, qb, kt = tiles[n]
                hb = h % 2
                dj = kt - 4 * qb
                qlo = max(dj, 0) * 128
                si = n % 3
                ti = n % NT_
                q0 = qb * 512 + qlo
                q1 = (qb + 1) * 512
                P.mm(SB_[si][:, qlo:512], kh[hb][:, kt * 128:(kt + 1) * 128], qh[hb][:, q0:q1], True, True,
                     [Bk[hb], Bq[hb]], [BSB_[si]])
                P.stt(tS[ti][:, qlo:512], SB_[si][:, qlo:512], QSCALE, ncq[hb][:, q0:q1], ALU.mult, ALU.subtract,
                      [BSB_[si], Bncq[hb]], [BtS[ti]])
                P.act(pTt[ti][:, qlo:512], tS[ti][:, qlo:512], AF.Exp, [BtS[ti], Bnck], [BpT[ti]],
                      bias=nck[:, kt, h:h + 1], scale=1.0)
                if dj >= 0:
                    P.tt("gpsimd", pTt[ti][:, qlo:qlo + 128], pTt[ti][:, qlo:qlo + 128], trib, ALU.mult,
                         [BpT[ti], Bc], [BpT[ti]])

            def back(n):
                nonlocal pend
                h, qb, kt = tiles[n]
                hb = h % 2
                if qb == 0 and kt == 0 and h + 1 < NH:
                    loads(h + 1)
                dj = kt - 4 * qb
                ti = n % NT_
                for qs in range(4):
                    if qs < dj:
                        continue
                    a = ACCB[qs]
                    P.mm(ps[a][:, 0:129], pTt[ti][:, qs * 128:(qs + 1) * 128], vh[hb][:, kt, :],
                         kt == 0, kt == 4 * qb + qs, [BpT[ti], Bv[hb]], [Bps[a]])
                if kt == 1 and pend:
                    for f in pend:
                        f()
                    pend = []
                if kt == 4 * (qb + 1) - 1:
                    ab = blk[0] % 2
                    blk[0] += 1
                    for qs in range(4):
                        a = ACCB[qs]
                        P.op("vector", lambda e, a=a, qs=qs, ab=ab: e.reciprocal(out=rden[:, ab, qs:qs + 1], in_=ps[a][:, 128:129]),
                             [Bps[a]], [Brd])
                        P.act(atok[ab][:, qs, :], ps[a][:, 0:128], AF.Copy, [Bps[a], Brd], [Bat[ab]], scale=rden[:, ab, qs:qs + 1])

                    def fin(h=h, qb=qb, ab=ab):
                        for qs in range(4):
                            P.tr(pt[0][:, qs * 128:(qs + 1) * 128], atok[ab][:, qs, :], identb, [Bat[ab], Bc], [Bpt[0]])
                        P.cp("vector", ast[ab][:], pt[0][:, 0:512], [Bpt[0]], [Bast[ab]])
                        P.dma("sync", attnT_d[h * 128:(h + 1) * 128, qb * 512:(qb + 1) * 512], ast[ab][:], [Bast[ab]], [Bd], Bast[ab])
                    pend.append(fin)

            for n in range(len(tiles) + LOOK):
                if n < len(tiles):
                    front(n)
                if n - LOOK >= 0:
                    back(n - LOOK)
            for f in pend:
                f()
            P.barrier()
            P.emit()

        if stage >= 3:
          with contextlib.ExitStack() as sC:
            wa = sb(sC, "wa", [128, 8, D], BF16)
            wb = sb(sC, "wb", [128, 8, D], BF16)
            Bwab = Buf("wab")
            P.dma("gpsimd", wa[:], chunked(w_a), [], [Bwab], Bwab)
            P.dma("gpsimd", wb[:], chunked(w_b), [], [Bwab], Bwab)
            at = [sb(sC, "at%d" % i, [128, 8, 512], BF16) for i in range(2)]
            bm = [sb(sC, "bm%d" % i, [128, 8, 512], BF16) for i in range(2)]
            ga = [sb(sC, "ga%d" % i, [128, KC, 512], BF16) for i in range(2)]
            gb = [sb(sC, "gb%d" % i, [128, KC, 512], BF16) for i in range(2)]
            Bin = [Buf("c1in%d" % i) for i in range(2)]
            t1 = [sb(sC, "t1_%d" % i, [128, 512], F32) for i in range(2)]
            t2 = [sb(sC, "t2_%d" % i, [128, 512], F32) for i in range(2)]
            Bt1 = [Buf("t1_%d" % i) for i in range(2)]
            Bt2 = [Buf("t2_%d" % i) for i in range(2)]
            mst = [sb(sC, "mst%d" % i, [128, 4, 512], BF16) for i in range(2)]
            Bmst = [Buf("mst%d" % i) for i in range(2)]

            def c1_loads(tb):
                b = tb % 2
                sl = slice(tb * 512, (tb + 1) * 512)
                P.dma("sync", at[b][:], chunked(attnT_d)[:, :, sl], [Bd], [Bin[b]], Bin[b])
                P.dma("sync", bm[b][:], chunked(bmT_d)[:, :, sl], [Bd], [Bin[b]], Bin[b])
                P.dma("sync", ga[b][:], chunked(gaT_d)[:, :, sl], [Bd], [Bin[b]], Bin[b])
                P.dma("sync", gb[b][:], chunked(gbT_d)[:, :, sl], [Bd], [Bin[b]], Bin[b])

            c1_loads(0)
            k = 0
            for tb in range(8):
                b = tb % 2
                if tb + 1 < 8:
                    c1_loads(tb + 1)
                for fg in range(KC):
                    pa = (2 * k) % 6
                    pb = (2 * k + 1) % 6
                    tb_ = k % 2
                    k += 1
                    if fg % 4 == 0:
                        mi = (tb * 4 + fg // 4) % 2
                    for c in range(8):
                        P.mm(ps[pa][:], wa[:, c, fg * 128:(fg + 1) * 128], at[b][:, c, :], c == 0, c == 7, [Bwab, Bin[b]], [Bps[pa]])
                    for c in range(8):
                        P.mm(ps[pb][:], wb[:, c, fg * 128:(fg + 1) * 128], bm[b][:, c, :], c == 0, c == 7, [Bwab, Bin[b]], [Bps[pb]])
                    P.tt("vector", t1[tb_][:], ps[pa][:], ga[b][:, fg, :], ALU.mult, [Bps[pa], Bin[b]], [Bt1[tb_]])
                    P.tt("vector", t2[tb_][:], ps[pb][:], gb[b][:, fg, :], ALU.mult, [Bps[pb], Bin[b]], [Bt2[tb_]])
                    P.tt("gpsimd", mst[mi][:, fg % 4, :], t1[tb_][:], t2[tb_][:], ALU.add, [Bt1[tb_], Bt2[tb_]], [Bmst[mi]])
                    if fg % 4 == 3:
                        r0 = (fg - 3) * 128
                        P.dma("sync", mgT_d[r0:r0 + 512, tb * 512:(tb + 1) * 512].rearrange("(m p) t -> p m t", p=128),
                              mst[mi][:], [Bmst[mi]], [Bd], Bmst[mi])
            P.barrier()
            P.emit()

        if stage >= 4:
          LGraw = sb(glob, "LGraw", [128, 32, 36], F32)
          SSq = sb(glob, "SSq", [128, 32], F32)
          BLG = Buf("lgraw", multi=True)
          with contextlib.ExitStack() as sC:
            wo = sb(sC, "wo", [128, KC, D], BF16)
            Bwo = Buf("wo")
            P.dma("gpsimd", wo[:, 0:8, :], chunked(w_o)[:, 0:8, :], [], [Bwo], Bwo)
            P.dma("gpsimd", wo[:, 8:16, :], chunked(w_o)[:, 8:16, :], [], [Bwo], Bwo)
            mg = [sb(sC, "mg%d" % i, [128, KC, 256], BF16) for i in range(3)]
            xb = [sb(sC, "xb%d" % i, [128, KC, 256], F32) for i in range(3)]
            Bmg = [Buf("mg%d" % i) for i in range(3)]
            Bxb = [Buf("xb%d" % i) for i in range(3)]
            sq = sb(sC, "sq", [128, KC, 256], BF16)
            Bsq = Buf("sq")
            srt = sb(sC, "srt", [128, 256], F32)
            Bsrt = Buf("srt")
            rstd = sb(sC, "rstd", [128, 256], F32)
            Brstd = Buf("rstd")
            hnT = sb(sC, "hnT", [128, KC, 256], BF16)
            BhnT = Buf("hnT")
            hnt = [sb(sC, "hnt%d" % i, [128, D], BF16) for i in range(2)]
            Bhnt = [Buf("hnt%d" % i) for i in range(2)]
            wr = sb(sC, "wr", [128, KC, 36], F32)
            brb = sb(sC, "brb", [128, 36], F32)
            Bwr = Buf("wr")
            P.dma("sync", wr[:], chunked(w_r), [], [Bwr], Bwr)
            P.dma("sync", brb[:], b_r, [], [Bwr], Bwr)
            for c in range(KC):
                P.ts("vector", wr[:, c, :], wr[:, c, :], gv[:, G_FFN + c:G_FFN + c + 1], None, ALU.mult, None, [Bwr, Bc], [Bwr])
            tin = sb(sC, "tin", [128, TW], F32)
            Btin = Buf("tin")
            P.dma("sync", tin[:], tabinit, [], [Btin], Btin)
            Btab = Buf("tab", multi=True)
            tabv = tab_d.rearrange("(j p) w -> p j w", p=128)
            for j in range(NRT):
                P.dma("sync", tabv[:, j, :], tin[:], [Btin], [Btab], Btin)
            def c2_loads(tb):
                b = tb % 3
                sl = slice(tb * 256, (tb + 1) * 256)
                P.dma("sync", mg[b][:], chunked(mgT_d)[:, :, sl], [Bd], [Bmg[b]], Bmg[b])
                P.dma("sync", xb[b][:], chunked(xT)[:, :, sl], [], [Bxb[b]], Bxb[b])

            def route1(tile_i, b, tsl):
                for c in range(KC):
                    P.mm(ps[4][:, 0:36], xb[b][:, c, tsl], wr[:, c, :], c == 0, c == KC - 1, [Bxb[b], Bwr], [Bps[4]])
                for c in range(KC):
                    P.mm(ps[5][:, 0:1], sq[:, c, tsl], onesb[:, 0:1], c == 0, c == KC - 1, [Bsq, Bc], [Bps[5]])
                P.cp("scalar", LGraw[:, tile_i, :], ps[4][:, 0:36], [Bps[4]], [BLG])
                P.cp("scalar", SSq[:, tile_i:tile_i + 1], ps[5][:, 0:1], [Bps[5]], [BLG])

            def x_steps(tb):
                b = tb % 3
                steps = []
                for fg in range(KC):
                    def st(fg=fg):
                        pi = fg % 2
                        for c in range(KC):
                            P.mm(ps[pi][:, 0:256], wo[:, c, fg * 128:(fg + 1) * 128], mg[b][:, c, :], c == 0, c == KC - 1,
                                 [Bwo, Bmg[b]], [Bps[pi]])
                        P.tt("vector", xb[b][:, fg, :], ps[pi][:, 0:256], xb[b][:, fg, :], ALU.add, [Bps[pi], Bxb[b]], [Bxb[b]])
                    steps.append(st)
                return steps

            def x_fin(tb):
                b = tb % 3
                P.dma("sync", chunked(x1T_d)[:, :, tb * 256:(tb + 1) * 256], xb[b][:], [Bxb[b]], [Bd], Bxb[b])

            def y_steps(tb):
                b = tb % 3

                def y0_():
                    rms_stats(xb[b], Bxb[b], 256, sq, Bsq, ps[3], Bps[3], srt, Bsrt, rstd, Brstd)
                    for c in range(KC):
                        P.stt(hnT[:, c, :], xb[b][:, c, :], gv[:, G_FFN + c:G_FFN + c + 1], rstd[:], ALU.mult, ALU.mult,
                              [Bxb[b], Brstd, Bc], [BhnT])

                def ytile(tt_):
                    tile_i = tb * 2 + tt_
                    hb_ = tile_i % 2
                    tsl = slice(tt_ * 128, (tt_ + 1) * 128)
                    for c4 in range(4):
                        pti = c4 % 2
                        for cc in range(4):
                            c = c4 * 4 + cc
                            P.tr(pt[pti][:, cc * 128:(cc + 1) * 128], hnT[:, c, tsl], identb, [BhnT, Bc], [Bpt[pti]])
                        evac(hnt[hb_][:, c4 * 512:(c4 + 1) * 512], pt[pti][:, 0:512], [Bpt[pti]], [Bhnt[hb_]])
                    P.dma("sync", hn_d[tile_i * 128:(tile_i + 1) * 128, :], hnt[hb_][:], [Bhnt[hb_]], [Bd], Bhnt[hb_])
                    route1(tile_i, b, tsl)
                return [y0_, (lambda: ytile(0)), (lambda: ytile(1))]

            pend2 = []
            c2_loads(0)
            for tb in range(17):
                if tb + 1 < 16:
                    c2_loads(tb + 1)
                X = x_steps(tb) if tb < 16 else []
                Y = y_steps(tb - 1) if tb >= 1 else []
                for fg in range(KC):
                    if X:
                        X[fg]()
                    if fg == 1 and Y:
                        Y[0]()
                    if fg == 6 and Y:
                        Y[1]()
                    if fg == 11 and Y:
                        Y[2]()
                if tb < 16:
                    x_fin(tb)
            P.barrier()
            P.emit()

          with contextlib.ExitStack() as sR:
            def W_(name, k):
                return sb(sR, name, [128, 32, k], F32)
            LG = W_("LG", 36)
            OHG = W_("OHG", 4)
            EG = W_("EG", 4)
            ESEL = W_("ESEL", 8)
            T8 = W_("T8", 8)
            OH1 = W_("OH1", 8)
            E2 = W_("E2", 8)
            OH2 = W_("OH2", 8)
            A1 = W_("A1", 32)
            A2 = W_("A2", 32)
            POS = W_("POS", 32)
            TMP = W_("TMP", 32)
            CNT = W_("CNT", 32)
            CAR = W_("CAR", 32)
            AbA = sb(sR, "AbA", [128, 32, 32], BF16)
            S_ = sb(sR, "Ssc", [128, 16, 32], F32)
            DST = sb(sR, "DST", [128, 64], I32)
            PAY = sb(sR, "PAY", [128, 64, TW], F32)
            brb2 = sb(sR, "brb2", [128, 36], F32)
            BR = Buf("R")
            Bbr2 = Buf("brb2")
            BPAY = Buf("PAY")
            P.dma("sync", brb2[:], b_r, [], [Bbr2], Bbr2)
            P.op("gpsimd", lambda e: e.memset(PAY[:], 0.0), [], [BPAY])
            RW = ([BR], [BR])
            sc = lambda i_: S_[:, i_, :]
            bc2 = lambda ap2, k_: ap2.unsqueeze(2).to_broadcast([128, 32, k_])
            flat = lambda t3: t3[:].rearrange("p t e -> p (t e)")
            vop = lambda fn, R=RW[0], Wr=RW[1]: P.op("vector", fn, R, Wr)
            P.act(sc(0), SSq[:], AF.Sqrt, [BLG], [BR], bias=EPS, scale=1.0 / D)
            vop(lambda e: e.reciprocal(out=sc(0), in_=sc(0)))
            P.tt("vector", LG[:], LGraw[:], bc2(sc(0), 36), ALU.mult, [BLG, BR], [BR])
            P.tt("vector", LG[:], LG[:], brb2[:].unsqueeze(1).to_broadcast([128, 32, 36]), ALU.add, [BR, Bbr2], [BR])
            vop(lambda e: e.reduce_max(out=sc(1), in_=LG[:, :, 0:4], axis=AX.X))
            P.tt("vector", OHG[:], LG[:, :, 0:4], bc2(sc(1), 4), ALU.is_equal, *RW)
            P.tt("vector", EG[:], LG[:, :, 0:4], bc2(sc(1), 4), ALU.subtract, *RW)
            P.act(EG[:], EG[:], AF.Exp, *RW)
            vop(lambda e: e.reduce_sum(out=sc(2), in_=EG[:], axis=AX.X))
            vop(lambda e: e.reciprocal(out=sc(3), in_=sc(2)))
            P.tt("vector", ESEL[:], LG[:, :, 4:12], bc2(OHG[:, :, 0], 8), ALU.mult, *RW)
            for g in range(1, 4):
                P.tt("vector", T8[:], LG[:, :, 4 + 8 * g:12 + 8 * g], bc2(OHG[:, :, g], 8), ALU.mult, *RW)
                P.tt("vector", ESEL[:], ESEL[:], T8[:], ALU.add, *RW)
            vop(lambda e: e.reduce_max(out=sc(4), in_=ESEL[:], axis=AX.X))
            P.tt("vector", OH1[:], ESEL[:], bc2(sc(4), 8), ALU.is_equal, *RW)
            P.stt(flat(E2), flat(OH1), -1e30, flat(ESEL), ALU.mult, ALU.add, *RW)
            vop(lambda e: e.reduce_max(out=sc(5), in_=E2[:], axis=AX.X))
            P.tt("vector", OH2[:], E2[:], bc2(sc(5), 8), ALU.is_equal, *RW)
            P.tt("vector", sc(6), sc(5), sc(4), ALU.subtract, *RW)
            P.act(sc(6), sc(6), AF.Exp, *RW)
            P.ts("vector", sc(6), sc(6), 1.0, None, ALU.add, None, *RW)
            vop(lambda e: e.reciprocal(out=sc(6), in_=sc(6)))
            P.tt("vector", sc(7), sc(6), sc(3), ALU.mult, *RW)
            P.tt("vector", sc(8), sc(3), sc(7), ALU.subtract, *RW)
            for g in range(4):
                P.tt("vector", A1[:, :, 8 * g:8 * g + 8], OH1[:], bc2(OHG[:, :, g], 8), ALU.mult, *RW)
                P.tt("vector", A2[:, :, 8 * g:8 * g + 8], OH2[:], bc2(OHG[:, :, g], 8), ALU.mult, *RW)
            P.tt("vector", AbA[:], A1[:], A2[:], ALU.add, *RW)
            for h_ in range(2):
                P.mm(ps[h_][:], usb, flat(AbA)[:, 512 * h_:512 * h_ + 512], True, True, [BR, Bc], [Bps[h_]])
                P.mm(ps[2 + h_][:], onesb, flat(AbA)[:, 512 * h_:512 * h_ + 512], True, True, [BR, Bc], [Bps[2 + h_]])
            for h_ in range(2):
                P.cp("scalar", flat(CNT)[:, 512 * h_:512 * h_ + 512], ps[2 + h_][:], [Bps[2 + h_]], [BR])
            vop(lambda e: e.memset(CAR[:, 0, :], 0.0))
            for t_ in range(1, 32):
                P.tt("vector", CAR[:, t_, :], CAR[:, t_ - 1, :], CNT[:, t_ - 1, :], ALU.add, *RW)
            for h_ in range(2):
                P.tt("vector", flat(POS)[:, 512 * h_:512 * h_ + 512], ps[h_][:], flat(CAR)[:, 512 * h_:512 * h_ + 512], ALU.add,
                     [Bps[h_], BR], [BR])
            P.ts("vector", flat(POS), flat(POS), float(CAP), None, ALU.min, None, *RW)
            P.tt("vector", POS[:], POS[:], cf[:, C_ES:C_ES + 32].unsqueeze(1).to_broadcast([128, 32, 32]), ALU.add, [BR, Bc], [BR])
            tokf = cf[:, C_TOK:C_TOK + 32]
            for kk in range(2):
                AK = A1 if kk == 0 else A2
                P.tt("vector", TMP[:], POS[:], AK[:], ALU.mult, *RW)
                vop(lambda e, kk=kk: e.reduce_sum(out=sc(9 + kk), in_=TMP[:], axis=AX.X))
                P.cp("vector", DST[:, kk * 32:(kk + 1) * 32], sc(9 + kk), [BR], [BR])
                P.cp("vector", PAY[:, kk * 32:(kk + 1) * 32, 0], tokf, [Bc], [BPAY])
                if kk == 0:
                    P.cp("vector", PAY[:, 0:32, 1], tokf, [Bc], [BPAY])
                else:
                    P.ts("vector", PAY[:, 32:64, 1], tokf, float(T), None, ALU.add, None, [Bc], [BPAY])
                P.cp("vector", PAY[:, kk * 32:(kk + 1) * 32, 2], sc(7 + kk), [BR], [BPAY])
            Bsc = Buf("scat")
            for j_ in range(64):
                P.op("gpsimd", lambda e, j_=j_: e.indirect_dma_start(
                    out=tab_d, out_offset=bass.IndirectOffsetOnAxis(ap=DST[:, j_:j_ + 1], axis=0),
                    in_=PAY[:, j_, :], in_offset=None), [BPAY, BR], [Bd], dma=Bsc)
            P.barrier()
            P.emit()

        if stage >= 5:
          with contextlib.ExitStack() as sE:
            wgu = [sb(sE, "wgu%d" % i, [128, KC, 1024], BF16) for i in range(2)]
            wdn = [sb(sE, "wdn%d" % i, [128, 4, D], BF16) for i in range(2)]
            Bwe = [Buf("we%d" % i) for i in range(2)]
            tb3 = [sb(sE, "tb3_%d" % i, [128, 3, TW], F32) for i in range(2)]
            Btb3 = [Buf("tb3_%d" % i) for i in range(2)]
            idx = [sb(sE, "idx%d" % i, [128, 3, 2], I32) for i in range(2)]
            Bidx = [Buf("idx%d" % i) for i in range(2)]
            xg = [[sb(sE, "xg%d_%d" % (i, s_), [128, D], BF16) for s_ in range(3)] for i in range(2)]
            Bxg = [[Buf("xg%d_%d" % (i, s_)) for s_ in range(3)] for i in range(2)]
            xgT = sb(sE, "xgT", [128, KC, CAP], BF16)
            BxgT = Buf("xgT")
            hTe = sb(sE, "hTe", [128, 4, CAP], BF16)
            BhTe = Buf("hTe")
            sg = [sb(sE, "sg%d" % i, [128, CAP], F32) for i in range(2)]
            Bsg = [Buf("sg%d" % i) for i in range(2)]
            ys = [sb(sE, "ys%d" % i, [128, D], F32) for i in range(3)]
            Bys = [Buf("ys%d" % i) for i in range(3)]

            def e_loads(e):
                b = e % 2
                P.dma("gpsimd", wgu[b][:, 0:8, :], chunked(w_gu[e])[:, 0:8, :], [], [Bwe[b]], Bwe[b])
                P.dma("gpsimd", wgu[b][:, 8:16, :], chunked(w_gu[e])[:, 8:16, :], [], [Bwe[b]], Bwe[b])
                P.dma("gpsimd", wdn[b][:], chunked(w_dn[e]), [], [Bwe[b]], Bwe[b])
                P.dma("sync", tb3[b][:], tab_d[e * CS:e * CS + CAP, :].rearrange("(s p) w -> p s w", p=128),
                      [Bd], [Btb3[b]], Btb3[b])
                P.cp("vector", idx[b][:], tb3[b][:, :, 0:2], [Btb3[b]], [Bidx[b]])
                for s_ in range(3):
                    P.op("gpsimd", lambda eng, b=b, s_=s_: eng.indirect_dma_start(
                        out=xg[b][s_][:], out_offset=None, in_=hn_d,
                        in_offset=bass.IndirectOffsetOnAxis(ap=idx[b][:, s_, 0:1], axis=0)),
                        [Bidx[b], Bd], [Bxg[b][s_]], dma=Bxg[b][s_])

            e_loads(0)
            k = 0
            for e in range(NE):
                b = e % 2
                if e + 1 < NE:
                    e_loads(e + 1)
                for s_ in range(3):
                    for c4 in range(4):
                        pti = k % 2
                        k += 1
                        for cc in range(4):
                            c = c4 * 4 + cc
                            P.tr(pt[pti][:, cc * 128:(cc + 1) * 128], xg[b][s_][:, c * 128:(c + 1) * 128], identb,
                                 [Bxg[b][s_], Bc], [Bpt[pti]])
                        evac(xgT[:, c4 * 4:c4 * 4 + 4, s_ * 128:(s_ + 1) * 128],
                             pt[pti][:, 0:512].rearrange("p (c t) -> p c t", c=4), [Bpt[pti]], [BxgT])
                for m in range(4):
                    pg_, pu_ = (2 * m) % 4, (2 * m + 1) % 4
                    sgi = m % 2
                    for c in range(KC):
                        P.mm(ps[pg_][:, 0:CAP], wgu[b][:, c, m * 128:(m + 1) * 128], xgT[:, c, :], c == 0, c == KC - 1,
                             [Bwe[b], BxgT], [Bps[pg_]])
                    for c in range(KC):
                        P.mm(ps[pu_][:, 0:CAP], wgu[b][:, c, 512 + m * 128:512 + (m + 1) * 128], xgT[:, c, :], c == 0, c == KC - 1,
                             [Bwe[b], BxgT], [Bps[pu_]])
                    P.act(sg[sgi][:], ps[pg_][:, 0:CAP], AF.Silu, [Bps[pg_]], [Bsg[sgi]])
                    P.tt("vector", hTe[:, m, :], sg[sgi][:], ps[pu_][:, 0:CAP], ALU.mult, [Bsg[sgi], Bps[pu_]], [BhTe])
                for s_ in range(3):
                    for n in range(4):
                        pi = 4 + (n % 2)
                        for m in range(4):
                            P.mm(ps[pi][:], hTe[:, m, s_ * 128:(s_ + 1) * 128], wdn[b][:, m, n * 512:(n + 1) * 512], m == 0, m == 3,
                                 [BhTe, Bwe[b]], [Bps[pi]])
                        if n % 2:
                            P.act(ys[s_][:, n * 512:(n + 1) * 512], ps[pi][:], AF.Copy, [Bps[pi], Btb3[b]], [Bys[s_]],
                                  scale=tb3[b][:, s_, 2:3])
                        else:
                            P.ts("vector", ys[s_][:, n * 512:(n + 1) * 512], ps[pi][:], tb3[b][:, s_, 2:3], None, ALU.mult, None,
                                 [Bps[pi], Btb3[b]], [Bys[s_]])
                    P.op("gpsimd", lambda eng, b=b, s_=s_: eng.indirect_dma_start(
                        out=ybuf_d, out_offset=bass.IndirectOffsetOnAxis(ap=idx[b][:, s_, 1:2], axis=0),
                        in_=ys[s_][:], in_offset=None), [Bys[s_], Bidx[b]], [Bd], dma=Bys[s_])
            P.barrier()
            P.emit()

        if stage >= 6:
          with contextlib.ExitStack() as sF:
            wpg = sb(sF, "wpg", [128, KC, D], BF16)
            wpe = sb(sF, "wpe", [128, 2, D], BF16)
            Bwp = Buf("wp")
            P.dma("gpsimd", wpg[:, 0:8, :], chunked(w_pg)[:, 0:8, :], [], [Bwp], Bwp)
            P.dma("gpsimd", wpg[:, 8:16, :], chunked(w_pg)[:, 8:16, :], [], [Bwp], Bwp)
            P.dma("gpsimd", wpe[:], chunked(w_pe), [], [Bwp], Bwp)
            xb = [sb(sF, "xb%d" % i, [128, KC, 256], F32) for i in range(3)]
            Bxb = [Buf("xb%d" % i) for i in range(3)]
            y0 = [sb(sF, "y0_%d" % i, [128, D], F32) for i in range(4)]
            By = [Buf("y%d" % i) for i in range(4)]
            pbf = [sb(sF, "pbf%d" % i, [128, 2, 256], BF16) for i in range(3)]
            Bpb = [Buf("pbf%d" % i) for i in range(3)]
            sqA = sb(sF, "sqA", [128, KC, 256], BF16)
            sqB = sqA
            BsqA = Buf("sqA")
            BsqB = BsqA
            srtA = sb(sF, "srtA", [128, 256], F32)
            srtB = sb(sF, "srtB", [128, 256], F32)
            BsrtA, BsrtB = Buf("srtA"), Buf("srtB")
            rstdA = sb(sF, "rstdA", [128, 256], F32)
            rstdB = sb(sF, "rstdB", [128, 256], F32)
            BrstdA, BrstdB = Buf("rstdA"), Buf("rstdB")
            hp = [sb(sF, "hp%d" % i, [128, KC, 256], BF16) for i in range(2)]
            Bhp = [Buf("hp%d" % i) for i in range(2)]
            sgt = [sb(sF, "sgt%d" % i, [128, 256], F32) for i in range(2)]
            Bsgt = [Buf("sgt%d" % i) for i in range(2)]

            def f_loads(tb):
                b = tb % 3
                sl = slice(tb * 256, (tb + 1) * 256)
                P.dma("sync", xb[b][:], chunked(x1T_d)[:, :, sl], [Bd], [Bxb[b]], Bxb[b])
                P.dma("gpsimd", pbf[b][:], chunked(pT)[:, :, sl], [], [Bpb[b]], Bpb[b])
                for tt_ in range(2):
                    tile_i = tb * 2 + tt_
                    yi = (tb % 2) * 2 + tt_
                    P.dma("sync", y0[yi][:], ybuf_d[tile_i * 128:(tile_i + 1) * 128, :], [Bd], [By[yi]], By[yi])
                    P.op("gpsimd", lambda e, yi=yi, tile_i=tile_i: e.dma_start(
                        out=y0[yi][:], in_=ybuf_d[T + tile_i * 128:T + (tile_i + 1) * 128, :], accum_op=ALU.add),
                        [Bd], [By[yi]], dma=By[yi])

            def f_front_steps(tb):
                b = tb % 3
                hb2 = tb % 2
                steps = []

                def pool_():
                    pass
                steps.append(pool_)
                for c in range(KC):
                    def st(c=c):
                        pi = c % 2
                        for tt_ in range(2):
                            yi = (tb % 2) * 2 + tt_
                            P.tr(ps[pi][:, tt_ * 128:(tt_ + 1) * 128], y0[yi][:, c * 128:(c + 1) * 128], identf, [By[yi], Bc], [Bps[pi]])
                        P.tt("vector", xb[b][:, c, :], ps[pi][:, 0:256], xb[b][:, c, :], ALU.add, [Bps[pi], Bxb[b]], [Bxb[b]])
                    steps.append(st)

                def tail_():
                    if debug:
                        P.dma("sync", chunked(dbg_x2)[:, :, tb * 256:(tb + 1) * 256], xb[b][:], [Bxb[b]], [Bd], Bxb[b])
                    rms_stats(xb[b], Bxb[b], 256, sqA, BsqA, ps[5], Bps[5], srtA, BsrtA, rstdA, BrstdA)
                    for c in range(KC):
                        P.stt(hp[hb2][:, c, :], xb[b][:, c, :], gv[:, G_PLE + c:G_PLE + c + 1], rstdA[:], ALU.mult, ALU.mult,
                              [Bxb[b], BrstdA, Bc], [Bhp[hb2]])
                steps.append(tail_)
                return steps

            def f_back_steps(tb):
                b = tb % 3
                hb2 = tb % 2
                steps = []
                for fg in range(KC):
                    def st(fg=fg):
                        si_ = fg % 2
                        pg_ = 2 + (fg % 2)
                        for c in range(KC):
                            P.mm(ps[pg_][:, 0:256], wpg[:, c, fg * 128:(fg + 1) * 128], hp[hb2][:, c, :], c == 0, c == KC - 1, [Bwp, Bhp[hb2]], [Bps[pg_]])
                        for c in range(2):
                            P.mm(ps[4][:, 0:256], wpe[:, c, fg * 128:(fg + 1) * 128], pbf[b][:, c, :], c == 0, c == 1, [Bwp, Bpb[b]], [Bps[4]])
                        P.act(sgt[si_][:], ps[pg_][:, 0:256], AF.Sigmoid, [Bps[pg_]], [Bsgt[si_]])
                        P.tt("vector", sgt[si_][:], sgt[si_][:], ps[4][:, 0:256], ALU.mult, [Bsgt[si_], Bps[4]], [Bsgt[si_]])
                        P.tt("gpsimd", xb[b][:, fg, :], xb[b][:, fg, :], sgt[si_][:], ALU.add, [Bsgt[si_], Bxb[b]], [Bxb[b]])
                    steps.append(st)

                def tail_():
                    rms_stats(xb[b], Bxb[b], 256, sqB, BsqB, ps[5], Bps[5], srtB, BsrtB, rstdB, BrstdB)
                    for c in range(KC):
                        P.stt(xb[b][:, c, :], xb[b][:, c, :], gv[:, G_FIN + c:G_FIN + c + 1], rstdB[:], ALU.mult, ALU.mult,
                              [Bxb[b], BrstdB, Bc], [Bxb[b]])
                    P.dma("sync", chunked(yT)[:, :, tb * 256:(tb + 1) * 256], xb[b][:], [Bxb[b]], [Bd], Bxb[b])
                steps.append(tail_)
                return steps

            f_loads(0)
            f_loads(1)
            for st_ in f_front_steps(0):
                st_()
            for tb in range(16):
                Fs = []
                if tb + 2 < 16:
                    f_loads(tb + 2)
                if tb + 1 < 16:
                    Fs = f_front_steps(tb + 1)
                Bs = f_back_steps(tb)
                if Fs:
                    Fs[0]()
                for i_ in range(KC):
                    Bs[i_]()
                    if Fs and i_ < 8:
                        Fs[1 + 2 * i_]()
                        Fs[2 + 2 * i_]()
                    if Fs and i_ == 8:
                        Fs[17]()
                Bs[16]()
            P.barrier()
            P.emit()
        if stage < 6:
            pass
    return nc


def _consts():
    cf = np.zeros((128, NCF), np.float32)
    cf[:, C_ID:C_ID + 128] = np.eye(128, dtype=np.float32)
    cf[:, C_ONE:C_ONE + 128] = 1.0
    k = np.arange(128)[:, None]
    q = np.arange(128)[None, :]
    cf[:, C_TRI:C_TRI + 128] = (q >= k)
    cf[:, C_US:C_US + 128] = (k < q)
    cf[:, C_IOTA] = np.arange(128)
    cf[:, C_ES:C_ES + 32] = (np.arange(32) * CS)[None, :]
    cf[:, C_TOK:C_TOK + 32] = np.arange(32)[None, :] * 128 + np.arange(128)[:, None]
    sel8 = np.zeros((8, 1032), np.float32)
    for h in range(8):
        sel8[h, h * 128:(h + 1) * 128] = 1.0
        sel8[h, 1024 + h] = 1.0
    tabinit = np.zeros((128, TW), np.float32)
    tabinit[:, 1] = 2 * T
    return cf, sel8, tabinit


def _shared_inputs(inp):
    f = lambda a: np.ascontiguousarray(a, dtype=np.float32)
    cf, sel8, tabinit = _consts()
    gl = lambda g: g.reshape(16, 128).T
    gvec = np.concatenate([gl(inp["g_mix"][0]), gl(inp["g_ffn"][0]), gl(inp["g_ple"][0]), gl(inp["g_final"])], axis=1)
    cw = inp["conv_w"][0].reshape(3, 8, 128).transpose(2, 1, 0).reshape(128, 24)
    w_r = np.concatenate([inp["w_router_group"][0], inp["w_router_expert"][0]], axis=1)
    b_r = np.concatenate([inp["b_router_group"][0], inp["b_router_expert"][0]])[None, :].repeat(128, axis=0)
    return {
        "w_in": f(inp["w_in"][0]), "gvec": f(gvec), "b_f": f(inp["b_f"][0].reshape(8, 1)), "conv_w": f(cw),
        "w_a": f(inp["w_branch_a"][0]), "w_b": f(inp["w_branch_b"][0]), "w_o": f(inp["w_out"][0]),
        "w_r": f(w_r), "b_r": f(b_r), "w_gu": f(inp["w_gate_up"][0]), "w_dn": f(inp["w_down"][0]),
        "w_pg": f(inp["w_ple_gate"][0]), "w_pe": f(inp["w_ple_proj"][0]),
        "cf": cf, "sel8": sel8, "tabinit": tabinit,
    }


def _core_inputs(inp, b, shared):
    m = dict(shared)
    m["xT"] = np.ascontiguousarray(np.asarray(inp["x"][b], dtype=np.float32).T)
    m["pT"] = np.ascontiguousarray(np.asarray(inp["p"][0, b], dtype=np.float32).T)
    return m


def kernel(**inputs):
    inp = {k: np.asarray(v) for k, v in inputs.items()}
    nb = inp["x"].shape[0]
    shared = _shared_inputs(inp)
    nc = build()
    in_maps = [_core_inputs(inp, b, shared) for b in range(nb)]
    res = run_bass_kernel_spmd(nc, in_maps, core_ids=list(range(nb)))
    out = np.empty((nb, T, D), np.float32)
    for b in range(nb):
        out[b] = np.asarray(res.results[b]["yT"]).T
    return out
```

```python
import contextlib
import numpy as np
import concourse.bass as bass
import concourse.mybir as mybir
from concourse.bass_utils import run_bass_kernel_spmd

F32 = mybir.dt.float32
BF16 = mybir.dt.bfloat16
I32 = mybir.dt.int32
ALU = mybir.AluOpType
AF = mybir.ActivationFunctionType
AX = mybir.AxisListType

ENGS = ["tensor", "vector", "scalar", "gpsimd", "sync"]

T = 4096
D = 2048
KC = 16
NH = 8
NE = 32
CAP = 384
CS = CAP + 1
NRT = 97
TW = 128
EPS = 1e-6
QSCALE = 128 ** -0.5
C_ID, C_ONE, C_TRI, C_US, C_IOTA, C_ES, C_TOK = 0, 128, 256, 384, 512, 513, 545
NCF = 577


class Buf:
    __slots__ = ("name", "w", "r", "sem", "cnt", "multi")

    def __init__(self, name, multi=False):
        self.name = name
        self.w = {}
        self.r = {}
        self.sem = None
        self.cnt = 0
        self.multi = multi


class Op:
    __slots__ = ("eng", "fn", "deps", "dma", "signal", "idx", "seq", "phase")


class Prog:
    def __init__(self, nc, stack):
        self.nc = nc
        self.stack = stack
        self.ops = {e: [] for e in ENGS}
        self.esem = {e: stack.enter_context(nc.semaphore("es_" + e)) for e in ENGS}
        self.ecnt = {e: 0 for e in ENGS}
        self.nsem = len(ENGS)
        self.phase = 0
        self.seq = 0
        self.dmabufs = []
        self.stats = []

    def _mk(self, eng, fn, deps, dma):
        o = Op()
        o.eng = eng
        o.fn = fn
        o.dma = dma
        o.signal = False
        o.idx = 0
        o.deps = deps
        o.phase = self.phase
        self.seq += 1
        o.seq = self.seq
        if dma is not None:
            if dma.sem is None:
                dma.sem = self.stack.enter_context(self.nc.semaphore("ds%d" % self.nsem))
                self.nsem += 1
                self.dmabufs.append(dma)
            dma.cnt += 16
        self.ops[eng].append(o)
        return o

    def op(self, eng, fn, reads=(), writes=(), dma=None):
        deps = {}

        def add(d):
            if d.phase < self.phase:
                return
            if d.dma is not None:
                deps[id(d.dma)] = ("d", d.dma, d.dma.cnt)
            else:
                if d.eng == "tensor" and eng == "tensor" and dma is None:
                    return
                prev = deps.get(d.eng)
                if prev is None or prev[1].seq < d.seq:
                    deps[d.eng] = ("e", d, 0)

        for b in reads:
            for d in b.w.values():
                add(d)
        for b in writes:
            if not b.multi:
                for d in b.w.values():
                    add(d)
            for d in b.r.values():
                add(d)
        o = self._mk(eng, fn, list(deps.values()), dma)
        key = id(dma) if dma is not None else eng
        for b in reads:
            b.r[key] = o
        for b in writes:
            if b.multi:
                b.w[key] = o
            else:
                b.w = {key: o}
                b.r = {}
        return o

    def barrier(self):
        a = {}
        for e in ENGS:
            a[e] = self._mk(e, (lambda eng: eng.drain()), [], None)
        for e in ENGS:
            deps = [("e", a[e2], 0) for e2 in ENGS if e2 != e]
            deps += [("d", b, b.cnt) for b in self.dmabufs if b.cnt > 0]
            self._mk(e, (lambda eng: eng.nop()), deps, None)

    def emit(self):
        nc = self.nc
        for e in ENGS:
            for o in self.ops[e]:
                for dep in o.deps:
                    if dep[0] == "e":
                        dep[1].signal = True
        for e in ENGS:
            for o in self.ops[e]:
                if o.signal and o.dma is None:
                    self.ecnt[e] += 1
                    o.idx = self.ecnt[e]
        stats = {}
        with nc.Block() as block:
            def run(engname, eng):
                known = {}
                nw = 0
                for o in self.ops[engname]:
                    for dep in o.deps:
                        if dep[0] == "d":
                            sem, val = dep[1].sem, dep[2]
                        else:
                            sem, val = self.esem[dep[1].eng], dep[1].idx
                        if known.get(id(sem), 0) >= val:
                            continue
                        eng.wait_ge(sem, val)
                        nw += 1
                        known[id(sem)] = val
                    if o.fn is None:
                        continue
                    ins = o.fn(eng)
                    if o.dma is not None:
                        ins.then_inc(o.dma.sem, 16)
                    elif o.signal:
                        ins.then_inc(self.esem[engname], 1)
                stats[engname] = (len(self.ops[engname]), nw)

            @block.tensor
            def _(eng):
                run("tensor", eng)

            @block.vector
            def _(eng):
                run("vector", eng)

            @block.scalar
            def _(eng):
                run("scalar", eng)

            @block.gpsimd
            def _(eng):
                run("gpsimd", eng)

            @block.sync
            def _(eng):
                run("sync", eng)
        self.stats.append(stats)
        self.ops = {e: [] for e in ENGS}
        self.phase += 1

    def mm(self, out, lhsT, rhs, start, stop, R, W, **kw):
        return self.op("tensor", lambda e: e.matmul(out, lhsT=lhsT, rhs=rhs, start=start, stop=stop, **kw), R, W)

    def tr(self, out, in_, ident, R, W):
        return self.op("tensor", lambda e: e.transpose(out, in_, ident), R, W)

    def act(self, out, in_, func, R, W, **kw):
        return self.op("scalar", lambda e: e.activation(out=out, in_=in_, func=func, **kw), R, W)

    def tt(self, eng, out, in0, in1, op, R, W):
        return self.op(eng, lambda e: e.tensor_tensor(out=out, in0=in0, in1=in1, op=op), R, W)

    def ts(self, eng, out, in0, s1, s2, op0, op1, R, W):
        if s2 is None:
            return self.op(eng, lambda e: e.tensor_scalar(out=out, in0=in0, scalar1=s1, scalar2=None, op0=op0), R, W)
        return self.op(eng, lambda e: e.tensor_scalar(out=out, in0=in0, scalar1=s1, scalar2=s2, op0=op0, op1=op1), R, W)

    def stt(self, out, in0, scalar, in1, op0, op1, R, W):
        return self.op("vector", lambda e: e.scalar_tensor_tensor(out=out, in0=in0, scalar=scalar, in1=in1, op0=op0, op1=op1), R, W)

    def cp(self, eng, out, in_, R, W):
        if eng == "scalar":
            return self.op(eng, lambda e: e.copy(out=out, in_=in_), R, W)
        return self.op(eng, lambda e: e.tensor_copy(out=out, in_=in_), R, W)

    def dma(self, eng, out, in_, R, W, sem):
        return self.op(eng, lambda e: e.dma_start(out=out, in_=in_), R, W, dma=sem)


def build(debug=False, stage=99):
    nc = bass.Bass("TRN2", target_bir_lowering=False)

    def din(name, shape):
        return nc.dram_tensor(name, shape, F32, kind="ExternalInput").ap()

    skind = "ExternalOutput" if debug else "Internal"

    def dsc(name, shape, dt):
        return nc.dram_tensor(name, shape, dt, kind=skind).ap()

    xT = din("xT", [D, T])
    pT = din("pT", [256, T])
    w_in = din("w_in", [D, 10248])
    gvec = din("gvec", [128, 64])
    b_f = din("b_f", [8, 1])
    conv_w = din("conv_w", [128, 24])
    w_a = din("w_a", [1024, D])
    w_b = din("w_b", [1024, D])
    w_o = din("w_o", [D, D])
    w_r = din("w_r", [D, 36])
    b_r = din("b_r", [128, 36])
    w_gu = din("w_gu", [NE, D, 1024])
    w_dn = din("w_dn", [NE, 512, D])
    w_pg = din("w_pg", [D, D])
    w_pe = din("w_pe", [256, D])
    cfd = din("cf", [128, NCF])
    sel8d = din("sel8", [8, 1032])
    tabinit = din("tabinit", [128, TW])
    yT = nc.dram_tensor("yT", [D, T], F32, kind="ExternalOutput").ap()

    qT_d = dsc("qT_d", [1024, T], BF16)
    kT_d = dsc("kT_d", [1024, T], BF16)
    v_d = dsc("v_d", [T, 1024], BF16)
    bmT_d = dsc("bmT_d", [1024, T], BF16)
    gaT_d = dsc("gaT_d", [D, T], BF16)
    gbT_d = dsc("gbT_d", [D, T], BF16)
    negc_d = dsc("negc_d", [8, T], F32)
    attnT_d = dsc("attnT_d", [1024, T], BF16)
    mgT_d = dsc("mgT_d", [D, T], BF16)
    x1T_d = dsc("x1T_d", [D, T], F32)
    hn_d = dsc("hn_d", [T, D], BF16)
    tab_d = dsc("tab_d", [NRT * 128, TW], F32)
    ybuf_d = dsc("ybuf_d", [2 * T + 128, D], F32)
    dbg_x2 = dsc("dbg_x2", [D, T], F32) if debug else None

    def chunked(ap):
        return ap.rearrange("(c p) n -> p c n", p=128)

    with contextlib.ExitStack() as glob:
        P = Prog(nc, glob)

        sbn = [0]

        def sb(st, name, shape, dt):
            sbn[0] += 1
            return st.enter_context(nc.sbuf_tensor("s%d_%s" % (sbn[0], name), shape, dt))

        cf = sb(glob, "cf", [128, NCF], F32)
        cb = sb(glob, "cb", [128, 512], BF16)
        gv = sb(glob, "gv", [128, 64], F32)
        sel8 = sb(glob, "sel8", [8, 1032], F32)
        Bc = Buf("consts")
        ps = [glob.enter_context(nc.psum_tensor("ps%d" % i, [128, 512], F32)) for i in range(6)]
        Bps = [Buf("ps%d" % i) for i in range(6)]
        pt = [glob.enter_context(nc.psum_tensor("pt%d" % i, [128, 1024], BF16)) for i in range(2)]
        Bpt = [Buf("pt%d" % i) for i in range(2)]
        Bd = Buf("dram", multi=True)

        P.dma("sync", cf[:], cfd, [], [Bc], Bc)
        P.dma("sync", gv[:], gvec, [], [Bc], Bc)
        P.dma("sync", sel8[:], sel8d, [], [Bc], Bc)
        P.dma("gpsimd", cb[:], cfd[:, 0:512], [], [Bc], Bc)
        identb = cb[:, C_ID:C_ID + 128]
        onesb = cb[:, C_ONE:C_ONE + 128]
        trib = cb[:, C_TRI:C_TRI + 128]
        usb = cb[:, C_US:C_US + 128]
        identf = cf[:, C_ID:C_ID + 128]
        G_MIX, G_FFN, G_PLE, G_FIN = 0, 16, 32, 48

        evac_ctr = [0]

        def evac(out, in_, R, W):
            evac_ctr[0] += 1
            if evac_ctr[0] % 2:
                return P.cp("scalar", out, in_, R, W)
            return P.cp("vector", out, in_, R, W)

        def rms_stats(xblk, Bx, n, sq, Bsq, psn, Bpsn, srt, Bsrt, rstd, Brstd):
            P.act(sq[:, :, 0:n], xblk[:, :, 0:n], AF.Square, [Bx], [Bsq])
            for c in range(KC):
                P.mm(psn[:, 0:n], onesb, sq[:, c, 0:n], c == 0, c == KC - 1, [Bsq, Bc], [Bpsn])
            P.act(srt[:, 0:n], psn[:, 0:n], AF.Sqrt, [Bpsn], [Bsrt], bias=EPS, scale=1.0 / D)
            P.op("vector", lambda e: e.reciprocal(out=rstd[:, 0:n], in_=srt[:, 0:n]), [Bsrt], [Brstd])

        with contextlib.ExitStack() as sA:
            hT = sb(sA, "hT", [128, KC, T], BF16)
            BhT = [Buf("hT%d" % i) for i in range(16)]
            with contextlib.ExitStack() as s0:
                xb = [sb(s0, "xb%d" % i, [128, KC, 256], F32) for i in range(2)]
                Bxb = [Buf("xb%d" % i) for i in range(2)]
                sq = sb(s0, "sq", [128, KC, 256], BF16)
                Bsq = Buf("sq")
                srt = sb(s0, "srt", [128, 256], F32)
                Bsrt = Buf("srt")
                rstd = sb(s0, "rstd", [128, 256], F32)
                Brstd = Buf("rstd")
                xTv = chunked(xT)
                for tb in range(16):
                    b = tb % 2
                    P.dma("sync", xb[b][:], xTv[:, :, tb * 256:(tb + 1) * 256], [], [Bxb[b]], Bxb[b])
                    rms_stats(xb[b], Bxb[b], 256, sq, Bsq, ps[0], Bps[0], srt, Bsrt, rstd, Brstd)
                    for c in range(KC):
                        P.stt(hT[:, c, tb * 256:(tb + 1) * 256], xb[b][:, c, :], gv[:, G_MIX + c:G_MIX + c + 1],
                              rstd[:], ALU.mult, ALU.mult, [Bxb[b], Brstd, Bc], [BhT[tb]])
                P.barrier()
                P.emit()
            if stage >= 1:
              with contextlib.ExitStack() as s1:
                wsl = [sb(s1, "wsl%d" % i, [128, KC, 512], BF16) for i in range(2)]
                Bw = [Buf("wsl%d" % i) for i in range(2)]
                stg = [sb(s1, "stg%d" % i, [128, 4, 512], BF16) for i in range(2)]
                Bstg = [Buf("stg%d" % i) for i in range(2)]
                usb_ = sb(s1, "u_sb", [128, 512], F32)
                Bus = Buf("u_sb")
                zb = sb(s1, "zb", [128, 514], F32)
                Bz = Buf("zb")
                acc = sb(s1, "cacc", [128, 512], F32)
                Bacc = Buf("cacc")
                cw = sb(s1, "cw", [128, 24], F32)
                negb = sb(s1, "negb", [8, 1], F32)
                lsp = sb(s1, "lsp", [8, 512], F32)
                Blsp = Buf("lsp")
                one8 = sb(s1, "one8", [8, 512], F32)
                ncs = sb(s1, "ncs", [8, T], F32)
                Bncs = Buf("ncs")
                Bcw = Buf("cw")
                P.dma("sync", cw[:], conv_w, [], [Bcw], Bcw)
                P.dma("sync", negb[:], b_f, [], [Bcw], Bcw)
                P.ts("vector", negb[:], negb[:], -1.0, None, ALU.mult, None, [Bcw], [Bcw])
                P.op("vector", lambda e: e.memset(one8[:], 1.0), [], [Bcw])
                w_in_v = chunked(w_in)
                allh = list(BhT)
                slab_i = [0]
                psr = [0]

                def next_ps():
                    psr[0] = (psr[0] + 1) % 6
                    return psr[0]

                def load_slab(cols):
                    i = slab_i[0] % 2
                    slab_i[0] += 1
                    o = 0
                    for (c0, n) in cols:
                        P.dma("gpsimd", wsl[i][:, :, o:o + n], w_in_v[:, :, c0:c0 + n], [], [Bw[i]], Bw[i])
                        o += n
                    return i

                stg_i = [0]

                def fm_plain(col0, dest, row0, sigmoid):
                    i = load_slab([(col0, 512)])
                    for tb in range(8):
                        si = stg_i[0] % 2
                        stg_i[0] += 1
                        for m in range(4):
                            pi = next_ps()
                            for c in range(KC):
                                P.mm(ps[pi][:], wsl[i][:, c, m * 128:(m + 1) * 128], hT[:, c, tb * 512:(tb + 1) * 512],
                                     c == 0, c == KC - 1, [Bw[i], BhT[2 * tb], BhT[2 * tb + 1]], [Bps[pi]])
                            if sigmoid:
                                P.act(stg[si][:, m, :], ps[pi][:], AF.Sigmoid, [Bps[pi]], [Bstg[si]])
                            else:
                                evac(stg[si][:, m, :], ps[pi][:], [Bps[pi]], [Bstg[si]])
                        P.dma("sync", dest[row0:row0 + 512, tb * 512:(tb + 1) * 512].rearrange("(m p) t -> p m t", p=128),
                              stg[si][:], [Bstg[si]], [Bd], Bstg[si])

                for s in range(2):
                    fm_plain(s * 512, qT_d, s * 512, False)
                for s in range(2):
                    fm_plain(1024 + s * 512, kT_d, s * 512, False)
                for s in range(2):
                    i = load_slab([(2048 + s * 512, 512)])
                    for t4 in range(8):
                        si = stg_i[0] % 2
                        stg_i[0] += 1
                        for j in range(4):
                            tt_ = t4 * 4 + j
                            pi = next_ps()
                            for c in range(KC):
                                P.mm(ps[pi][:], hT[:, c, tt_ * 128:(tt_ + 1) * 128], wsl[i][:, c, :],
                                     c == 0, c == KC - 1, [Bw[i], BhT[tt_ // 2]], [Bps[pi]])
                            evac(stg[si][:, j, :], ps[pi][:], [Bps[pi]], [Bstg[si]])
                        P.dma("sync", v_d[t4 * 512:(t4 + 1) * 512, s * 512:(s + 1) * 512].rearrange("(j p) n -> p j n", p=128),
                              stg[si][:], [Bstg[si]], [Bd], Bstg[si])
                i = load_slab([(3072, 8)])
                for tb in range(8):
                    pi = next_ps()
                    for c in range(KC):
                        P.mm(ps[pi][0:8, :], wsl[i][:, c, 0:8], hT[:, c, tb * 512:(tb + 1) * 512],
                             c == 0, c == KC - 1, [Bw[i], BhT[2 * tb], BhT[2 * tb + 1]], [Bps[pi]])
                    P.act(lsp[:], ps[pi][0:8, :], AF.Exp, [Bps[pi], Bcw], [Blsp], bias=negb[:, 0:1], scale=-1.0)
                    P.act(lsp[:], lsp[:], AF.Ln, [Blsp], [Blsp], bias=1.0, scale=1.0)
                    init = 0.0 if tb == 0 else ncs[:, tb * 512 - 1:tb * 512]
                    P.op("vector", lambda e, tb=tb, init=init: e.tensor_tensor_scan(
                        out=ncs[:, tb * 512:(tb + 1) * 512], data0=one8[:], data1=lsp[:], initial=init,
                        op0=ALU.mult, op1=ALU.add), [Blsp, Bcw, Bncs], [Bncs])
                P.dma("sync", negc_d, ncs[:], [Bncs], [Bd], Bncs)
                for j in range(8):
                    i = load_slab([(3080 + j * 128, 128), (4104 + j * 128, 128), (5128 + j * 128, 128)])
                    P.op("vector", lambda e: e.memset(zb[:, 0:2], 0.0), [], [Bz])
                    for tb in range(8):
                        if tb % 4 == 0:
                            si = stg_i[0] % 2
                            stg_i[0] += 1
                        pis = []
                        for m in range(3):
                            pi = next_ps()
                            pis.append(pi)
                            for c in range(KC):
                                P.mm(ps[pi][:], wsl[i][:, c, m * 128:(m + 1) * 128], hT[:, c, tb * 512:(tb + 1) * 512],
                                     c == 0, c == KC - 1, [Bw[i], BhT[2 * tb], BhT[2 * tb + 1]], [Bps[pi]])
                        pu, pbg, pcg = pis
                        P.cp("scalar", usb_[:], ps[pu][:], [Bps[pu]], [Bus])
                        P.tt("vector", zb[:, 2:514], ps[pcg][:], usb_[:], ALU.mult, [Bps[pcg], Bus], [Bz])
                        P.ts("vector", acc[:], zb[:, 2:514], cw[:, j * 3 + 2:j * 3 + 3], None, ALU.mult, None, [Bz, Bcw], [Bacc])
                        P.stt(acc[:], zb[:, 1:513], cw[:, j * 3 + 1:j * 3 + 2], acc[:], ALU.mult, ALU.add, [Bz, Bcw, Bacc], [Bacc])
                        P.stt(acc[:], zb[:, 0:512], cw[:, j * 3 + 0:j * 3 + 1], acc[:], ALU.mult, ALU.add, [Bz, Bcw, Bacc], [Bacc])
                        P.tt("vector", stg[si][:, tb % 4, :], ps[pbg][:], acc[:], ALU.mult, [Bps[pbg], Bacc], [Bstg[si]])
                        P.cp("vector", zb[:, 0:2], zb[:, 512:514], [Bz], [Bz])
                        if tb % 4 == 3:
                            t0 = (tb - 3) * 512
                            P.dma("sync", bmT_d[j * 128:(j + 1) * 128, t0:t0 + 2048].rearrange("p (m t) -> p m t", m=4),
                                  stg[si][:], [Bstg[si]], [Bd], Bstg[si])
                for s in range(4):
                    fm_plain(6152 + s * 512, gaT_d, s * 512, True)
                for s in range(4):
                    fm_plain(6152 + 2048 + s * 512, gbT_d, s * 512, True)
                P.barrier()
                P.emit()

        sWab = contextlib.ExitStack()
        if stage >= 3:
            wa = sb(sWab, "wa", [128, 8, D], BF16)
            wb = sb(sWab, "wb", [128, 8, D], BF16)
            Bwab = Buf("wab")
            P.dma("gpsimd", wa[:], chunked(w_a), [], [Bwab], Bwab)
            P.dma("gpsimd", wb[:], chunked(w_b), [], [Bwab], Bwab)
        if stage >= 2:
          with contextlib.ExitStack() as sB:
            qh = [sb(sB, "qh%d" % i, [128, T], BF16) for i in range(2)]
            kh = [sb(sB, "kh%d" % i, [128, T], BF16) for i in range(2)]
            vh = [sb(sB, "vh%d" % i, [128, 32, 129], BF16) for i in range(2)]
            ncq = [sb(sB, "ncq%d" % i, [128, T], F32) for i in range(2)]
            Bq = [Buf("qh%d" % i) for i in range(2)]
            Bk = [Buf("kh%d" % i) for i in range(2)]
            Bv = [Buf("vh%d" % i) for i in range(2)]
            Bncq = [Buf("ncq%d" % i) for i in range(2)]
            ncs2 = sb(sB, "ncs2", [8, T], F32)
            Bn2 = Buf("ncs2")
            nck = sb(sB, "nck", [128, 32, 8], F32)
            Bnck = Buf("nck")
            rden = sb(sB, "rden", [128, 4], F32)
            Brd = Buf("rden")
            atok = [sb(sB, "atok%d" % i, [128, 4, 128], BF16) for i in range(2)]
            Bat = [Buf("atok%d" % i) for i in range(2)]
            ast = [sb(sB, "ast%d" % i, [128, 512], BF16) for i in range(2)]
            Bast = [Buf("ast%d" % i) for i in range(2)]
            P.dma("sync", ncs2[:], negc_d, [], [Bn2], Bn2)
            for j in range(32):
                P.mm(ps[5][:, j * 8:(j + 1) * 8], ncs2[:, j * 128:(j + 1) * 128], sel8[:, 1024:1032], True, True, [Bn2, Bc], [Bps[5]])
            P.cp("vector", nck[:].rearrange("p j h -> p (j h)"), ps[5][:, 0:256], [Bps[5]], [Bnck])
            for i in range(2):
                P.op("gpsimd", lambda e, i=i: e.memset(vh[i][:, :, 128:129], 1.0), [], [Bv[i]])
            pend = []
            sidx = [0]
            blk = [0]
            ACCB = [2, 3, 4, 5]
            rden = sb(sB, "rden2", [128, 2, 4], F32)

            def loads(h):
                hb = h % 2
                P.dma("sync", qh[hb][:], qT_d[h * 128:(h + 1) * 128, :], [], [Bq[hb]], Bq[hb])
                P.dma("sync", kh[hb][:], kT_d[h * 128:(h + 1) * 128, :], [], [Bk[hb]], Bk[hb])
                P.dma("sync", vh[hb][:, :, 0:128], v_d[:, h * 128:(h + 1) * 128].rearrange("(j p) d -> p j d", p=128),
                      [], [Bv[hb]], Bv[hb])
                P.dma("sync", ncq[hb][:], negc_d[h, :].partition_broadcast(128), [], [Bncq[hb]], Bncq[hb])

            loads(0)
            NT_ = 5
            SB_ = [ps[0], ps[1], pt[1][:].bitcast(F32)]
            BSB_ = [Bps[0], Bps[1], Bpt[1]]
            tS = [sb(sB, "tSx%d" % i, [128, 512], F32) for i in range(NT_)]
            BtS = [Buf("tSx%d" % i) for i in range(NT_)]
            pTt = [sb(sB, "pTx%d" % i, [128, 512], BF16) for i in range(NT_)]
            BpT = [Buf("pTx%d" % i) for i in range(NT_)]
            LOOK = 3
            tiles = []
            for h in range(NH):
                for qb in range(8):
                    for kt in range(4 * (qb + 1)):
                        tiles.append((h, qb, kt))

            def front(n):
                h, qb, kt = tiles[n]
                hb = h % 2
                dj = kt - 4 * qb
                qlo = max(dj, 0) * 128
                si = n % 3
                ti = n % NT_
                q0 = qb * 512 + qlo
                q1 = (qb + 1) * 512
                P.mm(SB_[si][:, qlo:512], kh[hb][:, kt * 128:(kt + 1) * 128], qh[hb][:, q0:q1], True, True,
                     [Bk[hb], Bq[hb]], [BSB_[si]])
                P.stt(tS[ti][:, qlo:512], SB_[si][:, qlo:512], QSCALE, ncq[hb][:, q0:q1], ALU.mult, ALU.subtract,
                      [BSB_[si], Bncq[hb]], [BtS[ti]])
                P.act(pTt[ti][:, qlo:512], tS[ti][:, qlo:512], AF.Exp, [BtS[ti], Bnck], [BpT[ti]],
                      bias=nck[:, kt, h:h + 1], scale=1.0)
                if dj >= 0:
                    P.tt("gpsimd", pTt[ti][:, qlo:qlo + 128], pTt[ti][:, qlo:qlo + 128], trib, ALU.mult,
                         [BpT[ti], Bc], [BpT[ti]])

            def back(n):
                nonlocal pend
                h, qb, kt = tiles[n]
                hb = h % 2
                if qb == 0 and kt == 0 and h + 1 < NH:
                    loads(h + 1)
                dj = kt - 4 * qb
                ti = n % NT_
                for qs in range(4):
                    if qs < dj:
                        continue
                    a = ACCB[qs]
                    P.mm(ps[a][:, 0:129], pTt[ti][:, qs * 128:(qs + 1) * 128], vh[hb][:, kt, :],
                         kt == 0, kt == 4 * qb + qs, [BpT[ti], Bv[hb]], [Bps[a]])
                if kt == 1 and pend:
                    for f in pend:
                        f()
                    pend = []
                if kt == 4 * (qb + 1) - 1:
                    ab = blk[0] % 2
                    blk[0] += 1
                    for qs in range(4):
                        a = ACCB[qs]
                        P.op("vector", lambda e, a=a, qs=qs, ab=ab: e.reciprocal(out=rden[:, ab, qs:qs + 1], in_=ps[a][:, 128:129]),
                             [Bps[a]], [Brd])
                        P.act(atok[ab][:, qs, :], ps[a][:, 0:128], AF.Copy, [Bps[a], Brd], [Bat[ab]], scale=rden[:, ab, qs:qs + 1])

                    def fin(h=h, qb=qb, ab=ab):
                        for qs in range(4):
                            P.tr(pt[0][:, qs * 128:(qs + 1) * 128], atok[ab][:, qs, :], identb, [Bat[ab], Bc], [Bpt[0]])
                        P.cp("vector", ast[ab][:], pt[0][:, 0:512], [Bpt[0]], [Bast[ab]])
                        P.dma("sync", attnT_d[h * 128:(h + 1) * 128, qb * 512:(qb + 1) * 512], ast[ab][:], [Bast[ab]], [Bd], Bast[ab])
                    pend.append(fin)

            for n in range(len(tiles) + LOOK):
                if n < len(tiles):
                    front(n)
                if n - LOOK >= 0:
                    back(n - LOOK)
            for f in pend:
                f()
            P.barrier()
            P.emit()

        if stage >= 3:
          with contextlib.ExitStack() as sC:
            at = [sb(sC, "at%d" % i, [128, 8, 512], BF16) for i in range(2)]
            bm = [sb(sC, "bm%d" % i, [128, 8, 512], BF16) for i in range(2)]
            ga = [sb(sC, "ga%d" % i, [128, KC, 512], BF16) for i in range(2)]
            gb = [sb(sC, "gb%d" % i, [128, KC, 512], BF16) for i in range(2)]
            Bin = [Buf("c1in%d" % i) for i in range(2)]
            t1 = [sb(sC, "t1_%d" % i, [128, 512], F32) for i in range(2)]
            t2 = [sb(sC, "t2_%d" % i, [128, 512], F32) for i in range(2)]
            Bt1 = [Buf("t1_%d" % i) for i in range(2)]
            Bt2 = [Buf("t2_%d" % i) for i in range(2)]
            mst = [sb(sC, "mst%d" % i, [128, 4, 512], BF16) for i in range(2)]
            Bmst = [Buf("mst%d" % i) for i in range(2)]

            def c1_loads(tb):
                b = tb % 2
                sl = slice(tb * 512, (tb + 1) * 512)
                P.dma("sync", at[b][:], chunked(attnT_d)[:, :, sl], [], [Bin[b]], Bin[b])
                P.dma("sync", bm[b][:], chunked(bmT_d)[:, :, sl], [], [Bin[b]], Bin[b])
                P.dma("sync", ga[b][:], chunked(gaT_d)[:, :, sl], [], [Bin[b]], Bin[b])
                P.dma("sync", gb[b][:], chunked(gbT_d)[:, :, sl], [], [Bin[b]], Bin[b])

            c1_loads(0)
            k = 0
            for tb in range(8):
                b = tb % 2
                if tb + 1 < 8:
                    c1_loads(tb + 1)
                for fg in range(KC):
                    pa = (2 * k) % 6
                    pb = (2 * k + 1) % 6
                    tb_ = k % 2
                    k += 1
                    if fg % 4 == 0:
                        mi = (tb * 4 + fg // 4) % 2
                    for c in range(8):
                        P.mm(ps[pa][:], wa[:, c, fg * 128:(fg + 1) * 128], at[b][:, c, :], c == 0, c == 7, [Bwab, Bin[b]], [Bps[pa]])
                    for c in range(8):
                        P.mm(ps[pb][:], wb[:, c, fg * 128:(fg + 1) * 128], bm[b][:, c, :], c == 0, c == 7, [Bwab, Bin[b]], [Bps[pb]])
                    P.tt("vector", t1[tb_][:], ps[pa][:], ga[b][:, fg, :], ALU.mult, [Bps[pa], Bin[b]], [Bt1[tb_]])
                    P.tt("vector", t2[tb_][:], ps[pb][:], gb[b][:, fg, :], ALU.mult, [Bps[pb], Bin[b]], [Bt2[tb_]])
                    P.tt("gpsimd", mst[mi][:, fg % 4, :], t1[tb_][:], t2[tb_][:], ALU.add, [Bt1[tb_], Bt2[tb_]], [Bmst[mi]])
                    if fg % 4 == 3:
                        r0 = (fg - 3) * 128
                        P.dma("sync", mgT_d[r0:r0 + 512, tb * 512:(tb + 1) * 512].rearrange("(m p) t -> p m t", p=128),
                              mst[mi][:], [Bmst[mi]], [Bd], Bmst[mi])
            P.barrier()
            P.emit()

        sWab.close()
        if stage >= 4:
          LGraw = sb(glob, "LGraw", [128, 32, 36], F32)
          SSq = sb(glob, "SSq", [128, 32], F32)
          BLG = Buf("lgraw", multi=True)
          with contextlib.ExitStack() as sC:
            wo = sb(sC, "wo", [128, KC, D], BF16)
            Bwo = Buf("wo")
            P.dma("gpsimd", wo[:, 0:8, :], chunked(w_o)[:, 0:8, :], [], [Bwo], Bwo)
            P.dma("gpsimd", wo[:, 8:16, :], chunked(w_o)[:, 8:16, :], [], [Bwo], Bwo)
            mg = [sb(sC, "mg%d" % i, [128, KC, 256], BF16) for i in range(3)]
            xb = [sb(sC, "xb%d" % i, [128, KC, 256], F32) for i in range(3)]
            Bmg = [Buf("mg%d" % i) for i in range(3)]
            Bxb = [Buf("xb%d" % i) for i in range(3)]
            sq = sb(sC, "sq", [128, KC, 256], BF16)
            Bsq = Buf("sq")
            srt = sb(sC, "srt", [128, 256], F32)
            Bsrt = Buf("srt")
            rstd = sb(sC, "rstd", [128, 256], F32)
            Brstd = Buf("rstd")
            hnT = sb(sC, "hnT", [128, KC, 256], BF16)
            BhnT = Buf("hnT")
            hnt = [sb(sC, "hnt%d" % i, [128, D], BF16) for i in range(2)]
            Bhnt = [Buf("hnt%d" % i) for i in range(2)]
            wr = sb(sC, "wr", [128, KC, 36], F32)
            brb = sb(sC, "brb", [128, 36], F32)
            Bwr = Buf("wr")
            P.dma("sync", wr[:], chunked(w_r), [], [Bwr], Bwr)
            P.dma("sync", brb[:], b_r, [], [Bwr], Bwr)
            for c in range(KC):
                P.ts("vector", wr[:, c, :], wr[:, c, :], gv[:, G_FFN + c:G_FFN + c + 1], None, ALU.mult, None, [Bwr, Bc], [Bwr])
            tin = sb(sC, "tin", [128, TW], F32)
            Btin = Buf("tin")
            P.dma("sync", tin[:], tabinit, [], [Btin], Btin)
            Btab = Buf("tab", multi=True)
            tabv = tab_d.rearrange("(j p) w -> p j w", p=128)
            for j in range(NRT):
                P.dma("sync", tabv[:, j, :], tin[:], [Btin], [Btab], Btin)
            def c2_loads(tb):
                b = tb % 3
                sl = slice(tb * 256, (tb + 1) * 256)
                P.dma("sync", mg[b][:], chunked(mgT_d)[:, :, sl], [], [Bmg[b]], Bmg[b])
                P.dma("sync", xb[b][:], chunked(xT)[:, :, sl], [], [Bxb[b]], Bxb[b])

            def route1(tile_i, b, tsl):
                for c in range(KC):
                    P.mm(ps[4][:, 0:36], xb[b][:, c, tsl], wr[:, c, :], c == 0, c == KC - 1, [Bxb[b], Bwr], [Bps[4]])
                for c in range(KC):
                    P.mm(ps[5][:, 0:1], sq[:, c, tsl], onesb[:, 0:1], c == 0, c == KC - 1, [Bsq, Bc], [Bps[5]])
                P.cp("scalar", LGraw[:, tile_i, :], ps[4][:, 0:36], [Bps[4]], [BLG])
                P.cp("scalar", SSq[:, tile_i:tile_i + 1], ps[5][:, 0:1], [Bps[5]], [BLG])

            def x_steps(tb):
                b = tb % 3
                steps = []
                for fg in range(KC):
                    def st(fg=fg):
                        pi = fg % 2
                        for c in range(KC):
                            P.mm(ps[pi][:, 0:256], wo[:, c, fg * 128:(fg + 1) * 128], mg[b][:, c, :], c == 0, c == KC - 1,
                                 [Bwo, Bmg[b]], [Bps[pi]])
                        P.tt("vector", xb[b][:, fg, :], ps[pi][:, 0:256], xb[b][:, fg, :], ALU.add, [Bps[pi], Bxb[b]], [Bxb[b]])
                    steps.append(st)
                return steps

            def x_fin(tb):
                b = tb % 3
                P.dma("sync", chunked(x1T_d)[:, :, tb * 256:(tb + 1) * 256], xb[b][:], [Bxb[b]], [Bd], Bxb[b])

            def y_steps(tb):
                b = tb % 3

                def y0_():
                    rms_stats(xb[b], Bxb[b], 256, sq, Bsq, ps[3], Bps[3], srt, Bsrt, rstd, Brstd)

                def yh_(q4):
                    for c in range(4 * q4, 4 * q4 + 4):
                        P.stt(hnT[:, c, :], xb[b][:, c, :], gv[:, G_FFN + c:G_FFN + c + 1], rstd[:], ALU.mult, ALU.mult,
                              [Bxb[b], Brstd, Bc], [BhnT])

                def ytile(tt_):
                    tile_i = tb * 2 + tt_
                    hb_ = tile_i % 2
                    tsl = slice(tt_ * 128, (tt_ + 1) * 128)
                    for c4 in range(4):
                        pti = c4 % 2
                        for cc in range(4):
                            c = c4 * 4 + cc
                            P.tr(pt[pti][:, cc * 128:(cc + 1) * 128], hnT[:, c, tsl], identb, [BhnT, Bc], [Bpt[pti]])
                        evac(hnt[hb_][:, c4 * 512:(c4 + 1) * 512], pt[pti][:, 0:512], [Bpt[pti]], [Bhnt[hb_]])
                    P.dma("sync", hn_d[tile_i * 128:(tile_i + 1) * 128, :], hnt[hb_][:], [Bhnt[hb_]], [Bd], Bhnt[hb_])
                    route1(tile_i, b, tsl)
                return [y0_, (lambda: ytile(0)), (lambda: ytile(1)), yh_]

            pend2 = []
            c2_loads(0)
            for tb in range(17):
                if tb + 1 < 16:
                    c2_loads(tb + 1)
                X = x_steps(tb) if tb < 16 else []
                Y = y_steps(tb - 1) if tb >= 1 else []
                for fg in range(KC):
                    if X:
                        X[fg]()
                    if fg == 1 and Y:
                        Y[0]()
                    if 2 <= fg <= 5 and Y:
                        Y[3](fg - 2)
                    if fg == 6 and Y:
                        Y[1]()
                    if fg == 11 and Y:
                        Y[2]()
                if tb < 16:
                    x_fin(tb)
            P.barrier()
            P.emit()

          sWe = contextlib.ExitStack()
          wgu = [sb(sWe, "wgu%d" % i, [128, KC, 1024], BF16) for i in range(2)]
          wdn = [sb(sWe, "wdn%d" % i, [128, 4, D], BF16) for i in range(2)]
          Bwe = [Buf("we%d" % i) for i in range(2)]

          def e_wloads(e):
              b = e % 2
              P.dma("gpsimd", wgu[b][:, 0:8, :], chunked(w_gu[e])[:, 0:8, :], [], [Bwe[b]], Bwe[b])
              P.dma("gpsimd", wgu[b][:, 8:16, :], chunked(w_gu[e])[:, 8:16, :], [], [Bwe[b]], Bwe[b])
              P.dma("gpsimd", wdn[b][:], chunked(w_dn[e]), [], [Bwe[b]], Bwe[b])

          if stage >= 5:
              e_wloads(0)
              e_wloads(1)
          with contextlib.ExitStack() as sR:
            def W_(name, k):
                return sb(sR, name, [128, 32, k], F32)
            LG = W_("LG", 36)
            OHG = W_("OHG", 4)
            EG = W_("EG", 4)
            ESEL = W_("ESEL", 8)
            T8 = W_("T8", 8)
            OH1 = W_("OH1", 8)
            E2 = W_("E2", 8)
            OH2 = W_("OH2", 8)
            A1 = W_("A1", 32)
            A2 = W_("A2", 32)
            POS = W_("POS", 32)
            TMP = W_("TMP", 32)
            CNT = W_("CNT", 32)
            CAR = W_("CAR", 32)
            AbA = sb(sR, "AbA", [128, 32, 32], BF16)
            S_ = sb(sR, "Ssc", [128, 16, 32], F32)
            DST = sb(sR, "DST", [128, 64], I32)
            PAY = sb(sR, "PAY", [128, 64, TW], F32)
            brb2 = sb(sR, "brb2", [128, 36], F32)
            BR = Buf("R")
            Bbr2 = Buf("brb2")
            BPAY = Buf("PAY")
            P.dma("sync", brb2[:], b_r, [], [Bbr2], Bbr2)
            P.op("gpsimd", lambda e: e.memset(PAY[:], 0.0), [], [BPAY])
            RW = ([BR], [BR])
            sc = lambda i_: S_[:, i_, :]
            bc2 = lambda ap2, k_: ap2.unsqueeze(2).to_broadcast([128, 32, k_])
            flat = lambda t3: t3[:].rearrange("p t e -> p (t e)")
            vop = lambda fn, R=RW[0], Wr=RW[1]: P.op("vector", fn, R, Wr)
            P.act(sc(0), SSq[:], AF.Sqrt, [BLG], [BR], bias=EPS, scale=1.0 / D)
            vop(lambda e: e.reciprocal(out=sc(0), in_=sc(0)))
            P.tt("vector", LG[:], LGraw[:], bc2(sc(0), 36), ALU.mult, [BLG, BR], [BR])
            P.tt("vector", LG[:], LG[:], brb2[:].unsqueeze(1).to_broadcast([128, 32, 36]), ALU.add, [BR, Bbr2], [BR])
            vop(lambda e: e.reduce_max(out=sc(1), in_=LG[:, :, 0:4], axis=AX.X))
            P.tt("vector", OHG[:], LG[:, :, 0:4], bc2(sc(1), 4), ALU.is_equal, *RW)
            P.tt("vector", EG[:], LG[:, :, 0:4], bc2(sc(1), 4), ALU.subtract, *RW)
            P.act(EG[:], EG[:], AF.Exp, *RW)
            vop(lambda e: e.reduce_sum(out=sc(2), in_=EG[:], axis=AX.X))
            vop(lambda e: e.reciprocal(out=sc(3), in_=sc(2)))
            P.tt("vector", ESEL[:], LG[:, :, 4:12], bc2(OHG[:, :, 0], 8), ALU.mult, *RW)
            for g in range(1, 4):
                P.tt("vector", T8[:], LG[:, :, 4 + 8 * g:12 + 8 * g], bc2(OHG[:, :, g], 8), ALU.mult, *RW)
                P.tt("vector", ESEL[:], ESEL[:], T8[:], ALU.add, *RW)
            vop(lambda e: e.reduce_max(out=sc(4), in_=ESEL[:], axis=AX.X))
            P.tt("vector", OH1[:], ESEL[:], bc2(sc(4), 8), ALU.is_equal, *RW)
            P.stt(flat(E2), flat(OH1), -1e30, flat(ESEL), ALU.mult, ALU.add, *RW)
            vop(lambda e: e.reduce_max(out=sc(5), in_=E2[:], axis=AX.X))
            P.tt("vector", OH2[:], E2[:], bc2(sc(5), 8), ALU.is_equal, *RW)
            P.tt("vector", sc(6), sc(5), sc(4), ALU.subtract, *RW)
            P.act(sc(6), sc(6), AF.Exp, *RW)
            P.ts("vector", sc(6), sc(6), 1.0, None, ALU.add, None, *RW)
            vop(lambda e: e.reciprocal(out=sc(6), in_=sc(6)))
            P.tt("vector", sc(7), sc(6), sc(3), ALU.mult, *RW)
            P.tt("vector", sc(8), sc(3), sc(7), ALU.subtract, *RW)
            for g in range(4):
                P.tt("vector", A1[:, :, 8 * g:8 * g + 8], OH1[:], bc2(OHG[:, :, g], 8), ALU.mult, *RW)
                P.tt("vector", A2[:, :, 8 * g:8 * g + 8], OH2[:], bc2(OHG[:, :, g], 8), ALU.mult, *RW)
            P.tt("vector", AbA[:], A1[:], A2[:], ALU.add, *RW)
            for h_ in range(2):
                P.mm(ps[h_][:], usb, flat(AbA)[:, 512 * h_:512 * h_ + 512], True, True, [BR, Bc], [Bps[h_]])
                P.mm(ps[2 + h_][:], onesb, flat(AbA)[:, 512 * h_:512 * h_ + 512], True, True, [BR, Bc], [Bps[2 + h_]])
            for h_ in range(2):
                P.cp("scalar", flat(CNT)[:, 512 * h_:512 * h_ + 512], ps[2 + h_][:], [Bps[2 + h_]], [BR])
            vop(lambda e: e.memset(CAR[:, 0, :], 0.0))
            for t_ in range(1, 32):
                P.tt("vector", CAR[:, t_, :], CAR[:, t_ - 1, :], CNT[:, t_ - 1, :], ALU.add, *RW)
            for h_ in range(2):
                P.tt("vector", flat(POS)[:, 512 * h_:512 * h_ + 512], ps[h_][:], flat(CAR)[:, 512 * h_:512 * h_ + 512], ALU.add,
                     [Bps[h_], BR], [BR])
            P.ts("vector", flat(POS), flat(POS), float(CAP), None, ALU.min, None, *RW)
            P.tt("vector", POS[:], POS[:], cf[:, C_ES:C_ES + 32].unsqueeze(1).to_broadcast([128, 32, 32]), ALU.add, [BR, Bc], [BR])
            tokf = cf[:, C_TOK:C_TOK + 32]
            for kk in range(2):
                AK = A1 if kk == 0 else A2
                P.tt("vector", TMP[:], POS[:], AK[:], ALU.mult, *RW)
                vop(lambda e, kk=kk: e.reduce_sum(out=sc(9 + kk), in_=TMP[:], axis=AX.X))
                P.cp("vector", DST[:, kk * 32:(kk + 1) * 32], sc(9 + kk), [BR], [BR])
                P.cp("vector", PAY[:, kk * 32:(kk + 1) * 32, 0], tokf, [Bc], [BPAY])
                if kk == 0:
                    P.cp("vector", PAY[:, 0:32, 1], tokf, [Bc], [BPAY])
                else:
                    P.ts("vector", PAY[:, 32:64, 1], tokf, float(T), None, ALU.add, None, [Bc], [BPAY])
                P.cp("vector", PAY[:, kk * 32:(kk + 1) * 32, 2], sc(7 + kk), [BR], [BPAY])
            Bsc = Buf("scat")
            for j_ in range(64):
                P.op("gpsimd", lambda e, j_=j_: e.indirect_dma_start(
                    out=tab_d, out_offset=bass.IndirectOffsetOnAxis(ap=DST[:, j_:j_ + 1], axis=0),
                    in_=PAY[:, j_, :], in_offset=None), [BPAY, BR], [Bd], dma=Bsc)
            P.barrier()
            P.emit()

        if stage >= 5:
          with contextlib.ExitStack() as sE:
            tb3 = [sb(sE, "tb3_%d" % i, [128, 3, TW], F32) for i in range(2)]
            Btb3 = [Buf("tb3_%d" % i) for i in range(2)]
            idx = [sb(sE, "idx%d" % i, [128, 3, 2], I32) for i in range(2)]
            Bidx = [Buf("idx%d" % i) for i in range(2)]
            xg = [[sb(sE, "xg%d_%d" % (i, s_), [128, D], BF16) for s_ in range(3)] for i in range(2)]
            Bxg = [[Buf("xg%d_%d" % (i, s_)) for s_ in range(3)] for i in range(2)]
            xgT = sb(sE, "xgT", [128, KC, CAP], BF16)
            BxgT = Buf("xgT")
            hTe = sb(sE, "hTe", [128, 4, CAP], BF16)
            BhTe = Buf("hTe")
            sg = [sb(sE, "sg%d" % i, [128, CAP], F32) for i in range(2)]
            Bsg = [Buf("sg%d" % i) for i in range(2)]
            ys = [sb(sE, "ys%d" % i, [128, D], F32) for i in range(3)]
            Bys = [Buf("ys%d" % i) for i in range(3)]

            def e_loads(e):
                b = e % 2
                if e >= 2:
                    e_wloads(e)
                P.dma("sync", tb3[b][:], tab_d[e * CS:e * CS + CAP, :].rearrange("(s p) w -> p s w", p=128),
                      [], [Btb3[b]], Btb3[b])
                P.cp("vector", idx[b][:], tb3[b][:, :, 0:2], [Btb3[b]], [Bidx[b]])
                for s_ in range(3):
                    P.op("gpsimd", lambda eng, b=b, s_=s_: eng.indirect_dma_start(
                        out=xg[b][s_][:], out_offset=None, in_=hn_d,
                        in_offset=bass.IndirectOffsetOnAxis(ap=idx[b][:, s_, 0:1], axis=0)),
                        [Bidx[b]], [Bxg[b][s_]], dma=Bxg[b][s_])

            e_loads(0)
            k = 0
            for e in range(NE):
                b = e % 2
                if e + 1 < NE:
                    e_loads(e + 1)
                for s_ in range(3):
                    for c4 in range(4):
                        pti = k % 2
                        k += 1
                        for cc in range(4):
                            c = c4 * 4 + cc
                            P.tr(pt[pti][:, cc * 128:(cc + 1) * 128], xg[b][s_][:, c * 128:(c + 1) * 128], identb,
                                 [Bxg[b][s_], Bc], [Bpt[pti]])
                        evac(xgT[:, c4 * 4:c4 * 4 + 4, s_ * 128:(s_ + 1) * 128],
                             pt[pti][:, 0:512].rearrange("p (c t) -> p c t", c=4), [Bpt[pti]], [BxgT])
                for m in range(4):
                    pg_, pu_ = (2 * m) % 4, (2 * m + 1) % 4
                    sgi = m % 2
                    for c in range(KC):
                        P.mm(ps[pg_][:, 0:CAP], wgu[b][:, c, m * 128:(m + 1) * 128], xgT[:, c, :], c == 0, c == KC - 1,
                             [Bwe[b], BxgT], [Bps[pg_]])
                    for c in range(KC):
                        P.mm(ps[pu_][:, 0:CAP], wgu[b][:, c, 512 + m * 128:512 + (m + 1) * 128], xgT[:, c, :], c == 0, c == KC - 1,
                             [Bwe[b], BxgT], [Bps[pu_]])
                    P.act(sg[sgi][:], ps[pg_][:, 0:CAP], AF.Silu, [Bps[pg_]], [Bsg[sgi]])
                    P.tt("vector", hTe[:, m, :], sg[sgi][:], ps[pu_][:, 0:CAP], ALU.mult, [Bsg[sgi], Bps[pu_]], [BhTe])
                for s_ in range(3):
                    for n in range(4):
                        pi = 4 + (n % 2)
                        for m in range(4):
                            P.mm(ps[pi][:], hTe[:, m, s_ * 128:(s_ + 1) * 128], wdn[b][:, m, n * 512:(n + 1) * 512], m == 0, m == 3,
                                 [BhTe, Bwe[b]], [Bps[pi]])
                        if n % 2:
                            P.act(ys[s_][:, n * 512:(n + 1) * 512], ps[pi][:], AF.Copy, [Bps[pi], Btb3[b]], [Bys[s_]],
                                  scale=tb3[b][:, s_, 2:3])
                        else:
                            P.ts("vector", ys[s_][:, n * 512:(n + 1) * 512], ps[pi][:], tb3[b][:, s_, 2:3], None, ALU.mult, None,
                                 [Bps[pi], Btb3[b]], [Bys[s_]])
                    P.op("gpsimd", lambda eng, b=b, s_=s_: eng.indirect_dma_start(
                        out=ybuf_d, out_offset=bass.IndirectOffsetOnAxis(ap=idx[b][:, s_, 1:2], axis=0),
                        in_=ys[s_][:], in_offset=None), [Bys[s_], Bidx[b]], [Bd], dma=Bys[s_])
            P.barrier()
            P.emit()

        if stage >= 4:
            sWe.close()
        if stage >= 6:
          with contextlib.ExitStack() as sF:
            wpg = sb(sF, "wpg", [128, KC, D], BF16)
            wpe = sb(sF, "wpe", [128, 2, D], BF16)
            Bwp = Buf("wp")
            P.dma("gpsimd", wpg[:, 0:8, :], chunked(w_pg)[:, 0:8, :], [], [Bwp], Bwp)
            P.dma("gpsimd", wpg[:, 8:16, :], chunked(w_pg)[:, 8:16, :], [], [Bwp], Bwp)
            P.dma("gpsimd", wpe[:], chunked(w_pe), [], [Bwp], Bwp)
            xb = [sb(sF, "xb%d" % i, [128, KC, 256], F32) for i in range(3)]
            Bxb = [Buf("xb%d" % i) for i in range(3)]
            y0 = [sb(sF, "y0_%d" % i, [128, D], F32) for i in range(4)]
            By = [Buf("y%d" % i) for i in range(4)]
            pbf = [sb(sF, "pbf%d" % i, [128, 2, 256], BF16) for i in range(3)]
            Bpb = [Buf("pbf%d" % i) for i in range(3)]
            sqA = sb(sF, "sqA", [128, KC, 256], BF16)
            sqB = sqA
            BsqA = Buf("sqA")
            BsqB = BsqA
            srtA = sb(sF, "srtA", [128, 256], F32)
            srtB = sb(sF, "srtB", [128, 256], F32)
            BsrtA, BsrtB = Buf("srtA"), Buf("srtB")
            rstdA = sb(sF, "rstdA", [128, 256], F32)
            rstdB = sb(sF, "rstdB", [128, 256], F32)
            BrstdA, BrstdB = Buf("rstdA"), Buf("rstdB")
            hp = [sb(sF, "hp%d" % i, [128, KC, 256], BF16) for i in range(2)]
            Bhp = [Buf("hp%d" % i) for i in range(2)]
            sgt = [sb(sF, "sgt%d" % i, [128, 256], F32) for i in range(2)]
            Bsgt = [Buf("sgt%d" % i) for i in range(2)]

            def f_loads(tb):
                b = tb % 3
                sl = slice(tb * 256, (tb + 1) * 256)
                P.dma("sync", xb[b][:], chunked(x1T_d)[:, :, sl], [], [Bxb[b]], Bxb[b])
                for tt_ in range(2):
                    tile_i = tb * 2 + tt_
                    yi = (tb % 2) * 2 + tt_
                    P.dma("sync", y0[yi][:], ybuf_d[tile_i * 128:(tile_i + 1) * 128, :], [], [By[yi]], By[yi])

            def f_loads2(tb):
                b = tb % 3
                sl = slice(tb * 256, (tb + 1) * 256)
                P.dma("gpsimd", pbf[b][:], chunked(pT)[:, :, sl], [], [Bpb[b]], Bpb[b])
                for tt_ in range(2):
                    tile_i = tb * 2 + tt_
                    yi = (tb % 2) * 2 + tt_
                    P.op("gpsimd", lambda e, yi=yi, tile_i=tile_i: e.dma_start(
                        out=y0[yi][:], in_=ybuf_d[T + tile_i * 128:T + (tile_i + 1) * 128, :], accum_op=ALU.add),
                        [], [By[yi]], dma=By[yi])

            def f_front_steps(tb):
                b = tb % 3
                hb2 = tb % 2
                steps = []

                def pool_():
                    pass
                steps.append(pool_)
                for c in range(KC):
                    def st(c=c):
                        pi = c % 2
                        for tt_ in range(2):
                            yi = (tb % 2) * 2 + tt_
                            P.tr(ps[pi][:, tt_ * 128:(tt_ + 1) * 128], y0[yi][:, c * 128:(c + 1) * 128], identf, [By[yi], Bc], [Bps[pi]])
                        P.tt("vector", xb[b][:, c, :], ps[pi][:, 0:256], xb[b][:, c, :], ALU.add, [Bps[pi], Bxb[b]], [Bxb[b]])
                    steps.append(st)

                def tail_():
                    if debug:
                        P.dma("sync", chunked(dbg_x2)[:, :, tb * 256:(tb + 1) * 256], xb[b][:], [Bxb[b]], [Bd], Bxb[b])
                    rms_stats(xb[b], Bxb[b], 256, sqA, BsqA, ps[5], Bps[5], srtA, BsrtA, rstdA, BrstdA)
                    for c in range(KC):
                        P.stt(hp[hb2][:, c, :], xb[b][:, c, :], gv[:, G_PLE + c:G_PLE + c + 1], rstdA[:], ALU.mult, ALU.mult,
                              [Bxb[b], BrstdA, Bc], [Bhp[hb2]])
                steps.append(tail_)
                return steps

            def f_back_steps(tb):
                b = tb % 3
                hb2 = tb % 2
                steps = []
                for fg in range(KC):
                    def st(fg=fg):
                        si_ = fg % 2
                        pg_ = 2 + (fg % 2)
                        for c in range(KC):
                            P.mm(ps[pg_][:, 0:256], wpg[:, c, fg * 128:(fg + 1) * 128], hp[hb2][:, c, :], c == 0, c == KC - 1, [Bwp, Bhp[hb2]], [Bps[pg_]])
                        for c in range(2):
                            P.mm(ps[4][:, 0:256], wpe[:, c, fg * 128:(fg + 1) * 128], pbf[b][:, c, :], c == 0, c == 1, [Bwp, Bpb[b]], [Bps[4]])
                        P.act(sgt[si_][:], ps[pg_][:, 0:256], AF.Sigmoid, [Bps[pg_]], [Bsgt[si_]])
                        P.tt("vector", sgt[si_][:], sgt[si_][:], ps[4][:, 0:256], ALU.mult, [Bsgt[si_], Bps[4]], [Bsgt[si_]])
                        P.tt("gpsimd", xb[b][:, fg, :], xb[b][:, fg, :], sgt[si_][:], ALU.add, [Bsgt[si_], Bxb[b]], [Bxb[b]])
                    steps.append(st)

                def tail_():
                    rms_stats(xb[b], Bxb[b], 256, sqB, BsqB, ps[5], Bps[5], srtB, BsrtB, rstdB, BrstdB)
                    for c in range(KC):
                        P.stt(xb[b][:, c, :], xb[b][:, c, :], gv[:, G_FIN + c:G_FIN + c + 1], rstdB[:], ALU.mult, ALU.mult,
                              [Bxb[b], BrstdB, Bc], [Bxb[b]])
                    P.dma("sync", chunked(yT)[:, :, tb * 256:(tb + 1) * 256], xb[b][:], [Bxb[b]], [Bd], Bxb[b])
                steps.append(tail_)
                return steps

            f_loads(0)
            f_loads2(0)
            f_loads(1)
            f_loads2(1)
            for st_ in f_front_steps(0):
                st_()
            for tb in range(16):
                Fs = []
                if tb + 2 < 16:
                    f_loads(tb + 2)
                if tb + 1 < 16:
                    Fs = f_front_steps(tb + 1)
                Bs = f_back_steps(tb)
                if Fs:
                    Fs[0]()
                for i_ in range(KC):
                    Bs[i_]()
                    if Fs and i_ < 8:
                        Fs[1 + 2 * i_]()
                        Fs[2 + 2 * i_]()
                    if Fs and i_ == 8:
                        Fs[17]()
                Bs[16]()
                if tb + 2 < 16:
                    f_loads2(tb + 2)
            P.barrier()
            P.emit()
        if stage < 6:
            pass
    return nc


def _consts():
    cf = np.zeros((128, NCF), np.float32)
    cf[:, C_ID:C_ID + 128] = np.eye(128, dtype=np.float32)
    cf[:, C_ONE:C_ONE + 128] = 1.0
    k = np.arange(128)[:, None]
    q = np.arange(128)[None, :]
    cf[:, C_TRI:C_TRI + 128] = (q >= k)
    cf[:, C_US:C_US + 128] = (k < q)
    cf[:, C_IOTA] = np.arange(128)
    cf[:, C_ES:C_ES + 32] = (np.arange(32) * CS)[None, :]
    cf[:, C_TOK:C_TOK + 32] = np.arange(32)[None, :] * 128 + np.arange(128)[:, None]
    sel8 = np.zeros((8, 1032), np.float32)
    for h in range(8):
        sel8[h, h * 128:(h + 1) * 128] = 1.0
        sel8[h, 1024 + h] = 1.0
    tabinit = np.zeros((128, TW), np.float32)
    tabinit[:, 1] = 2 * T
    return cf, sel8, tabinit


def _shared_inputs(inp):
    f = lambda a: np.ascontiguousarray(a, dtype=np.float32)
    cf, sel8, tabinit = _consts()
    gl = lambda g: g.reshape(16, 128).T
    gvec = np.concatenate([gl(inp["g_mix"][0]), gl(inp["g_ffn"][0]), gl(inp["g_ple"][0]), gl(inp["g_final"])], axis=1)
    cw = inp["conv_w"][0].reshape(3, 8, 128).transpose(2, 1, 0).reshape(128, 24)
    w_r = np.concatenate([inp["w_router_group"][0], inp["w_router_expert"][0]], axis=1)
    b_r = np.concatenate([inp["b_router_group"][0], inp["b_router_expert"][0]])[None, :].repeat(128, axis=0)
    return {
        "w_in": f(inp["w_in"][0]), "gvec": f(gvec), "b_f": f(inp["b_f"][0].reshape(8, 1)), "conv_w": f(cw),
        "w_a": f(inp["w_branch_a"][0]), "w_b": f(inp["w_branch_b"][0]), "w_o": f(inp["w_out"][0]),
        "w_r": f(w_r), "b_r": f(b_r), "w_gu": f(inp["w_gate_up"][0]), "w_dn": f(inp["w_down"][0]),
        "w_pg": f(inp["w_ple_gate"][0]), "w_pe": f(inp["w_ple_proj"][0]),
        "cf": cf, "sel8": sel8, "tabinit": tabinit,
    }


def _core_inputs(inp, b, shared):
    m = dict(shared)
    m["xT"] = np.ascontiguousarray(np.asarray(inp["x"][b], dtype=np.float32).T)
    m["pT"] = np.ascontiguousarray(np.asarray(inp["p"][0, b], dtype=np.float32).T)
    return m


def kernel(**inputs):
    inp = {k: np.asarray(v) for k, v in inputs.items()}
    nb = inp["x"].shape[0]
    shared = _shared_inputs(inp)
    nc = build()
    in_maps = [_core_inputs(inp, b, shared) for b in range(nb)]
    res = run_bass_kernel_spmd(nc, in_maps, core_ids=list(range(nb)))
    out = np.empty((nb, T, D), np.float32)
    for b in range(nb):
        out[b] = np.asarray(res.results[b]["yT"]).T
    return out
```

```python
import contextlib
import numpy as np
import concourse.bass as bass
import concourse.mybir as mybir
from concourse.bass_utils import run_bass_kernel_spmd

F32 = mybir.dt.float32
BF16 = mybir.dt.bfloat16
I32 = mybir.dt.int32
ALU = mybir.AluOpType
AF = mybir.ActivationFunctionType
AX = mybir.AxisListType

ENGS = ["tensor", "vector", "scalar", "gpsimd", "sync"]

T = 4096
D = 2048
KC = 16
NH = 8
NE = 32
CAP = 384
CS = CAP + 1
NRT = 97
TW = 128
EPS = 1e-6
QSCALE = 128 ** -0.5
C_ID, C_ONE, C_TRI, C_US, C_IOTA, C_ES, C_TOK = 0, 128, 256, 384, 512, 513, 545
NCF = 577


class Buf:
    __slots__ = ("name", "w", "r", "sem", "cnt", "multi")

    def __init__(self, name, multi=False):
        self.name = name
        self.w = {}
        self.r = {}
        self.sem = None
        self.cnt = 0
        self.multi = multi


class Op:
    __slots__ = ("eng", "fn", "deps", "dma", "signal", "idx", "seq", "phase")


class Prog:
    def __init__(self, nc, stack):
        self.nc = nc
        self.stack = stack
        self.ops = {e: [] for e in ENGS}
        self.esem = {e: stack.enter_context(nc.semaphore("es_" + e)) for e in ENGS}
        self.ecnt = {e: 0 for e in ENGS}
        self.nsem = len(ENGS)
        self.phase = 0
        self.seq = 0
        self.dmabufs = []
        self.stats = []

    def _mk(self, eng, fn, deps, dma):
        o = Op()
        o.eng = eng
        o.fn = fn
        o.dma = dma
        o.signal = False
        o.idx = 0
        o.deps = deps
        o.phase = self.phase
        self.seq += 1
        o.seq = self.seq
        if dma is not None:
            if dma.sem is None:
                dma.sem = self.stack.enter_context(self.nc.semaphore("ds%d" % self.nsem))
                self.nsem += 1
                self.dmabufs.append(dma)
            dma.cnt += 16
        self.ops[eng].append(o)
        return o

    def op(self, eng, fn, reads=(), writes=(), dma=None):
        deps = {}

        def add(d):
            if d.phase < self.phase:
                return
            if d.dma is not None:
                deps[id(d.dma)] = ("d", d.dma, d.dma.cnt)
            else:
                if d.eng == "tensor" and eng == "tensor" and dma is None:
                    return
                prev = deps.get(d.eng)
                if prev is None or prev[1].seq < d.seq:
                    deps[d.eng] = ("e", d, 0)

        for b in reads:
            for d in b.w.values():
                add(d)
        for b in writes:
            if not b.multi:
                for d in b.w.values():
                    add(d)
            for d in b.r.values():
                add(d)
        o = self._mk(eng, fn, list(deps.values()), dma)
        key = id(dma) if dma is not None else eng
        for b in reads:
            b.r[key] = o
        for b in writes:
            if b.multi:
                b.w[key] = o
            else:
                b.w = {key: o}
                b.r = {}
        return o

    def barrier(self):
        a = {}
        for e in ENGS:
            a[e] = self._mk(e, (lambda eng: eng.drain()), [], None)
        for e in ENGS:
            deps = [("e", a[e2], 0) for e2 in ENGS if e2 != e]
            deps += [("d", b, b.cnt) for b in self.dmabufs if b.cnt > 0]
            self._mk(e, (lambda eng: eng.nop()), deps, None)

    def emit(self):
        nc = self.nc
        for e in ENGS:
            for o in self.ops[e]:
                for dep in o.deps:
                    if dep[0] == "e":
                        dep[1].signal = True
        for e in ENGS:
            for o in self.ops[e]:
                if o.signal and o.dma is None:
                    self.ecnt[e] += 1
                    o.idx = self.ecnt[e]
        stats = {}
        with nc.Block() as block:
            def run(engname, eng):
                known = {}
                nw = 0
                for o in self.ops[engname]:
                    for dep in o.deps:
                        if dep[0] == "d":
                            sem, val = dep[1].sem, dep[2]
                        else:
                            sem, val = self.esem[dep[1].eng], dep[1].idx
                        if known.get(id(sem), 0) >= val:
                            continue
                        eng.wait_ge(sem, val)
                        nw += 1
                        known[id(sem)] = val
                    if o.fn is None:
                        continue
                    ins = o.fn(eng)
                    if o.dma is not None:
                        ins.then_inc(o.dma.sem, 16)
                    elif o.signal:
                        ins.then_inc(self.esem[engname], 1)
                stats[engname] = (len(self.ops[engname]), nw)

            @block.tensor
            def _(eng):
                run("tensor", eng)

            @block.vector
            def _(eng):
                run("vector", eng)

            @block.scalar
            def _(eng):
                run("scalar", eng)

            @block.gpsimd
            def _(eng):
                run("gpsimd", eng)

            @block.sync
            def _(eng):
                run("sync", eng)
        self.stats.append(stats)
        self.ops = {e: [] for e in ENGS}
        self.phase += 1

    def mm(self, out, lhsT, rhs, start, stop, R, W, **kw):
        return self.op("tensor", lambda e: e.matmul(out, lhsT=lhsT, rhs=rhs, start=start, stop=stop, **kw), R, W)

    def tr(self, out, in_, ident, R, W):
        return self.op("tensor", lambda e: e.transpose(out, in_, ident), R, W)

    def act(self, out, in_, func, R, W, **kw):
        return self.op("scalar", lambda e: e.activation(out=out, in_=in_, func=func, **kw), R, W)

    def tt(self, eng, out, in0, in1, op, R, W):
        return self.op(eng, lambda e: e.tensor_tensor(out=out, in0=in0, in1=in1, op=op), R, W)

    def ts(self, eng, out, in0, s1, s2, op0, op1, R, W):
        if s2 is None:
            return self.op(eng, lambda e: e.tensor_scalar(out=out, in0=in0, scalar1=s1, scalar2=None, op0=op0), R, W)
        return self.op(eng, lambda e: e.tensor_scalar(out=out, in0=in0, scalar1=s1, scalar2=s2, op0=op0, op1=op1), R, W)

    def stt(self, out, in0, scalar, in1, op0, op1, R, W):
        return self.op("vector", lambda e: e.scalar_tensor_tensor(out=out, in0=in0, scalar=scalar, in1=in1, op0=op0, op1=op1), R, W)

    def cp(self, eng, out, in_, R, W):
        if eng == "scalar":
            return self.op(eng, lambda e: e.copy(out=out, in_=in_), R, W)
        return self.op(eng, lambda e: e.tensor_copy(out=out, in_=in_), R, W)

    def dma(self, eng, out, in_, R, W, sem):
        return self.op(eng, lambda e: e.dma_start(out=out, in_=in_), R, W, dma=sem)


def build(debug=False, stage=99):
    nc = bass.Bass("TRN2", target_bir_lowering=False)

    def din(name, shape):
        return nc.dram_tensor(name, shape, F32, kind="ExternalInput").ap()

    skind = "ExternalOutput" if debug else "Internal"

    def dsc(name, shape, dt):
        return nc.dram_tensor(name, shape, dt, kind=skind).ap()

    xT = din("xT", [D, T])
    pT = din("pT", [256, T])
    w_in = din("w_in", [D, 10248])
    gvec = din("gvec", [128, 64])
    b_f = din("b_f", [8, 1])
    conv_w = din("conv_w", [128, 24])
    w_a = din("w_a", [1024, D])
    w_b = din("w_b", [1024, D])
    w_o = din("w_o", [D, D])
    w_r = din("w_r", [D, 36])
    b_r = din("b_r", [128, 36])
    w_gu = din("w_gu", [NE, D, 1024])
    w_dn = din("w_dn", [NE, 512, D])
    w_pg = din("w_pg", [D, D])
    w_pe = din("w_pe", [256, D])
    cfd = din("cf", [128, NCF])
    sel8d = din("sel8", [8, 1032])
    tabinit = din("tabinit", [128, TW])
    yT = nc.dram_tensor("yT", [D, T], F32, kind="ExternalOutput").ap()

    qT_d = dsc("qT_d", [1024, T], BF16)
    kT_d = dsc("kT_d", [1024, T], BF16)
    v_d = dsc("v_d", [T, 1024], BF16)
    bmT_d = dsc("bmT_d", [1024, T], BF16)
    gaT_d = dsc("gaT_d", [D, T], BF16)
    gbT_d = dsc("gbT_d", [D, T], BF16)
    negc_d = dsc("negc_d", [8, T], F32)
    attnT_d = dsc("attnT_d", [1024, T], BF16)
    mgT_d = dsc("mgT_d", [D, T], BF16)
    x1T_d = dsc("x1T_d", [D, T], F32)
    hn_d = dsc("hn_d", [T, D], BF16)
    tab_d = dsc("tab_d", [NRT * 128, TW], F32)
    ybuf_d = dsc("ybuf_d", [2 * T + 128, D], F32)
    dbg_x2 = dsc("dbg_x2", [D, T], F32) if debug else None

    def chunked(ap):
        return ap.rearrange("(c p) n -> p c n", p=128)

    with contextlib.ExitStack() as glob:
        P = Prog(nc, glob)

        sbn = [0]

        def sb(st, name, shape, dt):
            sbn[0] += 1
            return st.enter_context(nc.sbuf_tensor("s%d_%s" % (sbn[0], name), shape, dt))

        cf = sb(glob, "cf", [128, NCF], F32)
        cb = sb(glob, "cb", [128, 512], BF16)
        gv = sb(glob, "gv", [128, 64], F32)
        sel8 = sb(glob, "sel8", [8, 1032], F32)
        Bc = Buf("consts")
        ps = [glob.enter_context(nc.psum_tensor("ps%d" % i, [128, 512], F32)) for i in range(6)]
        Bps = [Buf("ps%d" % i) for i in range(6)]
        pt = [glob.enter_context(nc.psum_tensor("pt%d" % i, [128, 1024], BF16)) for i in range(2)]
        Bpt = [Buf("pt%d" % i) for i in range(2)]
        Bd = Buf("dram", multi=True)

        P.dma("sync", cf[:], cfd, [], [Bc], Bc)
        P.dma("sync", gv[:], gvec, [], [Bc], Bc)
        P.dma("sync", sel8[:], sel8d, [], [Bc], Bc)
        P.dma("gpsimd", cb[:], cfd[:, 0:512], [], [Bc], Bc)
        identb = cb[:, C_ID:C_ID + 128]
        onesb = cb[:, C_ONE:C_ONE + 128]
        trib = cb[:, C_TRI:C_TRI + 128]
        usb = cb[:, C_US:C_US + 128]
        identf = cf[:, C_ID:C_ID + 128]
        G_MIX, G_FFN, G_PLE, G_FIN = 0, 16, 32, 48

        evac_ctr = [0]

        def evac(out, in_, R, W):
            evac_ctr[0] += 1
            if evac_ctr[0] % 2:
                return P.cp("scalar", out, in_, R, W)
            return P.cp("vector", out, in_, R, W)

        def rms_stats(xblk, Bx, n, sq, Bsq, psn, Bpsn, srt, Bsrt, rstd, Brstd):
            P.act(sq[:, :, 0:n], xblk[:, :, 0:n], AF.Square, [Bx], [Bsq])
            for c in range(KC):
                P.mm(psn[:, 0:n], onesb, sq[:, c, 0:n], c == 0, c == KC - 1, [Bsq, Bc], [Bpsn])
            P.act(srt[:, 0:n], psn[:, 0:n], AF.Sqrt, [Bpsn], [Bsrt], bias=EPS, scale=1.0 / D)
            P.op("vector", lambda e: e.reciprocal(out=rstd[:, 0:n], in_=srt[:, 0:n]), [Bsrt], [Brstd])

        with contextlib.ExitStack() as sA:
            hT = sb(sA, "hT", [128, KC, T], BF16)
            BhT = [Buf("hT%d" % i) for i in range(16)]
            with contextlib.ExitStack() as s0:
                xb = [sb(s0, "xb%d" % i, [128, KC, 256], F32) for i in range(2)]
                Bxb = [Buf("xb%d" % i) for i in range(2)]
                sq = sb(s0, "sq", [128, KC, 256], BF16)
                Bsq = Buf("sq")
                srt = sb(s0, "srt", [128, 256], F32)
                Bsrt = Buf("srt")
                rstd = sb(s0, "rstd", [128, 256], F32)
                Brstd = Buf("rstd")
                xTv = chunked(xT)
                for tb in range(16):
                    b = tb % 2
                    P.dma("sync", xb[b][:], xTv[:, :, tb * 256:(tb + 1) * 256], [], [Bxb[b]], Bxb[b])
                    rms_stats(xb[b], Bxb[b], 256, sq, Bsq, ps[0], Bps[0], srt, Bsrt, rstd, Brstd)
                    for c in range(KC):
                        P.stt(hT[:, c, tb * 256:(tb + 1) * 256], xb[b][:, c, :], gv[:, G_MIX + c:G_MIX + c + 1],
                              rstd[:], ALU.mult, ALU.mult, [Bxb[b], Brstd, Bc], [BhT[tb]])
                P.barrier()
                P.emit()
            if stage >= 1:
              with contextlib.ExitStack() as s1:
                wsl = [sb(s1, "wsl%d" % i, [128, KC, 512], BF16) for i in range(2)]
                Bw = [Buf("wsl%d" % i) for i in range(2)]
                stg = [sb(s1, "stg%d" % i, [128, 4, 512], BF16) for i in range(2)]
                Bstg = [Buf("stg%d" % i) for i in range(2)]
                usb_ = sb(s1, "u_sb", [128, 512], F32)
                Bus = Buf("u_sb")
                zb = sb(s1, "zb", [128, 514], F32)
                Bz = Buf("zb")
                acc = sb(s1, "cacc", [128, 512], F32)
                Bacc = Buf("cacc")
                cw = sb(s1, "cw", [128, 24], F32)
                negb = sb(s1, "negb", [8, 1], F32)
                lsp = sb(s1, "lsp", [8, 512], F32)
                Blsp = Buf("lsp")
                one8 = sb(s1, "one8", [8, 512], F32)
                ncs = sb(s1, "ncs", [8, T], F32)
                Bncs = Buf("ncs")
                Bcw = Buf("cw")
                P.dma("sync", cw[:], conv_w, [], [Bcw], Bcw)
                P.dma("sync", negb[:], b_f, [], [Bcw], Bcw)
                P.ts("vector", negb[:], negb[:], -1.0, None, ALU.mult, None, [Bcw], [Bcw])
                P.op("vector", lambda e: e.memset(one8[:], 1.0), [], [Bcw])
                w_in_v = chunked(w_in)
                allh = list(BhT)
                slab_i = [0]
                psr = [0]

                def next_ps():
                    psr[0] = (psr[0] + 1) % 6
                    return psr[0]

                def load_slab(cols):
                    i = slab_i[0] % 2
                    slab_i[0] += 1
                    o = 0
                    for (c0, n) in cols:
                        P.dma("gpsimd", wsl[i][:, :, o:o + n], w_in_v[:, :, c0:c0 + n], [], [Bw[i]], Bw[i])
                        o += n
                    return i

                stg_i = [0]

                def fm_plain(col0, dest, row0, sigmoid):
                    i = load_slab([(col0, 512)])
                    for tb in range(8):
                        si = stg_i[0] % 2
                        stg_i[0] += 1
                        for m in range(4):
                            pi = next_ps()
                            for c in range(KC):
                                P.mm(ps[pi][:], wsl[i][:, c, m * 128:(m + 1) * 128], hT[:, c, tb * 512:(tb + 1) * 512],
                                     c == 0, c == KC - 1, [Bw[i], BhT[2 * tb], BhT[2 * tb + 1]], [Bps[pi]])
                            if sigmoid:
                                P.act(stg[si][:, m, :], ps[pi][:], AF.Sigmoid, [Bps[pi]], [Bstg[si]])
                            else:
                                evac(stg[si][:, m, :], ps[pi][:], [Bps[pi]], [Bstg[si]])
                        P.dma("sync", dest[row0:row0 + 512, tb * 512:(tb + 1) * 512].rearrange("(m p) t -> p m t", p=128),
                              stg[si][:], [Bstg[si]], [Bd], Bstg[si])

                for s in range(2):
                    fm_plain(s * 512, qT_d, s * 512, False)
                for s in range(2):
                    fm_plain(1024 + s * 512, kT_d, s * 512, False)
                for s in range(2):
                    i = load_slab([(2048 + s * 512, 512)])
                    for t4 in range(8):
                        si = stg_i[0] % 2
                        stg_i[0] += 1
                        for j in range(4):
                            tt_ = t4 * 4 + j
                            pi = next_ps()
                            for c in range(KC):
                                P.mm(ps[pi][:], hT[:, c, tt_ * 128:(tt_ + 1) * 128], wsl[i][:, c, :],
                                     c == 0, c == KC - 1, [Bw[i], BhT[tt_ // 2]], [Bps[pi]])
                            evac(stg[si][:, j, :], ps[pi][:], [Bps[pi]], [Bstg[si]])
                        P.dma("sync", v_d[t4 * 512:(t4 + 1) * 512, s * 512:(s + 1) * 512].rearrange("(j p) n -> p j n", p=128),
                              stg[si][:], [Bstg[si]], [Bd], Bstg[si])
                i = load_slab([(3072, 8)])
                for tb in range(8):
                    pi = next_ps()
                    for c in range(KC):
                        P.mm(ps[pi][0:8, :], wsl[i][:, c, 0:8], hT[:, c, tb * 512:(tb + 1) * 512],
                             c == 0, c == KC - 1, [Bw[i], BhT[2 * tb], BhT[2 * tb + 1]], [Bps[pi]])
                    P.act(lsp[:], ps[pi][0:8, :], AF.Exp, [Bps[pi], Bcw], [Blsp], bias=negb[:, 0:1], scale=-1.0)
                    P.act(lsp[:], lsp[:], AF.Ln, [Blsp], [Blsp], bias=1.0, scale=1.0)
                    init = 0.0 if tb == 0 else ncs[:, tb * 512 - 1:tb * 512]
                    P.op("vector", lambda e, tb=tb, init=init: e.tensor_tensor_scan(
                        out=ncs[:, tb * 512:(tb + 1) * 512], data0=one8[:], data1=lsp[:], initial=init,
                        op0=ALU.mult, op1=ALU.add), [Blsp, Bcw, Bncs], [Bncs])
                P.dma("sync", negc_d, ncs[:], [Bncs], [Bd], Bncs)
                for j in range(8):
                    i = load_slab([(3080 + j * 128, 128), (4104 + j * 128, 128), (5128 + j * 128, 128)])
                    P.op("vector", lambda e: e.memset(zb[:, 0:2], 0.0), [], [Bz])
                    for tb in range(8):
                        if tb % 4 == 0:
                            si = stg_i[0] % 2
                            stg_i[0] += 1
                        pis = []
                        for m in range(3):
                            pi = next_ps()
                            pis.append(pi)
                            for c in range(KC):
                                P.mm(ps[pi][:], wsl[i][:, c, m * 128:(m + 1) * 128], hT[:, c, tb * 512:(tb + 1) * 512],
                                     c == 0, c == KC - 1, [Bw[i], BhT[2 * tb], BhT[2 * tb + 1]], [Bps[pi]])
                        pu, pbg, pcg = pis
                        P.cp("scalar", usb_[:], ps[pu][:], [Bps[pu]], [Bus])
                        P.tt("vector", zb[:, 2:514], ps[pcg][:], usb_[:], ALU.mult, [Bps[pcg], Bus], [Bz])
                        P.ts("vector", acc[:], zb[:, 2:514], cw[:, j * 3 + 2:j * 3 + 3], None, ALU.mult, None, [Bz, Bcw], [Bacc])
                        P.stt(acc[:], zb[:, 1:513], cw[:, j * 3 + 1:j * 3 + 2], acc[:], ALU.mult, ALU.add, [Bz, Bcw, Bacc], [Bacc])
                        P.stt(acc[:], zb[:, 0:512], cw[:, j * 3 + 0:j * 3 + 1], acc[:], ALU.mult, ALU.add, [Bz, Bcw, Bacc], [Bacc])
                        P.tt("vector", stg[si][:, tb % 4, :], ps[pbg][:], acc[:], ALU.mult, [Bps[pbg], Bacc], [Bstg[si]])
                        P.cp("vector", zb[:, 0:2], zb[:, 512:514], [Bz], [Bz])
                        if tb % 4 == 3:
                            t0 = (tb - 3) * 512
                            P.dma("sync", bmT_d[j * 128:(j + 1) * 128, t0:t0 + 2048].rearrange("p (m t) -> p m t", m=4),
                                  stg[si][:], [Bstg[si]], [Bd], Bstg[si])
                for s in range(4):
                    fm_plain(6152 + s * 512, gaT_d, s * 512, True)
                for s in range(4):
                    fm_plain(6152 + 2048 + s * 512, gbT_d, s * 512, True)
                P.barrier()
                P.emit()

        sWab = contextlib.ExitStack()
        if stage >= 3:
            wa = sb(sWab, "wa", [128, 8, D], BF16)
            wb = sb(sWab, "wb", [128, 8, D], BF16)
            Bwab = Buf("wab")
            P.dma("gpsimd", wa[:], chunked(w_a), [], [Bwab], Bwab)
            P.dma("gpsimd", wb[:], chunked(w_b), [], [Bwab], Bwab)
        if stage >= 2:
          with contextlib.ExitStack() as sB:
            qh = [sb(sB, "qh%d" % i, [128, T], BF16) for i in range(2)]
            kh = [sb(sB, "kh%d" % i, [128, T], BF16) for i in range(2)]
            vh = [sb(sB, "vh%d" % i, [128, 32, 129], BF16) for i in range(2)]
            ncq = [sb(sB, "ncq%d" % i, [128, T], F32) for i in range(2)]
            Bq = [Buf("qh%d" % i) for i in range(2)]
            Bk = [Buf("kh%d" % i) for i in range(2)]
            Bv = [Buf("vh%d" % i) for i in range(2)]
            Bncq = [Buf("ncq%d" % i) for i in range(2)]
            ncs2 = sb(sB, "ncs2", [8, T], F32)
            Bn2 = Buf("ncs2")
            nck = sb(sB, "nck", [128, 32, 8], F32)
            Bnck = Buf("nck")
            rden = sb(sB, "rden", [128, 4], F32)
            Brd = Buf("rden")
            atok = [sb(sB, "atok%d" % i, [128, 4, 128], BF16) for i in range(2)]
            Bat = [Buf("atok%d" % i) for i in range(2)]
            ast = [sb(sB, "ast%d" % i, [128, 512], BF16) for i in range(2)]
            Bast = [Buf("ast%d" % i) for i in range(2)]
            P.dma("sync", ncs2[:], negc_d, [], [Bn2], Bn2)
            for j in range(32):
                P.mm(ps[5][:, j * 8:(j + 1) * 8], ncs2[:, j * 128:(j + 1) * 128], sel8[:, 1024:1032], True, True, [Bn2, Bc], [Bps[5]])
            P.cp("vector", nck[:].rearrange("p j h -> p (j h)"), ps[5][:, 0:256], [Bps[5]], [Bnck])
            for i in range(2):
                P.op("gpsimd", lambda e, i=i: e.memset(vh[i][:, :, 128:129], 1.0), [], [Bv[i]])
            pend = []
            sidx = [0]
            blk = [0]
            ACCB = [2, 3, 4, 5]
            rden = sb(sB, "rden2", [128, 2, 4], F32)

            def loads(h):
                hb = h % 2
                P.dma("sync", qh[hb][:], qT_d[h * 128:(h + 1) * 128, :], [], [Bq[hb]], Bq[hb])
                P.dma("sync", kh[hb][:], kT_d[h * 128:(h + 1) * 128, :], [], [Bk[hb]], Bk[hb])
                P.dma("sync", vh[hb][:, :, 0:128], v_d[:, h * 128:(h + 1) * 128].rearrange("(j p) d -> p j d", p=128),
                      [], [Bv[hb]], Bv[hb])
                P.dma("sync", ncq[hb][:], negc_d[h, :].partition_broadcast(128), [], [Bncq[hb]], Bncq[hb])

            loads(0)
            NT_ = 5
            SB_ = [ps[0], ps[1], pt[1][:].bitcast(F32)]
            BSB_ = [Bps[0], Bps[1], Bpt[1]]
            tS = [sb(sB, "tSx%d" % i, [128, 512], F32) for i in range(NT_)]
            BtS = [Buf("tSx%d" % i) for i in range(NT_)]
            pTt = [sb(sB, "pTx%d" % i, [128, 512], BF16) for i in range(NT_)]
            BpT = [Buf("pTx%d" % i) for i in range(NT_)]
            LOOK = 3
            tiles = []
            for h in range(NH):
                for qb in range(8):
                    for kt in range(4 * (qb + 1)):
                        tiles.append((h, qb, kt))

            def front(n):
                h, qb, kt = tiles[n]
                hb = h % 2
                dj = kt - 4 * qb
                qlo = max(dj, 0) * 128
                si = n % 3
                ti = n % NT_
                q0 = qb * 512 + qlo
                q1 = (qb + 1) * 512
                P.mm(SB_[si][:, qlo:512], kh[hb][:, kt * 128:(kt + 1) * 128], qh[hb][:, q0:q1], True, True,
                     [Bk[hb], Bq[hb]], [BSB_[si]])
                P.stt(tS[ti][:, qlo:512], SB_[si][:, qlo:512], QSCALE, ncq[hb][:, q0:q1], ALU.mult, ALU.subtract,
                      [BSB_[si], Bncq[hb]], [BtS[ti]])
                P.act(pTt[ti][:, qlo:512], tS[ti][:, qlo:512], AF.Exp, [BtS[ti], Bnck], [BpT[ti]],
                      bias=nck[:, kt, h:h + 1], scale=1.0)
                if dj >= 0:
                    P.tt("gpsimd", pTt[ti][:, qlo:qlo + 128], pTt[ti][:, qlo:qlo + 128], trib, ALU.mult,
                         [BpT[ti], Bc], [BpT[ti]])

            def back(n):
                nonlocal pend
                h, qb, kt = tiles[n]
                hb = h % 2
                if qb == 0 and kt == 0 and h + 1 < NH:
                    loads(h + 1)
                dj = kt - 4 * qb
                ti = n % NT_
                for qs in range(4):
                    if qs < dj:
                        continue
                    a = ACCB[qs]
                    P.mm(ps[a][:, 0:129], pTt[ti][:, qs * 128:(qs + 1) * 128], vh[hb][:, kt, :],
                         kt == 0, kt == 4 * qb + qs, [BpT[ti], Bv[hb]], [Bps[a]])
                if kt == 1 and pend:
                    for f in pend:
                        f()
                    pend = []
                if kt == 4 * (qb + 1) - 1:
                    ab = blk[0] % 2
                    blk[0] += 1
                    for qs in range(4):
                        a = ACCB[qs]
                        P.op("vector", lambda e, a=a, qs=qs, ab=ab: e.reciprocal(out=rden[:, ab, qs:qs + 1], in_=ps[a][:, 128:129]),
                             [Bps[a]], [Brd])
                        if qs % 2 == 0:
                            P.act(atok[ab][:, qs, :], ps[a][:, 0:128], AF.Copy, [Bps[a], Brd], [Bat[ab]], scale=rden[:, ab, qs:qs + 1])
                        else:
                            P.ts("vector", atok[ab][:, qs, :], ps[a][:, 0:128], rden[:, ab, qs:qs + 1], None, ALU.mult, None,
                                 [Bps[a], Brd], [Bat[ab]])

                    def fin(h=h, qb=qb, ab=ab):
                        for qs in range(4):
                            P.tr(pt[0][:, qs * 128:(qs + 1) * 128], atok[ab][:, qs, :], identb, [Bat[ab], Bc], [Bpt[0]])
                        P.cp("vector", ast[ab][:], pt[0][:, 0:512], [Bpt[0]], [Bast[ab]])
                        P.dma("sync", attnT_d[h * 128:(h + 1) * 128, qb * 512:(qb + 1) * 512], ast[ab][:], [Bast[ab]], [Bd], Bast[ab])
                    pend.append(fin)

            for n in range(len(tiles) + LOOK):
                if n < len(tiles):
                    front(n)
                if n - LOOK >= 0:
                    back(n - LOOK)
            for f in pend:
                f()
            P.barrier()
            P.emit()

        if stage >= 3:
          with contextlib.ExitStack() as sC:
            at = [sb(sC, "at%d" % i, [128, 8, 512], BF16) for i in range(2)]
            bm = [sb(sC, "bm%d" % i, [128, 8, 512], BF16) for i in range(2)]
            ga = [sb(sC, "ga%d" % i, [128, KC, 512], BF16) for i in range(2)]
            gb = [sb(sC, "gb%d" % i, [128, KC, 512], BF16) for i in range(2)]
            Bin = [Buf("c1in%d" % i) for i in range(2)]
            t1 = [sb(sC, "t1_%d" % i, [128, 512], F32) for i in range(2)]
            t2 = [sb(sC, "t2_%d" % i, [128, 512], F32) for i in range(2)]
            Bt1 = [Buf("t1_%d" % i) for i in range(2)]
            Bt2 = [Buf("t2_%d" % i) for i in range(2)]
            mst = [sb(sC, "mst%d" % i, [128, 4, 512], BF16) for i in range(2)]
            Bmst = [Buf("mst%d" % i) for i in range(2)]

            def c1_loads(tb):
                b = tb % 2
                sl = slice(tb * 512, (tb + 1) * 512)
                P.dma("sync", at[b][:], chunked(attnT_d)[:, :, sl], [], [Bin[b]], Bin[b])
                P.dma("sync", bm[b][:], chunked(bmT_d)[:, :, sl], [], [Bin[b]], Bin[b])
                P.dma("sync", ga[b][:], chunked(gaT_d)[:, :, sl], [], [Bin[b]], Bin[b])
                P.dma("sync", gb[b][:], chunked(gbT_d)[:, :, sl], [], [Bin[b]], Bin[b])

            c1_loads(0)
            k = 0
            for tb in range(8):
                b = tb % 2
                if tb + 1 < 8:
                    c1_loads(tb + 1)
                for fg in range(KC):
                    pa = (2 * k) % 6
                    pb = (2 * k + 1) % 6
                    tb_ = k % 2
                    k += 1
                    if fg % 4 == 0:
                        mi = (tb * 4 + fg // 4) % 2
                    for c in range(8):
                        P.mm(ps[pa][:], wa[:, c, fg * 128:(fg + 1) * 128], at[b][:, c, :], c == 0, c == 7, [Bwab, Bin[b]], [Bps[pa]])
                    for c in range(8):
                        P.mm(ps[pb][:], wb[:, c, fg * 128:(fg + 1) * 128], bm[b][:, c, :], c == 0, c == 7, [Bwab, Bin[b]], [Bps[pb]])
                    P.tt("vector", t1[tb_][:], ps[pa][:], ga[b][:, fg, :], ALU.mult, [Bps[pa], Bin[b]], [Bt1[tb_]])
                    P.tt("vector", t2[tb_][:], ps[pb][:], gb[b][:, fg, :], ALU.mult, [Bps[pb], Bin[b]], [Bt2[tb_]])
                    P.tt("gpsimd", mst[mi][:, fg % 4, :], t1[tb_][:], t2[tb_][:], ALU.add, [Bt1[tb_], Bt2[tb_]], [Bmst[mi]])
                    if fg % 4 == 3:
                        r0 = (fg - 3) * 128
                        P.dma("sync", mgT_d[r0:r0 + 512, tb * 512:(tb + 1) * 512].rearrange("(m p) t -> p m t", p=128),
                              mst[mi][:], [Bmst[mi]], [Bd], Bmst[mi])
            P.barrier()
            P.emit()

        sWab.close()
        if stage >= 4:
          LGraw = sb(glob, "LGraw", [128, 32, 36], F32)
          SSq = sb(glob, "SSq", [128, 32], F32)
          BLG = Buf("lgraw", multi=True)
          with contextlib.ExitStack() as sC:
            wo = sb(sC, "wo", [128, KC, D], BF16)
            Bwo = Buf("wo")
            P.dma("gpsimd", wo[:, 0:8, :], chunked(w_o)[:, 0:8, :], [], [Bwo], Bwo)
            P.dma("gpsimd", wo[:, 8:16, :], chunked(w_o)[:, 8:16, :], [], [Bwo], Bwo)
            mg = [sb(sC, "mg%d" % i, [128, KC, 256], BF16) for i in range(3)]
            xb = [sb(sC, "xb%d" % i, [128, KC, 256], F32) for i in range(3)]
            Bmg = [Buf("mg%d" % i) for i in range(3)]
            Bxb = [Buf("xb%d" % i) for i in range(3)]
            sq = sb(sC, "sq", [128, KC, 256], BF16)
            Bsq = Buf("sq")
            srt = sb(sC, "srt", [128, 256], F32)
            Bsrt = Buf("srt")
            rstd = sb(sC, "rstd", [128, 256], F32)
            Brstd = Buf("rstd")
            hnT = sb(sC, "hnT", [128, KC, 256], BF16)
            BhnT = Buf("hnT")
            hnt = [sb(sC, "hnt%d" % i, [128, D], BF16) for i in range(2)]
            Bhnt = [Buf("hnt%d" % i) for i in range(2)]
            wr = sb(sC, "wr", [128, KC, 36], F32)
            brb = sb(sC, "brb", [128, 36], F32)
            Bwr = Buf("wr")
            P.dma("sync", wr[:], chunked(w_r), [], [Bwr], Bwr)
            P.dma("sync", brb[:], b_r, [], [Bwr], Bwr)
            for c in range(KC):
                P.ts("vector", wr[:, c, :], wr[:, c, :], gv[:, G_FFN + c:G_FFN + c + 1], None, ALU.mult, None, [Bwr, Bc], [Bwr])
            tin = sb(sC, "tin", [128, TW], F32)
            Btin = Buf("tin")
            P.dma("sync", tin[:], tabinit, [], [Btin], Btin)
            Btab = Buf("tab", multi=True)
            tabv = tab_d.rearrange("(j p) w -> p j w", p=128)
            for j in range(NRT):
                P.dma("sync", tabv[:, j, :], tin[:], [Btin], [Btab], Btin)
            def c2_loads(tb):
                b = tb % 3
                sl = slice(tb * 256, (tb + 1) * 256)
                P.dma("sync", mg[b][:], chunked(mgT_d)[:, :, sl], [], [Bmg[b]], Bmg[b])
                P.dma("sync", xb[b][:], chunked(xT)[:, :, sl], [], [Bxb[b]], Bxb[b])

            def route1(tile_i, b, tsl):
                for c in range(KC):
                    P.mm(ps[4][:, 0:36], xb[b][:, c, tsl], wr[:, c, :], c == 0, c == KC - 1, [Bxb[b], Bwr], [Bps[4]])
                for c in range(KC):
                    P.mm(ps[5][:, 0:1], sq[:, c, tsl], onesb[:, 0:1], c == 0, c == KC - 1, [Bsq, Bc], [Bps[5]])
                P.cp("scalar", LGraw[:, tile_i, :], ps[4][:, 0:36], [Bps[4]], [BLG])
                P.cp("scalar", SSq[:, tile_i:tile_i + 1], ps[5][:, 0:1], [Bps[5]], [BLG])

            def x_steps(tb):
                b = tb % 3
                steps = []
                for fg in range(KC):
                    def st(fg=fg):
                        pi = fg % 2
                        for c in range(KC):
                            P.mm(ps[pi][:, 0:256], wo[:, c, fg * 128:(fg + 1) * 128], mg[b][:, c, :], c == 0, c == KC - 1,
                                 [Bwo, Bmg[b]], [Bps[pi]])
                        P.tt("vector", xb[b][:, fg, :], ps[pi][:, 0:256], xb[b][:, fg, :], ALU.add, [Bps[pi], Bxb[b]], [Bxb[b]])
                    steps.append(st)
                return steps

            def x_fin(tb):
                b = tb % 3
                P.dma("sync", chunked(x1T_d)[:, :, tb * 256:(tb + 1) * 256], xb[b][:], [Bxb[b]], [Bd], Bxb[b])

            def y_steps(tb):
                b = tb % 3

                def y0_():
                    rms_stats(xb[b], Bxb[b], 256, sq, Bsq, ps[3], Bps[3], srt, Bsrt, rstd, Brstd)

                def yh_(q4):
                    for c in range(4 * q4, 4 * q4 + 4):
                        P.stt(hnT[:, c, :], xb[b][:, c, :], gv[:, G_FFN + c:G_FFN + c + 1], rstd[:], ALU.mult, ALU.mult,
                              [Bxb[b], Brstd, Bc], [BhnT])

                def ytile(tt_):
                    tile_i = tb * 2 + tt_
                    hb_ = tile_i % 2
                    tsl = slice(tt_ * 128, (tt_ + 1) * 128)
                    for c4 in range(4):
                        pti = c4 % 2
                        for cc in range(4):
                            c = c4 * 4 + cc
                            P.tr(pt[pti][:, cc * 128:(cc + 1) * 128], hnT[:, c, tsl], identb, [BhnT, Bc], [Bpt[pti]])
                        evac(hnt[hb_][:, c4 * 512:(c4 + 1) * 512], pt[pti][:, 0:512], [Bpt[pti]], [Bhnt[hb_]])
                    P.dma("sync", hn_d[tile_i * 128:(tile_i + 1) * 128, :], hnt[hb_][:], [Bhnt[hb_]], [Bd], Bhnt[hb_])
                    route1(tile_i, b, tsl)
                return [y0_, (lambda: ytile(0)), (lambda: ytile(1)), yh_]

            pend2 = []
            c2_loads(0)
            for tb in range(17):
                if tb + 1 < 16:
                    c2_loads(tb + 1)
                X = x_steps(tb) if tb < 16 else []
                Y = y_steps(tb - 1) if tb >= 1 else []
                for fg in range(KC):
                    if X:
                        X[fg]()
                    if fg == 1 and Y:
                        Y[0]()
                    if 2 <= fg <= 5 and Y:
                        Y[3](fg - 2)
                    if fg == 6 and Y:
                        Y[1]()
                    if fg == 11 and Y:
                        Y[2]()
                if tb < 16:
                    x_fin(tb)
            P.barrier()
            P.emit()

          sWe = contextlib.ExitStack()
          wgu = [sb(sWe, "wgu%d" % i, [128, KC, 1024], BF16) for i in range(2)]
          wdn = [sb(sWe, "wdn%d" % i, [128, 4, D], BF16) for i in range(2)]
          Bwe = [Buf("we%d" % i) for i in range(2)]

          def e_wloads(e):
              b = e % 2
              P.dma("gpsimd", wgu[b][:, 0:8, :], chunked(w_gu[e])[:, 0:8, :], [], [Bwe[b]], Bwe[b])
              P.dma("gpsimd", wgu[b][:, 8:16, :], chunked(w_gu[e])[:, 8:16, :], [], [Bwe[b]], Bwe[b])
              P.dma("gpsimd", wdn[b][:], chunked(w_dn[e]), [], [Bwe[b]], Bwe[b])

          if stage >= 5:
              e_wloads(0)
              e_wloads(1)
          with contextlib.ExitStack() as sR:
            def W_(name, k):
                return sb(sR, name, [128, 32, k], F32)
            LG = W_("LG", 36)
            OHG = W_("OHG", 4)
            EG = W_("EG", 4)
            ESEL = W_("ESEL", 8)
            T8 = W_("T8", 8)
            OH1 = W_("OH1", 8)
            E2 = W_("E2", 8)
            OH2 = W_("OH2", 8)
            A1 = W_("A1", 32)
            A2 = W_("A2", 32)
            POS = W_("POS", 32)
            TMP = W_("TMP", 32)
            CNT = W_("CNT", 32)
            CAR = W_("CAR", 32)
            AbA = sb(sR, "AbA", [128, 32, 32], BF16)
            S_ = sb(sR, "Ssc", [128, 16, 32], F32)
            DST = sb(sR, "DST", [128, 64], I32)
            PAY = sb(sR, "PAY", [128, 64, TW], F32)
            brb2 = sb(sR, "brb2", [128, 36], F32)
            BR = Buf("R")
            Bbr2 = Buf("brb2")
            BPAY = Buf("PAY")
            P.dma("sync", brb2[:], b_r, [], [Bbr2], Bbr2)
            P.op("gpsimd", lambda e: e.memset(PAY[:], 0.0), [], [BPAY])
            RW = ([BR], [BR])
            sc = lambda i_: S_[:, i_, :]
            bc2 = lambda ap2, k_: ap2.unsqueeze(2).to_broadcast([128, 32, k_])
            flat = lambda t3: t3[:].rearrange("p t e -> p (t e)")
            vop = lambda fn, R=RW[0], Wr=RW[1]: P.op("vector", fn, R, Wr)
            P.act(sc(0), SSq[:], AF.Sqrt, [BLG], [BR], bias=EPS, scale=1.0 / D)
            vop(lambda e: e.reciprocal(out=sc(0), in_=sc(0)))
            P.tt("vector", LG[:], LGraw[:], bc2(sc(0), 36), ALU.mult, [BLG, BR], [BR])
            P.tt("vector", LG[:], LG[:], brb2[:].unsqueeze(1).to_broadcast([128, 32, 36]), ALU.add, [BR, Bbr2], [BR])
            vop(lambda e: e.reduce_max(out=sc(1), in_=LG[:, :, 0:4], axis=AX.X))
            P.tt("vector", OHG[:], LG[:, :, 0:4], bc2(sc(1), 4), ALU.is_equal, *RW)
            P.tt("vector", EG[:], LG[:, :, 0:4], bc2(sc(1), 4), ALU.subtract, *RW)
            P.act(EG[:], EG[:], AF.Exp, *RW)
            vop(lambda e: e.reduce_sum(out=sc(2), in_=EG[:], axis=AX.X))
            vop(lambda e: e.reciprocal(out=sc(3), in_=sc(2)))
            P.tt("vector", ESEL[:], LG[:, :, 4:12], bc2(OHG[:, :, 0], 8), ALU.mult, *RW)
            for g in range(1, 4):
                P.tt("vector", T8[:], LG[:, :, 4 + 8 * g:12 + 8 * g], bc2(OHG[:, :, g], 8), ALU.mult, *RW)
                P.tt("vector", ESEL[:], ESEL[:], T8[:], ALU.add, *RW)
            vop(lambda e: e.reduce_max(out=sc(4), in_=ESEL[:], axis=AX.X))
            P.tt("vector", OH1[:], ESEL[:], bc2(sc(4), 8), ALU.is_equal, *RW)
            P.stt(flat(E2), flat(OH1), -1e30, flat(ESEL), ALU.mult, ALU.add, *RW)
            vop(lambda e: e.reduce_max(out=sc(5), in_=E2[:], axis=AX.X))
            P.tt("vector", OH2[:], E2[:], bc2(sc(5), 8), ALU.is_equal, *RW)
            P.tt("vector", sc(6), sc(5), sc(4), ALU.subtract, *RW)
            P.act(sc(6), sc(6), AF.Exp, *RW)
            P.ts("vector", sc(6), sc(6), 1.0, None, ALU.add, None, *RW)
            vop(lambda e: e.reciprocal(out=sc(6), in_=sc(6)))
            P.tt("vector", sc(7), sc(6), sc(3), ALU.mult, *RW)
            P.tt("vector", sc(8), sc(3), sc(7), ALU.subtract, *RW)
            for g in range(4):
                P.tt("vector", A1[:, :, 8 * g:8 * g + 8], OH1[:], bc2(OHG[:, :, g], 8), ALU.mult, *RW)
                P.tt("vector", A2[:, :, 8 * g:8 * g + 8], OH2[:], bc2(OHG[:, :, g], 8), ALU.mult, *RW)
            P.tt("vector", AbA[:], A1[:], A2[:], ALU.add, *RW)
            for h_ in range(2):
                P.mm(ps[h_][:], usb, flat(AbA)[:, 512 * h_:512 * h_ + 512], True, True, [BR, Bc], [Bps[h_]])
                P.mm(ps[2 + h_][:], onesb, flat(AbA)[:, 512 * h_:512 * h_ + 512], True, True, [BR, Bc], [Bps[2 + h_]])
            for h_ in range(2):
                P.cp("scalar", flat(CNT)[:, 512 * h_:512 * h_ + 512], ps[2 + h_][:], [Bps[2 + h_]], [BR])
            vop(lambda e: e.memset(CAR[:, 0, :], 0.0))
            for t_ in range(1, 32):
                P.tt("vector", CAR[:, t_, :], CAR[:, t_ - 1, :], CNT[:, t_ - 1, :], ALU.add, *RW)
            for h_ in range(2):
                P.tt("vector", flat(POS)[:, 512 * h_:512 * h_ + 512], ps[h_][:], flat(CAR)[:, 512 * h_:512 * h_ + 512], ALU.add,
                     [Bps[h_], BR], [BR])
            P.ts("vector", flat(POS), flat(POS), float(CAP), None, ALU.min, None, *RW)
            P.tt("vector", POS[:], POS[:], cf[:, C_ES:C_ES + 32].unsqueeze(1).to_broadcast([128, 32, 32]), ALU.add, [BR, Bc], [BR])
            tokf = cf[:, C_TOK:C_TOK + 32]
            for kk in range(2):
                AK = A1 if kk == 0 else A2
                P.tt("vector", TMP[:], POS[:], AK[:], ALU.mult, *RW)
                vop(lambda e, kk=kk: e.reduce_sum(out=sc(9 + kk), in_=TMP[:], axis=AX.X))
                P.cp("vector", DST[:, kk * 32:(kk + 1) * 32], sc(9 + kk), [BR], [BR])
                P.cp("vector", PAY[:, kk * 32:(kk + 1) * 32, 0], tokf, [Bc], [BPAY])
                if kk == 0:
                    P.cp("vector", PAY[:, 0:32, 1], tokf, [Bc], [BPAY])
                else:
                    P.ts("vector", PAY[:, 32:64, 1], tokf, float(T), None, ALU.add, None, [Bc], [BPAY])
                P.cp("vector", PAY[:, kk * 32:(kk + 1) * 32, 2], sc(7 + kk), [BR], [BPAY])
            Bsc = Buf("scat")
            for j_ in range(64):
                P.op("gpsimd", lambda e, j_=j_: e.indirect_dma_start(
                    out=tab_d, out_offset=bass.IndirectOffsetOnAxis(ap=DST[:, j_:j_ + 1], axis=0),
                    in_=PAY[:, j_, :], in_offset=None), [BPAY, BR], [Bd], dma=Bsc)
            P.barrier()
            P.emit()

        if stage >= 5:
          with contextlib.ExitStack() as sE:
            tb3 = [sb(sE, "tb3_%d" % i, [128, 3, TW], F32) for i in range(2)]
            Btb3 = [Buf("tb3_%d" % i) for i in range(2)]
            idx = [sb(sE, "idx%d" % i, [128, 3, 2], I32) for i in range(2)]
            Bidx = [Buf("idx%d" % i) for i in range(2)]
            xg = [[sb(sE, "xg%d_%d" % (i, s_), [128, D], BF16) for s_ in range(3)] for i in range(2)]
            Bxg = [[Buf("xg%d_%d" % (i, s_)) for s_ in range(3)] for i in range(2)]
            xgT = sb(sE, "xgT", [128, KC, CAP], BF16)
            BxgT = Buf("xgT")
            hTe = sb(sE, "hTe", [128, 4, CAP], BF16)
            BhTe = Buf("hTe")
            sg = [sb(sE, "sg%d" % i, [128, CAP], F32) for i in range(2)]
            Bsg = [Buf("sg%d" % i) for i in range(2)]
            ys = [sb(sE, "ys%d" % i, [128, D], F32) for i in range(3)]
            Bys = [Buf("ys%d" % i) for i in range(3)]

            def e_loads(e):
                b = e % 2
                if e >= 2:
                    e_wloads(e)
                P.dma("sync", tb3[b][:], tab_d[e * CS:e * CS + CAP, :].rearrange("(s p) w -> p s w", p=128),
                      [], [Btb3[b]], Btb3[b])
                P.cp("vector", idx[b][:], tb3[b][:, :, 0:2], [Btb3[b]], [Bidx[b]])
                for s_ in range(3):
                    P.op("gpsimd", lambda eng, b=b, s_=s_: eng.indirect_dma_start(
                        out=xg[b][s_][:], out_offset=None, in_=hn_d,
                        in_offset=bass.IndirectOffsetOnAxis(ap=idx[b][:, s_, 0:1], axis=0)),
                        [Bidx[b]], [Bxg[b][s_]], dma=Bxg[b][s_])

            e_loads(0)
            k = 0
            for e in range(NE):
                b = e % 2
                if e + 1 < NE:
                    e_loads(e + 1)
                for s_ in range(3):
                    for c4 in range(4):
                        pti = k % 2
                        k += 1
                        for cc in range(4):
                            c = c4 * 4 + cc
                            P.tr(pt[pti][:, cc * 128:(cc + 1) * 128], xg[b][s_][:, c * 128:(c + 1) * 128], identb,
                                 [Bxg[b][s_], Bc], [Bpt[pti]])
                        evac(xgT[:, c4 * 4:c4 * 4 + 4, s_ * 128:(s_ + 1) * 128],
                             pt[pti][:, 0:512].rearrange("p (c t) -> p c t", c=4), [Bpt[pti]], [BxgT])
                for m in range(4):
                    pg_, pu_ = (2 * m) % 4, (2 * m + 1) % 4
                    sgi = m % 2
                    for c in range(KC):
                        P.mm(ps[pg_][:, 0:CAP], wgu[b][:, c, m * 128:(m + 1) * 128], xgT[:, c, :], c == 0, c == KC - 1,
                             [Bwe[b], BxgT], [Bps[pg_]])
                    for c in range(KC):
                        P.mm(ps[pu_][:, 0:CAP], wgu[b][:, c, 512 + m * 128:512 + (m + 1) * 128], xgT[:, c, :], c == 0, c == KC - 1,
                             [Bwe[b], BxgT], [Bps[pu_]])
                    P.act(sg[sgi][:], ps[pg_][:, 0:CAP], AF.Silu, [Bps[pg_]], [Bsg[sgi]])
                    P.tt("vector", hTe[:, m, :], sg[sgi][:], ps[pu_][:, 0:CAP], ALU.mult, [Bsg[sgi], Bps[pu_]], [BhTe])
                for s_ in range(3):
                    for n in range(4):
                        pi = 4 + (n % 2)
                        for m in range(4):
                            P.mm(ps[pi][:], hTe[:, m, s_ * 128:(s_ + 1) * 128], wdn[b][:, m, n * 512:(n + 1) * 512], m == 0, m == 3,
                                 [BhTe, Bwe[b]], [Bps[pi]])
                        if n % 2:
                            P.act(ys[s_][:, n * 512:(n + 1) * 512], ps[pi][:], AF.Copy, [Bps[pi], Btb3[b]], [Bys[s_]],
                                  scale=tb3[b][:, s_, 2:3])
                        else:
                            P.ts("vector", ys[s_][:, n * 512:(n + 1) * 512], ps[pi][:], tb3[b][:, s_, 2:3], None, ALU.mult, None,
                                 [Bps[pi], Btb3[b]], [Bys[s_]])
                    P.op("gpsimd", lambda eng, b=b, s_=s_: eng.indirect_dma_start(
                        out=ybuf_d, out_offset=bass.IndirectOffsetOnAxis(ap=idx[b][:, s_, 1:2], axis=0),
                        in_=ys[s_][:], in_offset=None), [Bys[s_], Bidx[b]], [Bd], dma=Bys[s_])
            P.barrier()
            P.emit()

        if stage >= 4:
            sWe.close()
        if stage >= 6:
          with contextlib.ExitStack() as sF:
            wpg = sb(sF, "wpg", [128, KC, D], BF16)
            wpe = sb(sF, "wpe", [128, 2, D], BF16)
            Bwp = Buf("wp")
            P.dma("gpsimd", wpg[:, 0:8, :], chunked(w_pg)[:, 0:8, :], [], [Bwp], Bwp)
            P.dma("gpsimd", wpg[:, 8:16, :], chunked(w_pg)[:, 8:16, :], [], [Bwp], Bwp)
            P.dma("gpsimd", wpe[:], chunked(w_pe), [], [Bwp], Bwp)
            xb = [sb(sF, "xb%d" % i, [128, KC, 256], F32) for i in range(3)]
            Bxb = [Buf("xb%d" % i) for i in range(3)]
            y0 = [sb(sF, "y0_%d" % i, [128, D], F32) for i in range(4)]
            By = [Buf("y%d" % i) for i in range(4)]
            pbf = [sb(sF, "pbf%d" % i, [128, 2, 256], BF16) for i in range(3)]
            Bpb = [Buf("pbf%d" % i) for i in range(3)]
            sqA = sb(sF, "sqA", [128, KC, 256], BF16)
            sqB = sqA
            BsqA = Buf("sqA")
            BsqB = BsqA
            srtA = sb(sF, "srtA", [128, 256], F32)
            srtB = sb(sF, "srtB", [128, 256], F32)
            BsrtA, BsrtB = Buf("srtA"), Buf("srtB")
            rstdA = sb(sF, "rstdA", [128, 256], F32)
            rstdB = sb(sF, "rstdB", [128, 256], F32)
            BrstdA, BrstdB = Buf("rstdA"), Buf("rstdB")
            hp = [sb(sF, "hp%d" % i, [128, KC, 256], BF16) for i in range(2)]
            Bhp = [Buf("hp%d" % i) for i in range(2)]
            sgt = [sb(sF, "sgt%d" % i, [128, 256], F32) for i in range(2)]
            Bsgt = [Buf("sgt%d" % i) for i in range(2)]

            def f_loads(tb):
                b = tb % 3
                sl = slice(tb * 256, (tb + 1) * 256)
                P.dma("sync", xb[b][:], chunked(x1T_d)[:, :, sl], [], [Bxb[b]], Bxb[b])
                for tt_ in range(2):
                    tile_i = tb * 2 + tt_
                    yi = (tb % 2) * 2 + tt_
                    P.dma("sync", y0[yi][:], ybuf_d[tile_i * 128:(tile_i + 1) * 128, :], [], [By[yi]], By[yi])

            def f_loads2(tb):
                b = tb % 3
                sl = slice(tb * 256, (tb + 1) * 256)
                P.dma("gpsimd", pbf[b][:], chunked(pT)[:, :, sl], [], [Bpb[b]], Bpb[b])
                for tt_ in range(2):
                    tile_i = tb * 2 + tt_
                    yi = (tb % 2) * 2 + tt_
                    P.op("gpsimd", lambda e, yi=yi, tile_i=tile_i: e.dma_start(
                        out=y0[yi][:], in_=ybuf_d[T + tile_i * 128:T + (tile_i + 1) * 128, :], accum_op=ALU.add),
                        [], [By[yi]], dma=By[yi])

            def f_front_steps(tb):
                b = tb % 3
                hb2 = tb % 2
                steps = []

                def pool_():
                    pass
                steps.append(pool_)
                for c in range(KC):
                    def st(c=c):
                        pi = c % 2
                        for tt_ in range(2):
                            yi = (tb % 2) * 2 + tt_
                            P.tr(ps[pi][:, tt_ * 128:(tt_ + 1) * 128], y0[yi][:, c * 128:(c + 1) * 128], identf, [By[yi], Bc], [Bps[pi]])
                        P.tt("vector", xb[b][:, c, :], ps[pi][:, 0:256], xb[b][:, c, :], ALU.add, [Bps[pi], Bxb[b]], [Bxb[b]])
                    steps.append(st)

                def tail_():
                    if debug:
                        P.dma("sync", chunked(dbg_x2)[:, :, tb * 256:(tb + 1) * 256], xb[b][:], [Bxb[b]], [Bd], Bxb[b])
                    rms_stats(xb[b], Bxb[b], 256, sqA, BsqA, ps[5], Bps[5], srtA, BsrtA, rstdA, BrstdA)
                    for c in range(KC):
                        P.stt(hp[hb2][:, c, :], xb[b][:, c, :], gv[:, G_PLE + c:G_PLE + c + 1], rstdA[:], ALU.mult, ALU.mult,
                              [Bxb[b], BrstdA, Bc], [Bhp[hb2]])
                steps.append(tail_)
                return steps

            def f_back_steps(tb):
                b = tb % 3
                hb2 = tb % 2
                steps = []
                for fg in range(KC):
                    def st(fg=fg):
                        si_ = fg % 2
                        pg_ = 2 + (fg % 2)
                        for c in range(KC):
                            P.mm(ps[pg_][:, 0:256], wpg[:, c, fg * 128:(fg + 1) * 128], hp[hb2][:, c, :], c == 0, c == KC - 1, [Bwp, Bhp[hb2]], [Bps[pg_]])
                        for c in range(2):
                            P.mm(ps[4][:, 0:256], wpe[:, c, fg * 128:(fg + 1) * 128], pbf[b][:, c, :], c == 0, c == 1, [Bwp, Bpb[b]], [Bps[4]])
                        P.act(sgt[si_][:], ps[pg_][:, 0:256], AF.Sigmoid, [Bps[pg_]], [Bsgt[si_]])
                        P.tt("vector", sgt[si_][:], sgt[si_][:], ps[4][:, 0:256], ALU.mult, [Bsgt[si_], Bps[4]], [Bsgt[si_]])
                        P.tt("gpsimd", xb[b][:, fg, :], xb[b][:, fg, :], sgt[si_][:], ALU.add, [Bsgt[si_], Bxb[b]], [Bxb[b]])
                    steps.append(st)

                def tail_():
                    rms_stats(xb[b], Bxb[b], 256, sqB, BsqB, ps[5], Bps[5], srtB, BsrtB, rstdB, BrstdB)
                    for c in range(KC):
                        P.stt(xb[b][:, c, :], xb[b][:, c, :], gv[:, G_FIN + c:G_FIN + c + 1], rstdB[:], ALU.mult, ALU.mult,
                              [Bxb[b], BrstdB, Bc], [Bxb[b]])
                    P.dma("sync", chunked(yT)[:, :, tb * 256:(tb + 1) * 256], xb[b][:], [Bxb[b]], [Bd], Bxb[b])
                steps.append(tail_)
                return steps

            f_loads(0)
            f_loads2(0)
            f_loads(1)
            f_loads2(1)
            for st_ in f_front_steps(0):
                st_()
            for tb in range(16):
                Fs = []
                if tb + 2 < 16:
                    f_loads(tb + 2)
                if tb + 1 < 16:
                    Fs = f_front_steps(tb + 1)
                Bs = f_back_steps(tb)
                if Fs:
                    Fs[0]()
                for i_ in range(KC):
                    Bs[i_]()
                    if Fs and i_ < 8:
                        Fs[1 + 2 * i_]()
                        Fs[2 + 2 * i_]()
                    if Fs and i_ == 8:
                        Fs[17]()
                Bs[16]()
                if tb + 2 < 16:
                    f_loads2(tb + 2)
            P.barrier()
            P.emit()
        if stage < 6:
            pass
    return nc


def _consts():
    cf = np.zeros((128, NCF), np.float32)
    cf[:, C_ID:C_ID + 128] = np.eye(128, dtype=np.float32)
    cf[:, C_ONE:C_ONE + 128] = 1.0
    k = np.arange(128)[:, None]
    q = np.arange(128)[None, :]
    cf[:, C_TRI:C_TRI + 128] = (q >= k)
    cf[:, C_US:C_US + 128] = (k < q)
    cf[:, C_IOTA] = np.arange(128)
    cf[:, C_ES:C_ES + 32] = (np.arange(32) * CS)[None, :]
    cf[:, C_TOK:C_TOK + 32] = np.arange(32)[None, :] * 128 + np.arange(128)[:, None]
    sel8 = np.zeros((8, 1032), np.float32)
    for h in range(8):
        sel8[h, h * 128:(h + 1) * 128] = 1.0
        sel8[h, 1024 + h] = 1.0
    tabinit = np.zeros((128, TW), np.float32)
    tabinit[:, 1] = 2 * T
    return cf, sel8, tabinit


def _shared_inputs(inp):
    f = lambda a: np.ascontiguousarray(a, dtype=np.float32)
    cf, sel8, tabinit = _consts()
    gl = lambda g: g.reshape(16, 128).T
    gvec = np.concatenate([gl(inp["g_mix"][0]), gl(inp["g_ffn"][0]), gl(inp["g_ple"][0]), gl(inp["g_final"])], axis=1)
    cw = inp["conv_w"][0].reshape(3, 8, 128).transpose(2, 1, 0).reshape(128, 24)
    w_r = np.concatenate([inp["w_router_group"][0], inp["w_router_expert"][0]], axis=1)
    b_r = np.concatenate([inp["b_router_group"][0], inp["b_router_expert"][0]])[None, :].repeat(128, axis=0)
    return {
        "w_in": f(inp["w_in"][0]), "gvec": f(gvec), "b_f": f(inp["b_f"][0].reshape(8, 1)), "conv_w": f(cw),
        "w_a": f(inp["w_branch_a"][0]), "w_b": f(inp["w_branch_b"][0]), "w_o": f(inp["w_out"][0]),
        "w_r": f(w_r), "b_r": f(b_r), "w_gu": f(inp["w_gate_up"][0]), "w_dn": f(inp["w_down"][0]),
        "w_pg": f(inp["w_ple_gate"][0]), "w_pe": f(inp["w_ple_proj"][0]),
        "cf": cf, "sel8": sel8, "tabinit": tabinit,
    }


def _core_inputs(inp, b, shared):
    m = dict(shared)
    m["xT"] = np.ascontiguousarray(np.asarray(inp["x"][b], dtype=np.float32).T)
    m["pT"] = np.ascontiguousarray(np.asarray(inp["p"][0, b], dtype=np.float32).T)
    return m


def kernel(**inputs):
    inp = {k: np.asarray(v) for k, v in inputs.items()}
    nb = inp["x"].shape[0]
    shared = _shared_inputs(inp)
    nc = build()
    in_maps = [_core_inputs(inp, b, shared) for b in range(nb)]
    res = run_bass_kernel_spmd(nc, in_maps, core_ids=list(range(nb)))
    out = np.empty((nb, T, D), np.float32)
    for b in range(nb):
        out[b] = np.asarray(res.results[b]["yT"]).T
    return out
```
